# Optimizing a Trainium2 kernel written in Bass

```python
import math
import jax
import jax.numpy as jnp
from jax import lax
import numpy as np


D_MODEL = 1024
BATCH = 16
SEQ = 4096
DEPTH = 2

GRID_W = 64
CTX_LEN = 256
N_MOD = 6
NORM_EPS = 1e-6
NEG_INF = -1e30

FNET_GROUPS = 4
FNET_GROUP_DIM = D_MODEL // 8
FNET_WIDTH = FNET_GROUPS * FNET_GROUP_DIM
HY_WIDTH = D_MODEL // 2
HY_EMB_BANDS = 16
HY_EMB_DIM = 1 + 2 * HY_EMB_BANDS
HY_FILTER_HIDDEN = 64
HY_SHORT_CONV = 3
HY_DECAY_TARGET = 1e-2
HY_FAST_DECAY = 0.3
HY_SLOW_DECAY = 1.5
EVEN_IN_WIDTH = FNET_WIDTH + 3 * HY_WIDTH
EVEN_MIX_WIDTH = FNET_WIDTH + HY_WIDTH

HEAD_DIM = 64
N_HEADS = D_MODEL // HEAD_DIM
N_KV_HEADS = 4
GROUP = N_HEADS // N_KV_HEADS
Q_WIDTH = N_HEADS * HEAD_DIM
KV_WIDTH = N_KV_HEADS * HEAD_DIM
WINDOW = 128
BLOCK_Q = 128
ROPE_THETA = 10000.0
AXIS_ROPE_DIM = HEAD_DIM // 2

D_FF = 7 * D_MODEL // 2
N_EXPERTS = 8
TOP_K = 2
EXPERT_BLOCK = 256

kernel_name = 'hybrid_fnet_hyena_swa_moe_dit'


def _rmsnorm(t, g):
    t32 = t.astype(jnp.float32)
    y = t32 * lax.rsqrt(jnp.mean(t32 * t32, axis=-1, keepdims=True) + NORM_EPS)
    return (y * g.astype(jnp.float32)).astype(t.dtype)


def _modulate(h, shift, scale):
    return h * (1 + scale) + shift


def _swiglu(h, wg, wu, wd):
    return (jax.nn.silu(h @ wg) * (h @ wu)) @ wd


def _short_conv(u, w, b):
    up = jnp.pad(u, ((0, 0), (1, 1), (0, 0)))
    return up[:, :-2] * w[0] + up[:, 1:-1] * w[1] + up[:, 2:] * w[2] + b


def _hyena_filter_spectrum(L, fw0, fb0, fw1, fb1, fw2, fb2, fw3, freq):
    f32 = jnp.float32
    pos = jnp.arange(L, dtype=f32)
    t = pos / max(L - 1, 1)
    w = (2.0 * math.pi / L) * pos
    bands = jnp.linspace(1e-4, HY_EMB_BANDS - 1, HY_EMB_BANDS, dtype=f32)
    ang = w[:, None] * bands[None, :]
    z = jnp.concatenate([t[:, None], jnp.cos(ang), -jnp.sin(ang)], axis=-1)
    fr = freq.astype(f32)
    h = jnp.sin(fr * (z @ fw0.astype(f32) + fb0.astype(f32)))
    h = jnp.sin(fr * (h @ fw1.astype(f32) + fb1.astype(f32)))
    h = jnp.sin(fr * (h @ fw2.astype(f32) + fb2.astype(f32)))
    h = (h @ fw3.astype(f32)).reshape(L, 2, HY_WIDTH)
    deltas = jnp.abs(jnp.linspace(math.log(HY_DECAY_TARGET) / HY_SLOW_DECAY,
                                  math.log(HY_DECAY_TARGET) / HY_FAST_DECAY, HY_WIDTH, dtype=f32))
    h = h * jnp.exp(-t[:, None, None] * deltas)
    h_fwd, h_bwd = h[:, 0], h[:, 1]
    k = jnp.concatenate([h_fwd, jnp.zeros((1, HY_WIDTH), f32), h_bwd[1:][::-1]], axis=0)
    k = k * lax.rsqrt(jnp.sum(k * k, axis=0, keepdims=True) + NORM_EPS)
    return jnp.fft.rfft(k, axis=0)


def _fft_long_conv(u, k_spec, bias):
    L = u.shape[1]
    y = jnp.fft.irfft(jnp.fft.rfft(u, n=2 * L, axis=1) * k_spec[None], n=2 * L, axis=1)[:, :L]
    return y + u * bias


def _fourier_mix(u_a):
    b, L, _ = u_a.shape
    ua = u_a.astype(jnp.float32).reshape(b, L, FNET_GROUPS, FNET_GROUP_DIM)
    return jnp.fft.fft2(ua, axes=(1, 3), norm='ortho').real.reshape(b, L, FNET_WIDTH)


def _hyena_mix(u_b, conv_w, conv_b, bias, fw0, fb0, fw1, fb1, fw2, fb2, fw3, freq):
    f32 = jnp.float32
    L = u_b.shape[1]
    z = _short_conv(u_b.astype(f32), conv_w.astype(f32), conv_b.astype(f32))
    v, x1, x0 = jnp.split(z, 3, axis=-1)
    k_spec = _hyena_filter_spectrum(L, fw0, fb0, fw1, fb1, fw2, fb2, fw3, freq)
    return x0 * _fft_long_conv(v * x1, k_spec, bias.astype(f32))


def _even_mixer(h, w_in, w_out, conv_w, conv_b, hy_bias, fw0, fb0, fw1, fb1, fw2, fb2, fw3, freq):
    u = h @ w_in
    a = _fourier_mix(u[..., :FNET_WIDTH])
    b = _hyena_mix(u[..., FNET_WIDTH:], conv_w, conv_b, hy_bias, fw0, fb0, fw1, fb1, fw2, fb2, fw3, freq)
    return jnp.concatenate([a, b], axis=-1).astype(h.dtype) @ w_out


def _axial_rope(t, row, col):
    inv = ROPE_THETA ** (-jnp.arange(0, AXIS_ROPE_DIM, 2, dtype=jnp.float32) / AXIS_ROPE_DIM)

    def rot(seg, p):
        ang = p[:, None] * inv[None, :]
        cos = jnp.cos(ang)[None, :, None, :]
        sin = jnp.sin(ang)[None, :, None, :]
        s1, s2 = jnp.split(seg, 2, axis=-1)
        return jnp.concatenate([s1 * cos - s2 * sin, s1 * sin + s2 * cos], axis=-1)

    return jnp.concatenate([rot(t[..., :AXIS_ROPE_DIM], row), rot(t[..., AXIS_ROPE_DIM:], col)], axis=-1)


def _sink_attend(q, keys, values, masks, sink):
    scale = HEAD_DIM ** -0.5
    scores = []
    for k, m in zip(keys, masks):
        s = jnp.einsum('bqhgd,bkhd->bhgqk', q, k) * scale
        if m is not None:
            s = jnp.where(m, s, NEG_INF)
        scores.append(s)
    b, nq = q.shape[0], q.shape[1]
    sink_col = jnp.broadcast_to(sink.astype(jnp.float32).reshape(1, N_KV_HEADS, GROUP, 1, 1),
                                (b, N_KV_HEADS, GROUP, nq, 1))
    p = jax.nn.softmax(jnp.concatenate(scores + [sink_col], axis=-1), axis=-1)
    off = 0
    out = None
    for v in values:
        n = v.shape[1]
        o = jnp.einsum('bhgqk,bkhd->bqhgd', p[..., off:off + n], v)
        out = o if out is None else out + o
        off += n
    return out


def _window_attention(q, k, v, k_ctx, v_ctx, sink):
    b, L = q.shape[0], q.shape[1]
    nb = L // BLOCK_Q
    qb = jnp.moveaxis(q.reshape(b, nb, BLOCK_Q, N_KV_HEADS, GROUP, HEAD_DIM), 1, 0)

    def band(t):
        tp = jnp.pad(t, ((0, 0), (BLOCK_Q, BLOCK_Q), (0, 0), (0, 0)))
        tp = tp.reshape(b, nb + 2, BLOCK_Q, N_KV_HEADS, HEAD_DIM)
        t3 = jnp.concatenate([tp[:, :-2], tp[:, 1:-1], tp[:, 2:]], axis=2)
        return jnp.moveaxis(t3, 1, 0)

    kb, vb = band(k), band(v)
    offs_q = jnp.arange(BLOCK_Q)
    offs_k = jnp.arange(3 * BLOCK_Q) - BLOCK_Q

    def body(args):
        blk, qi, ki, vi = args
        qpos = blk * BLOCK_Q + offs_q
        kpos = blk * BLOCK_Q + offs_k
        m = ((kpos[None, :] >= 0) & (kpos[None, :] < L)
             & (jnp.abs(qpos[:, None] - kpos[None, :]) <= WINDOW))
        return _sink_attend(qi, [ki, k_ctx], [vi, v_ctx], [m, None], sink)

    o = lax.map(body, (jnp.arange(nb), qb, kb, vb))
    return jnp.moveaxis(o, 0, 1).reshape(b, L, Q_WIDTH)


def _heads(t, n):
    return t.astype(jnp.float32).reshape(t.shape[0], t.shape[1], n, HEAD_DIM)


def _odd_mixer(hx, hc, w_qkv, w_out, q_g, k_g, sink, need_ctx_out):
    dt = hx.dtype
    b, L, _ = hx.shape
    lc = hc.shape[1]
    qkv = hx @ w_qkv
    q = _rmsnorm(_heads(qkv[..., :Q_WIDTH], N_HEADS), q_g)
    k = _rmsnorm(_heads(qkv[..., Q_WIDTH:Q_WIDTH + KV_WIDTH], N_KV_HEADS), k_g)
    v = _heads(qkv[..., Q_WIDTH + KV_WIDTH:], N_KV_HEADS)
    rows = L // GRID_W
    row = jnp.repeat(jnp.arange(rows, dtype=jnp.float32), GRID_W)
    col = jnp.tile(jnp.arange(GRID_W, dtype=jnp.float32), rows)
    q = _axial_rope(q, row, col).reshape(b, L, N_KV_HEADS, GROUP, HEAD_DIM)
    k = _axial_rope(k, row, col)
    c_off = 0 if need_ctx_out else Q_WIDTH
    cp = hc @ w_qkv[:, c_off:]
    kc = _rmsnorm(_heads(cp[..., Q_WIDTH - c_off:Q_WIDTH - c_off + KV_WIDTH], N_KV_HEADS), k_g)
    vc = _heads(cp[..., Q_WIDTH - c_off + KV_WIDTH:], N_KV_HEADS)
    o_lat = _window_attention(q, k, v, kc, vc, sink).astype(dt) @ w_out
    o_ctx = None
    if need_ctx_out:
        qc = _rmsnorm(_heads(cp[..., :Q_WIDTH], N_HEADS), q_g).reshape(b, lc, N_KV_HEADS, GROUP, HEAD_DIM)
        o_ctx = _sink_attend(qc, [kc], [vc], [None], sink).reshape(b, lc, Q_WIDTH).astype(dt) @ w_out
    return o_lat, o_ctx


def _moe_swiglu(h, w_router, w_gate, w_up, w_down):
    dt = h.dtype
    b, L, d = h.shape
    n = b * L
    xf = h.reshape(n, d)
    logits = (xf @ w_router).astype(jnp.float32)
    top_val, top_idx = lax.top_k(logits, TOP_K)
    gates = jax.nn.softmax(top_val, axis=-1)
    e_flat = top_idx.reshape(-1)
    t_flat = jnp.repeat(jnp.arange(n, dtype=jnp.int32), TOP_K)
    g_flat = gates.reshape(-1)
    order = jnp.argsort(e_flat)
    e_sorted = e_flat[order]
    counts = jnp.bincount(e_flat, length=N_EXPERTS)
    padded = (counts + EXPERT_BLOCK - 1) // EXPERT_BLOCK * EXPERT_BLOCK
    start = jnp.cumsum(counts) - counts
    pend = jnp.cumsum(padded)
    pstart = pend - padded
    dest = pstart[e_sorted] + jnp.arange(n * TOP_K) - start[e_sorted]
    n_slots = (n * TOP_K + EXPERT_BLOCK - 1) // EXPERT_BLOCK * EXPERT_BLOCK + N_EXPERTS * EXPERT_BLOCK
    n_blocks = n_slots // EXPERT_BLOCK
    tok_buf = jnp.full((n_slots,), n, dtype=jnp.int32).at[dest].set(t_flat[order])
    gate_buf = jnp.zeros((n_slots,), jnp.float32).at[dest].set(g_flat[order])
    block_e = jnp.minimum(jnp.searchsorted(pend, jnp.arange(n_blocks) * EXPERT_BLOCK, side='right'),
                          N_EXPERTS - 1)
    xpad = jnp.concatenate([xf, jnp.zeros((1, d), dt)], axis=0)

    def block(args):
        tok, e = args
        xb = xpad[tok]
        return _swiglu(xb, w_gate[e], w_up[e], w_down[e])

    yb = lax.map(block, (tok_buf.reshape(n_blocks, EXPERT_BLOCK), block_e))
    y = jnp.zeros((n + 1, d), dt).at[tok_buf].add(yb.reshape(n_slots, d) * gate_buf[:, None].astype(dt))
    return y[:n].reshape(b, L, d)


def setup_inputs(seed: int = 0) -> dict:
    key = jax.random.key(seed)
    ks = iter(jax.random.split(key, 40))
    n_ev = (DEPTH + 1) // 2
    n_od = DEPTH // 2
    D = D_MODEL

    def nrm(shape, scale=1.0):
        return jax.random.normal(next(ks), shape, jnp.float32) * scale

    return {
        'x': nrm((BATCH, SEQ, D)),
        'c': nrm((BATCH, D)),
        'ctx': nrm((BATCH, CTX_LEN, D)),
        'c_ctx': nrm((D,)),
        'ada_w': nrm((DEPTH, D, N_MOD * D), 0.5 * D ** -0.5),
        'ada_b': nrm((DEPTH, N_MOD * D), 0.01),
        'norm1_g': 1.0 + nrm((DEPTH, D), 0.02),
        'norm2_g': 1.0 + nrm((DEPTH, D), 0.02),
        'ev_w_in': nrm((n_ev, D, EVEN_IN_WIDTH), D ** -0.5),
        'ev_w_out': nrm((n_ev, EVEN_MIX_WIDTH, D), EVEN_MIX_WIDTH ** -0.5),
        'hy_conv_w': nrm((n_ev, HY_SHORT_CONV, 3 * HY_WIDTH), HY_SHORT_CONV ** -0.5),
        'hy_conv_b': nrm((n_ev, 3 * HY_WIDTH), 0.02),
        'hy_bias': nrm((n_ev, HY_WIDTH), 0.5),
        'hf_w0': nrm((n_ev, HY_EMB_DIM, HY_FILTER_HIDDEN), HY_EMB_DIM ** -0.5),
        'hf_b0': nrm((n_ev, HY_FILTER_HIDDEN), 0.1),
        'hf_w1': nrm((n_ev, HY_FILTER_HIDDEN, HY_FILTER_HIDDEN), HY_FILTER_HIDDEN ** -0.5),
        'hf_b1': nrm((n_ev, HY_FILTER_HIDDEN), 0.1),
        'hf_w2': nrm((n_ev, HY_FILTER_HIDDEN, HY_FILTER_HIDDEN), HY_FILTER_HIDDEN ** -0.5),
        'hf_b2': nrm((n_ev, HY_FILTER_HIDDEN), 0.1),
        'hf_w3': nrm((n_ev, HY_FILTER_HIDDEN, 2 * HY_WIDTH), HY_FILTER_HIDDEN ** -0.5),
        'hf_freq': 1.0 + nrm((n_ev, HY_FILTER_HIDDEN), 0.1),
        'ffn_w_gate': nrm((n_ev, D, D_FF), D ** -0.5),
        'ffn_w_up': nrm((n_ev, D, D_FF), D ** -0.5),
        'ffn_w_down': nrm((n_ev, D_FF, D), D_FF ** -0.5),
        'od_w_qkv': nrm((n_od, D, Q_WIDTH + 2 * KV_WIDTH), D ** -0.5),
        'od_w_out': nrm((n_od, Q_WIDTH, D), Q_WIDTH ** -0.5),
        'q_norm_g': 1.0 + nrm((n_od, HEAD_DIM), 0.02),
        'k_norm_g': 1.0 + nrm((n_od, HEAD_DIM), 0.02),
        'attn_sink': nrm((n_od, N_HEADS), 0.5),
        'moe_router': nrm((n_od, D, N_EXPERTS), D ** -0.5),
        'moe_w_gate': nrm((n_od, N_EXPERTS, D, D_FF), D ** -0.5),
        'moe_w_up': nrm((n_od, N_EXPERTS, D, D_FF), D ** -0.5),
        'moe_w_down': nrm((n_od, N_EXPERTS, D_FF, D), D_FF ** -0.5),
    }


def reference(x, c, ctx, c_ctx, ada_w, ada_b, norm1_g, norm2_g, ev_w_in, ev_w_out,
              hy_conv_w, hy_conv_b, hy_bias, hf_w0, hf_b0, hf_w1, hf_b1, hf_w2, hf_b2, hf_w3, hf_freq,
              ffn_w_gate, ffn_w_up, ffn_w_down, od_w_qkv, od_w_out, q_norm_g, k_norm_g, attn_sink,
              moe_router, moe_w_gate, moe_w_up, moe_w_down):
    for i in range(DEPTH):
        j = i // 2
        last = i == DEPTH - 1
        ml = jnp.split((jax.nn.silu(c) @ ada_w[i] + ada_b[i])[:, None, :], N_MOD, axis=-1)
        mc = jnp.split((jax.nn.silu(c_ctx) @ ada_w[i] + ada_b[i])[None, None, :], N_MOD, axis=-1)
        hx = _modulate(_rmsnorm(x, norm1_g[i]), ml[0], ml[1])
        hc = _modulate(_rmsnorm(ctx, norm1_g[i]), mc[0], mc[1])
        if i % 2 == 0:
            ev = (ev_w_in[j], ev_w_out[j], hy_conv_w[j], hy_conv_b[j], hy_bias[j],
                  hf_w0[j], hf_b0[j], hf_w1[j], hf_b1[j], hf_w2[j], hf_b2[j], hf_w3[j], hf_freq[j])
            x = x + ml[2] * _even_mixer(hx, *ev)
            if not last:
                ctx = ctx + mc[2] * _even_mixer(hc, *ev)
            hx2 = _modulate(_rmsnorm(x, norm2_g[i]), ml[3], ml[4])
            x = x + ml[5] * _swiglu(hx2, ffn_w_gate[j], ffn_w_up[j], ffn_w_down[j])
            if not last:
                hc2 = _modulate(_rmsnorm(ctx, norm2_g[i]), mc[3], mc[4])
                ctx = ctx + mc[5] * _swiglu(hc2, ffn_w_gate[j], ffn_w_up[j], ffn_w_down[j])
        else:
            o_lat, o_ctx = _odd_mixer(hx, hc, od_w_qkv[j], od_w_out[j], q_norm_g[j], k_norm_g[j],
                                      attn_sink[j], not last)
            x = x + ml[2] * o_lat
            if not last:
                ctx = ctx + mc[2] * o_ctx
            hx2 = _modulate(_rmsnorm(x, norm2_g[i]), ml[3], ml[4])
            x = x + ml[5] * _moe_swiglu(hx2, moe_router[j], moe_w_gate[j], moe_w_up[j], moe_w_down[j])
            if not last:
                hc2 = _modulate(_rmsnorm(ctx, norm2_g[i]), mc[3], mc[4])
                ctx = ctx + mc[5] * _moe_swiglu(hc2, moe_router[j], moe_w_gate[j], moe_w_up[j], moe_w_down[j])
    return x
```

```python
import math
import numpy as np
from contextlib import ExitStack
import ml_dtypes
import concourse.bass as bass
import concourse.mybir as mybir
from concourse.bass_utils import run_bass_kernel_spmd

F32 = mybir.dt.float32
BF16 = mybir.dt.bfloat16
AF = mybir.ActivationFunctionType
ALU = mybir.AluOpType
NPBF = ml_dtypes.bfloat16

D = 1024
SEQ = 4096
CTX = 256
DFF = 3584
NE = 8
EPS = 1e-6
PI = math.pi
SBLK = 1024
NBLK = (2 * 2 * SEQ) // SBLK + NE - 1
NSLOT = NBLK * SBLK
NFG = 14
SPARSE = True


class Prog:
    COMPUTE = ("pe", "dve", "act", "pool")

    def __init__(self, nc, es, kq=8):
        self.nc = nc
        self.engs = {"pe": nc.tensor, "dve": nc.vector, "act": nc.scalar, "pool": nc.gpsimd, "sp": nc.sync}
        self.sem = {}
        self.cnt = {}
        for e in self.COMPUTE:
            self.sem[e] = es.enter_context(nc.semaphore("s_" + e))
            self.cnt[e] = 0
        self.kq = kq
        self.dsem = {}
        self.dn = {}
        for q in ("sp", "pool", "act"):
            self.dsem[q] = [es.enter_context(nc.semaphore("d_%s%d" % (q, i))) for i in range(kq)]
            self.dn[q] = 0
        self.known = {e: {} for e in self.engs}
        self.res = {}
        self.ninst = 0

    def _deps(self, reads, writes):
        ev = {}
        for r in reads:
            st = self.res.get(r)
            if st:
                for k, v in st["w"].items():
                    if ev.get(k, 0) < v:
                        ev[k] = v
        for w in writes:
            st = self.res.get(w)
            if st:
                for k, v in st["w"].items():
                    if ev.get(k, 0) < v:
                        ev[k] = v
                for k, v in st["r"].items():
                    if ev.get(k, 0) < v:
                        ev[k] = v
        return ev

    def _semobj(self, k):
        return self.sem[k[1]] if k[0] == "c" else self.dsem[k[1]][k[2]]

    def _wait(self, e, ev, skip_key=None):
        eng = self.engs[e]
        kn = self.known[e]
        for k, v in ev.items():
            if k == skip_key or kn.get(k, 0) >= v:
                continue
            eng.wait_ge(self._semobj(k), v)
            kn[k] = v

    def _commit(self, key, val, reads, writes):
        for r in reads:
            st = self.res.setdefault(r, {"w": {}, "r": {}})
            st["r"][key] = val
        for w in writes:
            st = self.res.setdefault(w, {"w": {}, "r": {}})
            st["w"][key] = val

    def op(self, e, fn, reads=(), writes=()):
        ev = self._deps(reads, writes)
        key = ("c", e)
        self._wait(e, ev, skip_key=key if e == "pe" else None)
        ins = fn(self.engs[e])
        self.cnt[e] += 1
        ins.then_inc(self.sem[e], 1)
        self._commit(key, self.cnt[e], reads, writes)
        self.ninst += 1

    def dma(self, q, out, in_, reads=(), writes=(), **kw):
        ev = self._deps(reads, writes)
        n = self.dn[q]
        j = n % self.kq
        val = 16 * (n // self.kq + 1)
        key = ("d", q, j)
        if val > 16 and ev.get(key, 0) < val - 16:
            ev[key] = val - 16
        self._wait(q, ev)
        ins = self.engs[q].dma_start(out=out, in_=in_, **kw)
        ins.then_inc(self.dsem[q][j], 16)
        self.dn[q] = n + 1
        self._commit(key, val, reads, writes)
        self.ninst += 1

    def idma(self, out, in_, out_off, in_off, bound, reads=(), writes=()):
        q = "pool"
        ev = self._deps(reads, writes)
        n = self.dn[q]
        j = n % self.kq
        val = 16 * (n // self.kq + 1)
        key = ("d", q, j)
        if val > 16 and ev.get(key, 0) < val - 16:
            ev[key] = val - 16
        self._wait(q, ev)
        oo = bass.IndirectOffsetOnAxis(ap=out_off, axis=0) if out_off is not None else None
        io = bass.IndirectOffsetOnAxis(ap=in_off, axis=0) if in_off is not None else None
        ins = self.nc.gpsimd.indirect_dma_start(out=out, out_offset=oo, in_=in_, in_offset=io)
        ins.then_inc(self.dsem[q][j], 16)
        self.dn[q] = n + 1
        self._commit(key, val, reads, writes)
        self.ninst += 1

    def barrier(self):
        ev = {}
        for e in self.COMPUTE:
            if self.cnt[e]:
                ev[("c", e)] = self.cnt[e]
        for q in self.dsem:
            n = self.dn[q]
            for j in range(self.kq):
                cntj = (n - j + self.kq - 1) // self.kq if n > j else 0
                if cntj:
                    ev[("d", q, j)] = 16 * cntj
        for e in self.engs:
            self._wait(e, dict(ev), skip_key=None)


def _tiled_table(fn, L):
    nk = L // 128
    a = np.arange(L, dtype=np.float64)
    full = fn(a[:, None], a[None, :])
    t = full.reshape(nk, 128, nk, 128).transpose(2, 1, 0, 3)
    return np.ascontiguousarray(t).astype(NPBF)


def host_consts():
    c = {}
    c["ident_f"] = np.eye(128, dtype=np.float32)
    c["ident_b"] = np.eye(128).astype(NPBF)
    c["ones_b"] = np.ones((128, 128)).astype(NPBF)
    c["ones_f"] = np.ones((128, 128), np.float32)
    for L, tag in ((SEQ, "4096"), (CTX, "256")):
        c["thc" + tag] = _tiled_table(lambda a, b: np.cos(PI * (2 * a + 1) * (2 * b + 1) / (4 * L)), L)
        c["ths" + tag] = _tiled_table(lambda a, b: np.sin(PI * (2 * a + 1) * (2 * b + 1) / (4 * L)), L)
        c["tfc" + tag] = _tiled_table(lambda a, b: np.cos(2 * PI * ((a * b) % L) / L), L)
        c["tfs" + tag] = _tiled_table(lambda a, b: np.sin(2 * PI * ((a * b) % L) / L), L)
        nk = L // 128
        f = np.arange(L, dtype=np.float64)
        al = PI * (2 * f + 1) / (4 * L)
        c["ca" + tag] = np.ascontiguousarray((np.cos(al) / L).reshape(nk, 128).T).astype(np.float32)
        c["sa" + tag] = np.ascontiguousarray((np.sin(al) / L).reshape(nk, 128).T).astype(np.float32)
        pos = np.arange(L, dtype=np.float32)
        t = pos / np.float32(max(L - 1, 1))
        w = np.float32(2.0 * PI / L) * pos
        bands = np.linspace(1e-4, 15, 16, dtype=np.float32)
        ang = w[:, None] * bands[None, :]
        emb = np.concatenate([t[:, None], np.cos(ang), -np.sin(ang)], axis=-1).astype(np.float32)
        c["emb" + tag] = np.ascontiguousarray(emb.T)
        c["negt" + tag] = np.ascontiguousarray((-t).reshape(nk, 128).T).astype(np.float32)
        t1 = (np.arange(L, dtype=np.float32) + 1) / np.float32(max(L - 1, 1))
        c["negt1" + tag] = np.ascontiguousarray((-t1).reshape(nk, 128).T).astype(np.float32)
    deltas = np.abs(np.linspace(math.log(1e-2) / 1.5, math.log(1e-2) / 0.3, 512, dtype=np.float32))
    c["delta_bc"] = np.ascontiguousarray(np.broadcast_to(deltas[None, :], (128, 512))).astype(np.float32)
    dd = np.arange(128, dtype=np.float64)
    ang = 2 * PI * ((dd[:, None] * dd[None, :]) % 128) / 128
    c["c128"] = np.cos(ang).astype(NPBF)
    c["ns128"] = (-np.sin(ang)).astype(NPBF)
    inv = (10000.0 ** (-np.arange(0, 32, 2, dtype=np.float32) / 32)).astype(np.float32)
    row = np.repeat(np.arange(SEQ // 64, dtype=np.float32), 64)
    col = np.tile(np.arange(64, dtype=np.float32), SEQ // 64)
    ar = (row[None, :] * inv[:, None]).astype(np.float32)
    ac = (col[None, :] * inv[:, None]).astype(np.float32)
    c["rope_cos"] = np.concatenate([np.cos(ar), np.cos(ar), np.cos(ac), np.cos(ac)], 0).astype(NPBF)
    c["rope_sin"] = np.concatenate([np.sin(ar), np.sin(ar), np.sin(ac), np.sin(ac)], 0).astype(NPBF)
    Rm = np.zeros((64, 64), np.float32)
    for base in (0, 32):
        for i in range(16):
            Rm[base + i, base + 16 + i] = -1.0
            Rm[base + 16 + i, base + i] = 1.0
    c["ropeRT"] = np.ascontiguousarray(np.concatenate([Rm.T, np.zeros((64, 64), np.float32)], 1)).astype(NPBF)
    j = np.arange(128)[:, None]
    r = np.arange(128)[None, :]
    mp = (j >= r).astype(np.float32)
    mn = (j <= r).astype(np.float32)
    c["maskp"] = np.ascontiguousarray(np.broadcast_to(mp[:, None, :], (128, 4, 128))).astype(np.float32)
    c["maskn"] = np.ascontiguousarray(np.broadcast_to(mn[:, None, :], (128, 4, 128))).astype(np.float32)
    sel = np.zeros((8, 8, 128), np.float32)
    for e in range(8):
        sel[e, e, :] = 1.0
    c["sel"] = sel
    ut = (np.arange(128)[:, None] < np.arange(128)[None, :]).astype(np.float32)
    c["utb"] = ut.astype(NPBF)
    c["iota_p"] = np.arange(128, dtype=np.float32).reshape(128, 1)
    c["bvals"] = np.ascontiguousarray(np.broadcast_to((np.arange(NBLK, dtype=np.float32) * SBLK)[None, :], (128, NBLK)))
    return c


_CONSTS = None


def build(debug=None, upto=99):
    debug = debug or set()
    nc = bass.Bass("TRN2", target_bir_lowering=False)
    ins = {}

    def inp(name, shape, dt=F32):
        ins[name] = nc.dram_tensor(name, list(shape), dt, kind="ExternalInput").ap()
        return ins[name]

    def scratch(name, shape, dt):
        kind = "ExternalOutput" if name in debug else "Internal"
        return nc.dram_tensor(name, list(shape), dt, kind=kind).ap()

    x_in = inp("x", [2, SEQ, D])
    ctx_in = inp("ctx", [2, CTX, D])
    cT = inp("cT", [128, 8, 3])
    ada_w = inp("ada_w", [2, D, 6 * D])
    ada_bT = inp("ada_bT", [128, 2, 48])
    n1g = inp("n1g", [128, 2, 8])
    n2g = inp("n2g", [128, 2, 8])
    w_in = inp("ev_w_in", [D, 2048])
    w_out0 = inp("ev_w_out", [D, D])
    cw = inp("cw", [128, 12, 3])
    cb = inp("cb", [128, 12])
    hyb = inp("hyb", [128, 4])
    hf_w0 = inp("hf_w0", [33, 64])
    hf_w1 = inp("hf_w1", [64, 64])
    hf_w2 = inp("hf_w2", [64, 64])
    hf_w3 = inp("hf_w3", [64, 1024])
    hf_vec = inp("hf_vec", [64, 4])
    ffn_wg = inp("ffn_w_gate", [1, D, DFF])
    ffn_wu = inp("ffn_w_up", [1, D, DFF])
    ffn_wd = inp("ffn_w_down", [1, DFF, D])
    w_qkv = inp("od_w_qkv", [D, 1536])
    w_out1 = inp("od_w_out", [D, D])
    qkg = inp("qkg", [64, 2])
    sinkr = inp("sinkr", [64, 16])
    routerT = inp("routerT", [128, 8, 8])
    moe_wg = inp("moe_w_gate", [NE * NFG * 128, 2048])
    moe_wu = inp("moe_w_up", [NE * NFG * 128, 2048])
    moe_wd = inp("moe_w_down", [NE * NFG * 128, 2048])
    cst = {}
    for k, v in _CONSTS.items():
        cst[k] = inp("k_" + k, v.shape, BF16 if v.dtype == NPBF else F32)
    out = nc.dram_tensor("out", [2, SEQ, D], F32, kind="ExternalOutput").ap()

    LS = [SEQ, SEQ, CTX, CTX]
    TAG = ["4096", "4096", "256", "256"]
    COL = [0, 1, 2, 2]
    xTd = [scratch("xTd%d" % i, [8, 128, LS[i]], F32) for i in range(4)]
    uTd = [scratch("uTd%d" % i, [16, 128, LS[i]], BF16) for i in range(4)]
    zTd = [scratch("zTd%d" % i, [4, 128, LS[i]], BF16) for i in range(4)]
    x0Td = [scratch("x0Td%d" % i, [4, 128, LS[i]], BF16) for i in range(4)]
    catTd = [scratch("catTd%d" % i, [8, 128, LS[i]], BF16) for i in range(4)]
    hTd = [scratch("hTd%d" % i, [8, 128, LS[i]], BF16) for i in range(4)]
    Md = {"4096": scratch("Md4096", [32, 128, 2, 512], F32), "256": scratch("Md256", [2, 128, 2, 512], F32)}
    attTd = [scratch("attTd%d" % i, [16, 64, SEQ], BF16) for i in range(2)]
    gTd = [scratch("gTd%d" % i, [8, SEQ], F32) for i in range(2)]
    Htok = scratch("Htok", [2 * SEQ, D], BF16)
    Xs = scratch("Xs", [NSLOT, D], BF16)
    Ys = scratch("Ys", [NSLOT, D], BF16)
    dbg_mod = scratch("dbg_mod", [128, 2 * 48 * 3], F32) if "dbg_mod" in debug else None
    dbg_plan = scratch("dbg_plan", [128, 168], F32) if "dbg_plan" in debug else None

    with ExitStack() as es0:
        P = Prog(nc, es0)
        cnt = [0]

        def sbt(es, shape, dt, name=None):
            cnt[0] += 1
            return es.enter_context(nc.sbuf_tensor(name or ("t%d" % cnt[0]), list(shape), dt))

        ps = [es0.enter_context(nc.psum_tensor("ps%d" % i, [128, 512], F32)) for i in range(8)]
        PSR = ["ps%d" % i for i in range(8)]

        def mm(bank, out_ap, lhsT, rhs, start, stop, reads):
            P.op("pe", lambda e: e.matmul(out_ap, lhsT=lhsT, rhs=rhs, start=start, stop=stop), reads=reads, writes=[PSR[bank]])

        ident_f = sbt(es0, [128, 128], F32)
        ident_b = sbt(es0, [128, 128], BF16)
        ones_b = sbt(es0, [128, 128], BF16)
        ones_f = sbt(es0, [128, 128], F32)
        epst = sbt(es0, [128, 1], F32)
        mod = sbt(es0, [128, 2, 48, 3], F32)
        gs = sbt(es0, [128, 2, 2, 8, 3], F32)
        for t, k in ((ident_f, "ident_f"), (ident_b, "ident_b"), (ones_b, "ones_b"), (ones_f, "ones_f")):
            P.dma("sp", t[:], cst[k][:, :], writes=["const"])
        P.op("dve", lambda e: e.memset(epst[:], EPS), writes=["const"])

        I32 = mybir.dt.int32
        MK = sbt(es0, [128, 64, 16], F32)
        GV = sbt(es0, [128, 64, 2], F32)
        dsl = sbt(es0, [128, 128], I32)
        idxw = sbt(es0, [128, NBLK * NFG], I32)
        with ExitStack() as es:
            cT_sb = sbt(es, [128, 8, 3], F32)
            sT = sbt(es, [128, 8, 3], F32)
            abT = sbt(es, [128, 2, 48], F32)
            g1 = sbt(es, [128, 2, 8], F32)
            g2 = sbt(es, [128, 2, 8], F32)
            tmpm = sbt(es, [128, 8, 3], F32)
            aw = [sbt(es, [128, 8, 768], F32) for _ in range(2)]
            zt0 = sbt(es, [128, 8, D], BF16)
            P.op("pool", lambda e: e.memset(zt0[:], 0.0), writes=["zt0"])
            for r_ in range(NSLOT // 1024):
                P.dma("pool", Xs[r_ * 1024:(r_ + 1) * 1024, :].rearrange("(t p) d -> p t d", p=128), zt0[:], reads=["zt0"], writes=["Xs"])
            P.dma("sp", cT_sb[:], cT[:, :, :], writes=["cT"])
            P.dma("sp", abT[:], ada_bT[:, :, :], writes=["abT"])
            P.dma("sp", g1[:], n1g[:, :, :], writes=["g12"])
            P.dma("sp", g2[:], n2g[:, :, :], writes=["g12"])
            P.op("act", lambda e: e.activation(out=sT[:], in_=cT_sb[:], func=AF.Silu), reads=["cT"], writes=["sT"])
            it = 0
            for l in range(2):
                awv = ada_w[l].rearrange("(k p) n -> p k n", p=128)
                for nb in range(8):
                    b = it % 2
                    it += 1
                    P.dma("sp", aw[b][:], awv[:, :, nb * 768:(nb + 1) * 768], writes=["aw%d" % b])
                    for j in range(6):
                        n = nb * 6 + j
                        bank = n % 2
                        for k in range(8):
                            mm(bank, ps[bank][:, 0:3], aw[b][:, k, j * 128:(j + 1) * 128], sT[:, k, :], k == 0, k == 7, ["aw%d" % b, "sT"])
                        P.op("dve", lambda e: e.tensor_scalar(out=mod[:, l, n, :], in0=ps[bank][:, 0:3], scalar1=abT[:, l, n:n + 1], scalar2=None, op0=ALU.add),
                             reads=[PSR[bank], "abT"], writes=["mod"])
            for l in range(2):
                for w, gt in ((0, g1), (1, g2)):
                    P.op("dve", lambda e: e.tensor_scalar(out=tmpm[:], in0=mod[:, l, 8 + 24 * w:16 + 24 * w, :], scalar1=1.0, scalar2=None, op0=ALU.add),
                         reads=["mod"], writes=["tmpm"])
                    for col in range(3):
                        P.op("dve", lambda e: e.tensor_tensor(out=gs[:, l, w, :, col], in0=tmpm[:, :, col], in1=gt[:, l, :], op=ALU.mult),
                             reads=["tmpm", "g12"], writes=["mod"])
            if dbg_mod is not None:
                P.dma("sp", dbg_mod[:, :], mod[:].rearrange("p a b c -> p (a b c)"), reads=["mod"], writes=["dbg_mod"])
            P.barrier()

        def SH(l, w, c, col):
            return mod[:, l, 24 * w + c, col:col + 1]

        def GATE(l, w, c, col):
            return mod[:, l, 16 + 24 * w + c, col:col + 1]

        def GS(l, w, c, col):
            return gs[:, l, w, c, col:col + 1]

        def norm_block(es_t, xt, rx, W, l, w, col, outb, rout, tiles, outf=None):
            sq, rs, tmp = tiles
            P.op("act", lambda e: e.activation(out=sq[:, :, :W], in_=xt[:, :, :W], func=AF.Square), reads=[rx], writes=["n_sq"])
            for c in range(8):
                mm(7, ps[7][:, :W], ones_b[:], sq[:, c, :W], c == 0, c == 7, ["n_sq", "const"])
            P.op("act", lambda e: e.activation(out=rs[:, :W], in_=ps[7][:, :W], func=AF.Ln, bias=epst[:, 0:1], scale=1.0 / D),
                 reads=[PSR[7], "const"], writes=["n_rs"])
            P.op("act", lambda e: e.activation(out=rs[:, :W], in_=rs[:, :W], func=AF.Exp, scale=-0.5), reads=["n_rs"], writes=["n_rs"])
            for c in range(8):
                P.op("dve", lambda e: e.scalar_tensor_tensor(out=tmp[:, c, :W], in0=xt[:, c, :W], scalar=GS(l, w, c, col), in1=rs[:, :W], op0=ALU.mult, op1=ALU.mult),
                     reads=[rx, "n_rs", "mod"], writes=["n_tmp"])
                P.op("act", lambda e: e.activation(out=outb[:, c, :W], in_=tmp[:, c, :W], func=AF.Identity, bias=SH(l, w, c, col), scale=1.0),
                     reads=["n_tmp", "mod"], writes=[rout])
                if outf is not None:
                    P.op("act", lambda e: e.activation(out=outf[:, c, :W], in_=tmp[:, c, :W], func=AF.Identity, bias=SH(l, w, c, col), scale=1.0),
                         reads=["n_tmp", "mod"], writes=[rout + "f"])

        def load_cast_w(es, src_view, kch, ncols, name, npart=128):
            wb = sbt(es, [npart, kch, ncols], BF16)
            with ExitStack() as e2:
                st = sbt(e2, [npart, kch, 512], F32)
                for j in range(0, ncols, 512):
                    P.dma("sp", st[:], src_view[:, :, j:j + 512], writes=["wstage"])
                    P.op("act", lambda e: e.copy(out=wb[:, :, j:j + 512], in_=st[:]), reads=["wstage"], writes=[name])
                P.barrier()
            return wb

        items0 = [0, 1, 2, 3]

        def xsrc(it):
            return x_in[it] if it < 2 else ctx_in[it - 2]

        if upto >= 1:
            with ExitStack() as es:
                win_b = load_cast_w(es, w_in.rearrange("(k p) n -> p k n", p=128), 8, 2048, "win_b")
                xin = [sbt(es, [128, 4, D], F32) for _ in range(2)]
                xt = sbt(es, [128, 8, 512], F32)
                sq = sbt(es, [128, 8, 512], BF16)
                rs = sbt(es, [128, 512], F32)
                tmp = sbt(es, [128, 8, 512], F32)
                hx = sbt(es, [128, 8, 512], BF16)
                ut = sbt(es, [128, 16, 512], BF16)
                bi = 0
                for it in items0:
                    L = LS[it]
                    W = min(512, L)
                    for blk in range(L // W):
                        b = bi % 2
                        bi += 1
                        t0 = blk * W
                        nt = W // 128
                        P.dma("sp", xin[b][:, 0:nt, :], xsrc(it)[t0:t0 + W, :].rearrange("(t p) d -> p t d", p=128), writes=["xin%d" % b])
                        for c in range(8):
                            bank = c % 4
                            for t in range(nt):
                                P.op("pe", lambda e: e.transpose(out=ps[bank][:, t * 128:(t + 1) * 128], in_=xin[b][:, t, c * 128:(c + 1) * 128], identity=ident_f[:]),
                                     reads=["xin%d" % b, "const"], writes=[PSR[bank]])
                            eng = "act" if c % 2 else "dve"
                            if eng == "act":
                                P.op("act", lambda e: e.copy(out=xt[:, c, :W], in_=ps[bank][:, :W]), reads=[PSR[bank]], writes=["xt"])
                            else:
                                P.op("dve", lambda e: e.tensor_copy(out=xt[:, c, :W], in_=ps[bank][:, :W]), reads=[PSR[bank]], writes=["xt"])
                        P.dma("pool", xTd[it].rearrange("c p t -> p c t")[:, :, t0:t0 + W], xt[:, :, :W], reads=["xt"], writes=["xTd%d_%d" % (it, blk)])
                        norm_block(es, xt, "xt", W, 0, 0, COL[it], hx, "hx", (sq, rs, tmp))
                        if "hTd0" in debug:
                            P.dma("pool", hTd[it].rearrange("c p t -> p c t")[:, :, t0:t0 + W], hx[:, :, :W], reads=["hx"], writes=["hTd%d_%d" % (it, blk)])
                        for n in range(16):
                            bank = 4 + n % 2
                            for k in range(8):
                                mm(bank, ps[bank][:, :W], win_b[:, k, n * 128:(n + 1) * 128], hx[:, k, :W], k == 0, k == 7, ["win_b", "hx"])
                            if n % 2:
                                P.op("act", lambda e: e.copy(out=ut[:, n, :W], in_=ps[bank][:, :W]), reads=[PSR[bank]], writes=["ut"])
                            else:
                                P.op("dve", lambda e: e.tensor_copy(out=ut[:, n, :W], in_=ps[bank][:, :W]), reads=[PSR[bank]], writes=["ut"])
                        P.dma("pool", uTd[it].rearrange("c p t -> p c t")[:, :, t0:t0 + W], ut[:, :, :W], reads=["ut"], writes=["uTd%d_%d" % (it, blk)])
                P.barrier()

        def blocks_of(it):
            L = LS[it]
            return range(L // min(512, L))

        if upto >= 2:
            for tag, L in (("4096", SEQ), ("256", CTX)):
                nK = L // 128
                W = min(512, L)
                with ExitStack() as es:
                    emb = sbt(es, [33, L], F32)
                    hA = sbt(es, [64, L + 1], F32)
                    hB = sbt(es, [64, L + 1], F32)
                    w0s = sbt(es, [33, 64], F32)
                    w1s = sbt(es, [64, 64], F32)
                    w2s = sbt(es, [64, 64], F32)
                    w3s = sbt(es, [64, 1024], F32)
                    vec = sbt(es, [64, 4], F32)
                    fb = sbt(es, [64, 3], F32)
                    arg = sbt(es, [64, 512], F32)
                    msk = sbt(es, [64, 512], F32)
                    negt = sbt(es, [128, nK], F32)
                    negt1 = sbt(es, [128, nK], F32)
                    ca = sbt(es, [128, nK], F32)
                    sa = sbt(es, [128, nK], F32)
                    dbc = sbt(es, [128, 512], F32)
                    A0 = sbt(es, [128, nK, 512], BF16)
                    B0 = sbt(es, [128, nK, 512], BF16)
                    dec = sbt(es, [128, 512], F32)
                    dec1 = sbt(es, [128, 512], F32)
                    hf = sbt(es, [128, 512], F32)
                    hb = sbt(es, [128, 512], F32)
                    sqf = sbt(es, [128, 512], F32)
                    sqb = sbt(es, [128, 512], F32)
                    rn = sbt(es, [128, 512], F32)
                    tC = [sbt(es, [128, nK, 128], BF16) for _ in range(2)]
                    tS = [sbt(es, [128, nK, 128], BF16) for _ in range(2)]
                    t1 = sbt(es, [128, 512], F32)
                    t2 = sbt(es, [128, 512], F32)
                    t3 = sbt(es, [128, 512], F32)
                    Mt = [sbt(es, [128, 2, 512], F32) for _ in range(2)]
                    for t, src in ((emb, cst["emb" + tag]), (w0s, hf_w0), (w1s, hf_w1), (w2s, hf_w2), (w3s, hf_w3), (vec, hf_vec),
                                   (negt, cst["negt" + tag]), (negt1, cst["negt1" + tag]), (ca, cst["ca" + tag]), (sa, cst["sa" + tag]),
                                   (dbc, cst["delta_bc"])):
                        P.dma("sp", t[:], src[:, :], writes=["fconst"])
                    P.op("dve", lambda e: e.memset(hA[:], 0.0), writes=["hA"])
                    for i in range(3):
                        P.op("dve", lambda e: e.tensor_tensor(out=fb[:, i:i + 1], in0=vec[:, i:i + 1], in1=vec[:, 3:4], op=ALU.mult), reads=["fconst"], writes=["fb"])
                    layers = ((w0s, emb, "fconst", hA, "hA", 0), (w1s, hA, "hA", hB, "hB", 1), (w2s, hB, "hB", hA, "hA", 2))
                    for (ws, src, rsrc, dst, rdst, li) in layers:
                        for cbk in range(L // W):
                            cs = slice(cbk * W, (cbk + 1) * W)
                            mm(0, ps[0][0:64, :W], ws[:], src[:, cs], True, True, ["fconst", rsrc])
                            P.op("dve", lambda e: e.tensor_scalar(out=arg[:, :W], in0=ps[0][0:64, :W], scalar1=vec[:, 3:4], scalar2=fb[:, li:li + 1], op0=ALU.mult, op1=ALU.add),
                                 reads=[PSR[0], "fconst", "fb"], writes=["arg"])
                            for _rep in range(2):
                                P.op("dve", lambda e: e.tensor_scalar(out=msk[:, :W], in0=arg[:, :W], scalar1=PI, scalar2=2 * PI, op0=ALU.is_gt, op1=ALU.mult), reads=["arg"], writes=["msk"])
                                P.op("dve", lambda e: e.tensor_tensor(out=arg[:, :W], in0=arg[:, :W], in1=msk[:, :W], op=ALU.subtract), reads=["arg", "msk"], writes=["arg"])
                                P.op("dve", lambda e: e.tensor_scalar(out=msk[:, :W], in0=arg[:, :W], scalar1=-PI, scalar2=2 * PI, op0=ALU.is_lt, op1=ALU.mult), reads=["arg"], writes=["msk"])
                                P.op("dve", lambda e: e.tensor_tensor(out=arg[:, :W], in0=arg[:, :W], in1=msk[:, :W], op=ALU.add), reads=["arg", "msk"], writes=["arg"])
                            P.op("act", lambda e: e.activation(out=dst[:, cs], in_=arg[:, :W], func=AF.Sin), reads=["arg"], writes=[rdst])
                    for jc in range(nK):
                        mm(0, ps[0][:, :], hA[:, jc * 128:(jc + 1) * 128], w3s[:, 0:512], True, True, ["hA", "fconst"])
                        mm(1, ps[1][:, :], hA[:, jc * 128 + 1:(jc + 1) * 128 + 1], w3s[:, 512:1024], True, True, ["hA", "fconst"])
                        P.op("act", lambda e: e.activation(out=dec[:], in_=dbc[:], func=AF.Exp, scale=negt[:, jc:jc + 1]), reads=["fconst"], writes=["dec"])
                        P.op("act", lambda e: e.activation(out=dec1[:], in_=dbc[:], func=AF.Exp, scale=negt1[:, jc:jc + 1]), reads=["fconst"], writes=["dec1"])
                        P.op("dve", lambda e: e.tensor_tensor(out=hf[:], in0=ps[0][:, :], in1=dec[:], op=ALU.mult), reads=[PSR[0], "dec"], writes=["hf"])
                        P.op("dve", lambda e: e.tensor_tensor(out=hb[:], in0=ps[1][:, :], in1=dec1[:], op=ALU.mult), reads=[PSR[1], "dec1"], writes=["hb"])
                        P.op("pool", lambda e: e.tensor_tensor(out=A0[:, jc, :], in0=hf[:], in1=hb[:], op=ALU.add), reads=["hf", "hb"], writes=["A0"])
                        P.op("pool", lambda e: e.tensor_tensor(out=B0[:, jc, :], in0=hb[:], in1=hf[:], op=ALU.subtract), reads=["hf", "hb"], writes=["B0"])
                        P.op("dve", lambda e: e.tensor_tensor(out=sqf[:], in0=hf[:], in1=hf[:], op=ALU.mult), reads=["hf"], writes=["sqf"])
                        P.op("dve", lambda e: e.tensor_tensor(out=sqb[:], in0=hb[:], in1=hb[:], op=ALU.mult), reads=["hb"], writes=["sqb"])
                        mm(7, ps[7][:, :], ones_f[:], sqf[:], jc == 0, False, ["sqf", "const"])
                        mm(7, ps[7][:, :], ones_f[:], sqb[:], False, jc == nK - 1, ["sqb", "const"])
                    P.op("act", lambda e: e.activation(out=rn[:], in_=ps[7][:, :], func=AF.Sqrt, bias=epst[:, 0:1], scale=1.0), reads=[PSR[7], "const"], writes=["rn"])
                    P.op("dve", lambda e: e.reciprocal(out=rn[:], in_=rn[:]), reads=["rn"], writes=["rn"])
                    for m in range(nK):
                        b = m % 2
                        P.dma("sp", tC[b][:], cst["thc" + tag][m], writes=["tC%d" % b])
                        P.dma("sp", tS[b][:], cst["ths" + tag][m], writes=["tS%d" % b])
                        for k in range(nK):
                            mm(2 * b, ps[2 * b][:, :], tC[b][:, k, :], A0[:, k, :], k == 0, k == nK - 1, ["tC%d" % b, "A0"])
                        for k in range(nK):
                            mm(2 * b + 1, ps[2 * b + 1][:, :], tS[b][:, k, :], B0[:, k, :], k == 0, k == nK - 1, ["tS%d" % b, "B0"])
                        P.op("dve", lambda e: e.tensor_tensor(out=t1[:], in0=ps[2 * b][:, :], in1=rn[:], op=ALU.mult), reads=[PSR[2 * b], "rn"], writes=["t1"])
                        P.op("dve", lambda e: e.tensor_tensor(out=t2[:], in0=ps[2 * b + 1][:, :], in1=rn[:], op=ALU.mult), reads=[PSR[2 * b + 1], "rn"], writes=["t2"])
                        P.op("pool", lambda e: e.tensor_scalar(out=t3[:], in0=t2[:], scalar1=sa[:, m:m + 1], scalar2=None, op0=ALU.mult), reads=["t2", "fconst"], writes=["t3"])
                        P.op("dve", lambda e: e.scalar_tensor_tensor(out=Mt[b][:, 0, :], in0=t1[:], scalar=ca[:, m:m + 1], in1=t3[:], op0=ALU.mult, op1=ALU.subtract),
                             reads=["t1", "t3", "fconst"], writes=["Mt%d" % b])
                        P.op("pool", lambda e: e.tensor_scalar(out=t3[:], in0=t2[:], scalar1=ca[:, m:m + 1], scalar2=None, op0=ALU.mult), reads=["t2", "fconst"], writes=["t3"])
                        P.op("dve", lambda e: e.scalar_tensor_tensor(out=Mt[b][:, 1, :], in0=t1[:], scalar=sa[:, m:m + 1], in1=t3[:], op0=ALU.mult, op1=ALU.add),
                             reads=["t1", "t3", "fconst"], writes=["Mt%d" % b])
                        P.dma("act", Md[tag][m], Mt[b][:], reads=["Mt%d" % b], writes=["Md" + tag])
                    P.barrier()

        if upto >= 3:
            for it in items0:
                L = LS[it]
                tag = TAG[it]
                nK = L // 128
                NB = list(blocks_of(it))
                with ExitStack() as esI:
                    z_tok = sbt(esI, [128, nK, 512], BF16)
                    tC = [sbt(esI, [128, nK, 128], BF16) for _ in range(2)]
                    tS = [sbt(esI, [128, nK, 128], BF16) for _ in range(2)]
                    with ExitStack() as es:
                        cws = sbt(es, [128, 12, 3], F32)
                        cbs = sbt(es, [128, 12], F32)
                        dg = sbt(es, [128, 36, 128], BF16)
                        pad = [sbt(es, [128, L + 2], BF16) for _ in range(3)]
                        v32 = [sbt(es, [128, 512], F32) for _ in range(2)]
                        zT = sbt(es, [128, L], BF16)
                        x0T = sbt(es, [128, L], BF16)
                        P.dma("sp", cws[:], cw[:, :, :], writes=["cws"])
                        P.dma("sp", cbs[:], cb[:, :], writes=["cws"])
                        for p_ in range(3):
                            P.op("pool", lambda e: e.memset(pad[p_][:], 0.0), writes=["pad%d" % p_])
                        for pi in range(12):
                            for tap in range(3):
                                P.op("dve", lambda e: e.tensor_scalar(out=dg[:, pi * 3 + tap, :], in0=ident_b[:], scalar1=cws[:, pi, tap:tap + 1], scalar2=None, op0=ALU.mult),
                                     reads=["const", "cws"], writes=["dg"])
                        Wc = min(512, L)
                        vi = 0
                        for c in range(4):
                            for part in range(3):
                                ch = 4 + part * 4 + c
                                P.dma("sp", pad[part][:, 1:L + 1], uTd[it][ch], reads=["uTd%d_%d" % (it, b_) for b_ in NB], writes=["pad%d" % part])
                            for blk in range(L // Wc):
                                t0 = blk * Wc
                                for part in range(3):
                                    pi = part * 4 + c
                                    for tap in range(3):
                                        mm(part, ps[part][:, :Wc], dg[:, pi * 3 + tap, :], pad[part][:, t0 + tap:t0 + tap + Wc], tap == 0, tap == 2, ["dg", "pad%d" % part])
                                vb = vi % 2
                                vi += 1
                                P.op("act", lambda e: e.activation(out=v32[vb][:, :Wc], in_=ps[0][:, :Wc], func=AF.Identity, bias=cbs[:, c:c + 1], scale=1.0), reads=[PSR[0], "cws"], writes=["v32_%d" % vb])
                                P.op("dve", lambda e: e.scalar_tensor_tensor(out=zT[:, t0:t0 + Wc], in0=ps[1][:, :Wc], scalar=cbs[:, 4 + c:5 + c], in1=v32[vb][:, :Wc], op0=ALU.add, op1=ALU.mult),
                                     reads=[PSR[1], "cws", "v32_%d" % vb], writes=["zT"])
                                P.op("act", lambda e: e.activation(out=x0T[:, t0:t0 + Wc], in_=ps[2][:, :Wc], func=AF.Identity, bias=cbs[:, 8 + c:9 + c], scale=1.0), reads=[PSR[2], "cws"], writes=["x0T"])
                            P.dma("pool", zTd[it][c], zT[:], reads=["zT"], writes=["zTd%d" % it])
                            P.dma("pool", x0Td[it][c], x0T[:], reads=["x0T"], writes=["x0Td%d" % it])
                            for l4 in range(0, nK, 4):
                                nn = min(4, nK - l4)
                                bank = 4 + (l4 // 4) % 2
                                for q in range(nn):
                                    lc = l4 + q
                                    mm(bank, ps[bank][:, q * 128:(q + 1) * 128], zT[:, lc * 128:(lc + 1) * 128], ident_b[:], True, True, ["zT", "const"])
                                P.op("act", lambda e: e.copy(out=z_tok[:, l4:l4 + nn, c * 128:(c + 1) * 128], in_=ps[bank][:, 0:nn * 128].rearrange("p (a b) -> p a b", b=128)),
                                     reads=[PSR[bank]], writes=["z_tok"])
                        P.barrier()
                    Wre = sbt(esI, [128, nK, 512], BF16)
                    nWim = sbt(esI, [128, nK, 512], BF16)
                    with ExitStack() as es:
                        Mt = [sbt(es, [128, 2, 512], F32) for _ in range(2)]
                        p1 = sbt(es, [128, 512], F32)
                        p2 = sbt(es, [128, 512], F32)
                        p3 = sbt(es, [128, 512], F32)
                        p4 = sbt(es, [128, 512], F32)
                        for m in range(nK):
                            b = m % 2
                            P.dma("sp", tC[b][:], cst["thc" + tag][m], writes=["tC%d" % b])
                            P.dma("sp", tS[b][:], cst["ths" + tag][m], writes=["tS%d" % b])
                            P.dma("sp", Mt[b][:], Md[tag][m], reads=["Md" + tag], writes=["Mt%d" % b])
                            ba, bb = 2 * b, 2 * b + 1
                            for k in range(nK):
                                mm(ba, ps[ba][:, :], tC[b][:, k, :], z_tok[:, k, :], k == 0, k == nK - 1, ["tC%d" % b, "z_tok"])
                            for k in range(nK):
                                mm(bb, ps[bb][:, :], tS[b][:, k, :], z_tok[:, k, :], k == 0, k == nK - 1, ["tS%d" % b, "z_tok"])
                            P.op("dve", lambda e: e.tensor_tensor(out=p1[:], in0=ps[ba][:, :], in1=Mt[b][:, 0, :], op=ALU.mult), reads=[PSR[ba], "Mt%d" % b], writes=["p1"])
                            P.op("dve", lambda e: e.tensor_tensor(out=p2[:], in0=ps[bb][:, :], in1=Mt[b][:, 1, :], op=ALU.mult), reads=[PSR[bb], "Mt%d" % b], writes=["p2"])
                            P.op("pool", lambda e: e.tensor_tensor(out=Wre[:, m, :], in0=p1[:], in1=p2[:], op=ALU.add), reads=["p1", "p2"], writes=["Wre"])
                            P.op("dve", lambda e: e.tensor_tensor(out=p3[:], in0=ps[bb][:, :], in1=Mt[b][:, 0, :], op=ALU.mult), reads=[PSR[bb], "Mt%d" % b], writes=["p3"])
                            P.op("dve", lambda e: e.tensor_tensor(out=p4[:], in0=ps[ba][:, :], in1=Mt[b][:, 1, :], op=ALU.mult), reads=[PSR[ba], "Mt%d" % b], writes=["p4"])
                            P.op("pool", lambda e: e.tensor_tensor(out=nWim[:, m, :], in0=p3[:], in1=p4[:], op=ALU.subtract), reads=["p3", "p4"], writes=["nWim"])
                        P.barrier()
                    with ExitStack() as es:
                        hybs = sbt(es, [128, 4], F32)
                        ysb = sbt(es, [128, 512], BF16)
                        zs = [sbt(es, [128, 4, 128], BF16) for _ in range(2)]
                        xs = [sbt(es, [128, 4, 128], BF16) for _ in range(2)]
                        tq = sbt(es, [128, 4, 128], F32)
                        bT = [sbt(es, [128, 4, 128], BF16) for _ in range(2)]
                        P.dma("sp", hybs[:], hyb[:, :], writes=["hybs"])
                        for m in range(nK):
                            b = m % 2
                            P.dma("sp", tC[b][:], cst["thc" + tag][m], writes=["tC%d" % b])
                            P.dma("sp", tS[b][:], cst["ths" + tag][m], writes=["tS%d" % b])
                            P.dma("sp", zs[b][:], zTd[it].rearrange("c p t -> p c t")[:, :, m * 128:(m + 1) * 128], reads=["zTd%d" % it], writes=["zs%d" % b])
                            P.dma("sp", xs[b][:], x0Td[it].rearrange("c p t -> p c t")[:, :, m * 128:(m + 1) * 128], reads=["x0Td%d" % it], writes=["xs%d" % b])
                            for k in range(nK):
                                mm(b, ps[b][:, :], tC[b][:, k, :], Wre[:, k, :], k == 0, False, ["tC%d" % b, "Wre"])
                            for k in range(nK):
                                mm(b, ps[b][:, :], tS[b][:, k, :], nWim[:, k, :], False, k == nK - 1, ["tS%d" % b, "nWim"])
                            P.op("act", lambda e: e.copy(out=ysb[:], in_=ps[b][:, :]), reads=[PSR[b]], writes=["ysb"])
                            for c in range(4):
                                mm(2 + b, ps[2 + b][:, c * 128:(c + 1) * 128], ysb[:, c * 128:(c + 1) * 128], ident_b[:], True, True, ["ysb", "const"])
                            for c in range(4):
                                P.op("dve", lambda e: e.scalar_tensor_tensor(out=tq[:, c, :], in0=zs[b][:, c, :], scalar=hybs[:, c:c + 1], in1=ps[2 + b][:, c * 128:(c + 1) * 128], op0=ALU.mult, op1=ALU.add),
                                     reads=["zs%d" % b, "hybs", PSR[2 + b]], writes=["tq"])
                            P.op("pool", lambda e: e.tensor_tensor(out=bT[b][:], in0=tq[:], in1=xs[b][:], op=ALU.mult), reads=["tq", "xs%d" % b], writes=["bT%d" % b])
                            P.dma("pool", catTd[it].rearrange("c p t -> p c t")[:, 4:8, m * 128:(m + 1) * 128], bT[b][:], reads=["bT%d" % b], writes=["catTd%d" % it])
                        P.barrier()
                    with ExitStack() as es:
                        UfT = sbt(es, [128, 4, L], BF16)
                        c128 = sbt(es, [128, 128], BF16)
                        ns128 = sbt(es, [128, 128], BF16)
                        asb = sbt(es, [128, 512], BF16)
                        aT = [sbt(es, [128, 4, 128], BF16) for _ in range(2)]
                        P_tok, Q_tok = Wre, nWim
                        P.dma("sp", c128[:], cst["c128"][:, :], writes=["c128"])
                        P.dma("sp", ns128[:], cst["ns128"][:, :], writes=["c128"])
                        P.dma("sp", UfT[:], uTd[it].rearrange("c p t -> p c t")[:, 0:4, :], reads=["uTd%d_%d" % (it, b_) for b_ in NB], writes=["UfT"])
                        for lc in range(nK):
                            b = lc % 2
                            for g in range(4):
                                mm(b, ps[b][:, g * 128:(g + 1) * 128], UfT[:, g, lc * 128:(lc + 1) * 128], c128[:], True, True, ["UfT", "c128"])
                            for g in range(4):
                                mm(2 + b, ps[2 + b][:, g * 128:(g + 1) * 128], UfT[:, g, lc * 128:(lc + 1) * 128], ns128[:], True, True, ["UfT", "c128"])
                            P.op("act", lambda e: e.copy(out=P_tok[:, lc, :], in_=ps[b][:, :]), reads=[PSR[b]], writes=["Wre"])
                            P.op("dve", lambda e: e.tensor_copy(out=Q_tok[:, lc, :], in_=ps[2 + b][:, :]), reads=[PSR[2 + b]], writes=["nWim"])
                        sc = 1.0 / math.sqrt(128.0 * L)
                        for m in range(nK):
                            b = m % 2
                            P.dma("sp", tC[b][:], cst["tfc" + tag][m], writes=["tC%d" % b])
                            P.dma("sp", tS[b][:], cst["tfs" + tag][m], writes=["tS%d" % b])
                            for k in range(nK):
                                mm(4 + b, ps[4 + b][:, :], tC[b][:, k, :], P_tok[:, k, :], k == 0, False, ["tC%d" % b, "Wre"])
                            for k in range(nK):
                                mm(4 + b, ps[4 + b][:, :], tS[b][:, k, :], Q_tok[:, k, :], False, k == nK - 1, ["tS%d" % b, "nWim"])
                            P.op("act", lambda e: e.activation(out=asb[:], in_=ps[4 + b][:, :], func=AF.Copy, scale=sc), reads=[PSR[4 + b]], writes=["asb"])
                            for g in range(4):
                                mm(6 + b, ps[6 + b][:, g * 128:(g + 1) * 128], asb[:, g * 128:(g + 1) * 128], ident_b[:], True, True, ["asb", "const"])
                            P.op("dve", lambda e: e.tensor_copy(out=aT[b][:], in_=ps[6 + b][:, :].rearrange("p (a b) -> p a b", b=128)), reads=[PSR[6 + b]], writes=["aT%d" % b])
                            P.dma("pool", catTd[it].rearrange("c p t -> p c t")[:, 0:4, m * 128:(m + 1) * 128], aT[b][:], reads=["aT%d" % b], writes=["catTd%d" % it])
                    P.barrier()

        def outproj_phase(l, items, wout_view, kparts, cat_loader, router, npart=128):
            with ExitStack() as es:
                nkc = len(kparts)
                wb = load_cast_w(es, wout_view, nkc, D, "wout_b", npart=npart)
                xt = sbt(es, [128, 8, 512], F32)
                x1 = sbt(es, [128, 8, 512], F32)
                sq = sbt(es, [128, 8, 512], BF16)
                rs = sbt(es, [128, 512], F32)
                tmp = sbt(es, [128, 8, 512], F32)
                hx = sbt(es, [128, 8, 512], BF16)
                hxf = sbt(es, [128, 8, 512], F32) if router else None
                cat = cat_loader(es)
                if router:
                    rw = sbt(es, [128, 8, 8], F32)
                    lg = sbt(es, [128, 8], F32)
                    m1 = sbt(es, [128, 1], F32)
                    m2 = sbt(es, [128, 1], F32)
                    mk1 = sbt(es, [128, 8], F32)
                    mk2 = sbt(es, [128, 8], F32)
                    l2 = sbt(es, [128, 8], F32)
                    dd = sbt(es, [128, 1], F32)
                    g1t = sbt(es, [128, 1], F32)
                    g2t = sbt(es, [128, 1], F32)
                    gt = sbt(es, [128, 8], F32)
                    htok = [sbt(es, [128, D], BF16) for _ in range(2)]
                    P.dma("sp", rw[:], routerT[:, :, :], writes=["rw"])
                for it in items:
                    L = LS[it]
                    W = min(512, L)
                    for blk in range(L // W):
                        t0 = blk * W
                        rcat = cat(it, t0, W)
                        P.dma("sp", xt[:, :, :W], xTd[it].rearrange("c p t -> p c t")[:, :, t0:t0 + W], reads=["xTd%d_%d" % (it, blk)], writes=["xt"])
                        for n in range(8):
                            bank = n % 4
                            for ki, (lh, rh) in enumerate(kparts):
                                mm(bank, ps[bank][:, :W], lh(wb, n), rh(W), ki == 0, ki == nkc - 1, ["wout_b", rcat])
                            P.op("dve", lambda e: e.scalar_tensor_tensor(out=x1[:, n, :W], in0=ps[bank][:, :W], scalar=GATE(l, 0, n, COL[it]), in1=xt[:, n, :W], op0=ALU.mult, op1=ALU.add),
                                 reads=[PSR[bank], "xt", "mod"], writes=["x1"])
                        P.dma("pool", xTd[it].rearrange("c p t -> p c t")[:, :, t0:t0 + W], x1[:, :, :W], reads=["x1"], writes=["xTd%d_%d" % (it, blk)])
                        norm_block(es, x1, "x1", W, l, 1, COL[it], hx, "hx", (sq, rs, tmp), outf=hxf)
                        P.dma("pool", hTd[it].rearrange("c p t -> p c t")[:, :, t0:t0 + W], hx[:, :, :W], reads=["hx"], writes=["hTd%d_%d" % (it, blk)])
                        if router:
                            for t in range(W // 128):
                                ts_ = slice(t * 128, (t + 1) * 128)
                                for k in range(8):
                                    mm(4, ps[4][:, 0:8], hxf[:, k, ts_], rw[:, k, :], k == 0, k == 7, ["hxf", "rw"])
                                P.op("dve", lambda e: e.tensor_copy(out=lg[:], in_=ps[4][:, 0:8]), reads=[PSR[4]], writes=["lg"])
                                P.op("dve", lambda e: e.reduce_max(out=m1[:], in_=lg[:], axis=mybir.AxisListType.X), reads=["lg"], writes=["m1"])
                                P.op("dve", lambda e: e.tensor_scalar(out=mk1[:], in0=lg[:], scalar1=m1[:, 0:1], scalar2=None, op0=ALU.is_ge), reads=["lg", "m1"], writes=["mk1"])
                                P.op("dve", lambda e: e.scalar_tensor_tensor(out=l2[:], in0=mk1[:], scalar=-1e30, in1=lg[:], op0=ALU.mult, op1=ALU.add), reads=["mk1", "lg"], writes=["l2"])
                                P.op("dve", lambda e: e.reduce_max(out=m2[:], in_=l2[:], axis=mybir.AxisListType.X), reads=["l2"], writes=["m2"])
                                P.op("dve", lambda e: e.tensor_scalar(out=mk2[:], in0=l2[:], scalar1=m2[:, 0:1], scalar2=None, op0=ALU.is_ge), reads=["l2", "m2"], writes=["mk2"])
                                P.op("dve", lambda e: e.tensor_tensor(out=dd[:], in0=m2[:], in1=m1[:], op=ALU.subtract), reads=["m1", "m2"], writes=["dd"])
                                P.op("act", lambda e: e.activation(out=g2t[:], in_=dd[:], func=AF.Exp), reads=["dd"], writes=["g2t"])
                                P.op("dve", lambda e: e.tensor_scalar(out=g1t[:], in0=g2t[:], scalar1=1.0, scalar2=None, op0=ALU.add), reads=["g2t"], writes=["g1t"])
                                P.op("dve", lambda e: e.reciprocal(out=g1t[:], in_=g1t[:]), reads=["g1t"], writes=["g1t"])
                                P.op("dve", lambda e: e.tensor_tensor(out=g2t[:], in0=g2t[:], in1=g1t[:], op=ALU.mult), reads=["g2t", "g1t"], writes=["g2t"])
                                P.op("dve", lambda e: e.tensor_scalar(out=gt[:], in0=mk1[:], scalar1=g1t[:, 0:1], scalar2=None, op0=ALU.mult), reads=["mk1", "g1t"], writes=["gt"])
                                P.op("dve", lambda e: e.scalar_tensor_tensor(out=gt[:], in0=mk2[:], scalar=g2t[:, 0:1], in1=gt[:], op0=ALU.mult, op1=ALU.add), reads=["mk2", "g2t", "gt"], writes=["gt"])
                                ti = it * 32 + blk * 4 + t
                                P.op("pool", lambda e: e.tensor_copy(out=MK[:, ti, 0:8], in_=mk1[:]), reads=["mk1"], writes=["MK"])
                                P.op("pool", lambda e: e.tensor_copy(out=MK[:, ti, 8:16], in_=mk2[:]), reads=["mk2"], writes=["MK"])
                                P.op("pool", lambda e: e.tensor_copy(out=GV[:, ti, 0:1], in_=g1t[:]), reads=["g1t"], writes=["GV"])
                                P.op("pool", lambda e: e.tensor_copy(out=GV[:, ti, 1:2], in_=g2t[:]), reads=["g2t"], writes=["GV"])
                                hb_ = ti % 2
                                for c in range(8):
                                    bank = 5 + c // 4
                                    mm(bank, ps[bank][:, (c % 4) * 128:(c % 4 + 1) * 128], hx[:, c, ts_], ident_b[:], True, True, ["hx", "const"])
                                P.op("act", lambda e: e.copy(out=htok[hb_][:, 0:512], in_=ps[5][:, :]), reads=[PSR[5]], writes=["htok%d" % hb_])
                                P.op("dve", lambda e: e.tensor_copy(out=htok[hb_][:, 512:1024], in_=ps[6][:, :]), reads=[PSR[6]], writes=["htok%d" % hb_])
                                P.dma("pool", Htok[ti * 128:(ti + 1) * 128, :], htok[hb_][:], reads=["htok%d" % hb_], writes=["Htok%d" % ti])
                P.barrier()

        def cat_loader0(es):
            cat = sbt(es, [128, 8, 512], BF16)

            def load(it, t0, W):
                P.dma("sp", cat[:, :, :W], catTd[it].rearrange("c p t -> p c t")[:, :, t0:t0 + W], reads=["catTd%d" % it], writes=["cat"])
                return "cat"
            load.tile = cat
            return load

        if upto >= 4:
            holder = {}

            def cl0(es):
                f = cat_loader0(es)
                holder["cat"] = f.tile
                return f
            kparts0 = [((lambda wb, n, k=k: wb[:, k, n * 128:(n + 1) * 128]), (lambda W, k=k: holder["cat"][:, k, :W])) for k in range(8)]
            outproj_phase(0, items0, w_out0.rearrange("(k p) n -> p k n", p=128), kparts0, cl0, router=False)

        def ffn_phase(l, blocks, wg, wu, wd, E, gated, final):
            FG = 256
            NFG = DFF // FG
            with ExitStack() as es:
                NTmax = max(sum(s[2] for s in segs) for segs in blocks)
                hT = sbt(es, [128, 8, NTmax], BF16)
                acc = sbt(es, [128, 8, NTmax], F32)
                if gated:
                    gT = sbt(es, [8, NTmax], F32)
                    Gbc = sbt(es, [128, NTmax], F32)
                    selt = sbt(es, [8, 8, 128], F32)
                    P.dma("sp", selt[:], cst["sel"][:, :, :], writes=["selt"])
                wi = 0
                oi = 0
                assert not final and not gated
                esW = es
                stg = [sbt(esW, [128, 8, FG], F32) for _ in range(2)]
                stgd = [sbt(esW, [128, 2, D], F32) for _ in range(2)]
                wgb = [sbt(esW, [128, 8, FG], BF16) for _ in range(2)]
                wub = [sbt(esW, [128, 8, FG], BF16) for _ in range(2)]
                wdb = [sbt(esW, [128, 2, D], BF16) for _ in range(2)]
                sg = [sbt(esW, [128, 512], F32) for _ in range(2)]
                tu = [sbt(esW, [128, 512], F32) for _ in range(2)]
                hh = [sbt(esW, [128, 2, 512], BF16) for _ in range(2)]
                x1 = sbt(esW, [128, 8, 512], F32)
                x2 = sbt(esW, [128, 8, 512], F32)
                for segs in blocks:
                    NT = sum(s[2] for s in segs)
                    off = 0
                    for (it, t0, n) in segs:
                        rds = ["hTd%d_%d" % (it, b_) for b_ in range(t0 // min(512, LS[it]), (t0 + n + min(512, LS[it]) - 1) // min(512, LS[it]))]
                        P.dma("sp", hT[:, :, off:off + n], hTd[it].rearrange("c p t -> p c t")[:, :, t0:t0 + n], reads=rds, writes=["hT"])
                        if gated:
                            rdg = ["gTd%d_%d" % (it, b_) for b_ in range(t0 // 512, (t0 + n + 511) // 512)]
                            P.dma("sp", gT[:, off:off + n], gTd[it][:, t0:t0 + n], reads=rdg, writes=["gT"])
                        off += n
                    nsub = (NT + 511) // 512
                    first = True
                    steps = []
                    gu_done = [0]
                    dn_done = [0]

                    def emit_gu(st, si, NT=NT):
                        b, s_, fst = st
                        hb = si % 2
                        ss_ = slice(s_ * 512, min(NT, (s_ + 1) * 512))
                        wdt = ss_.stop - ss_.start
                        for j in range(2):
                            for k in range(8):
                                mm(0 + j, ps[0 + j][:, :wdt], wgb[b][:, k, j * 128:(j + 1) * 128], hT[:, k, ss_], k == 0, k == 7, ["wgb%d" % b, "hT"])
                            for k in range(8):
                                mm(2 + j, ps[2 + j][:, :wdt], wub[b][:, k, j * 128:(j + 1) * 128], hT[:, k, ss_], k == 0, k == 7, ["wub%d" % b, "hT"])
                            P.op("act", lambda e: e.activation(out=sg[j][:, :wdt], in_=ps[0 + j][:, :wdt], func=AF.Silu), reads=[PSR[0 + j]], writes=["sg%d" % j])
                            if gated:
                                P.op("dve", lambda e: e.tensor_tensor(out=tu[j][:, :wdt], in0=ps[2 + j][:, :wdt], in1=Gbc[:, ss_], op=ALU.mult), reads=[PSR[2 + j], "Gbc"], writes=["tu%d" % j])
                                P.op("pool", lambda e: e.tensor_tensor(out=hh[hb][:, j, :wdt], in0=sg[j][:, :wdt], in1=tu[j][:, :wdt], op=ALU.mult), reads=["sg%d" % j, "tu%d" % j], writes=["hh%d" % hb])
                            else:
                                P.op("dve", lambda e: e.tensor_tensor(out=hh[hb][:, j, :wdt], in0=ps[2 + j][:, :wdt], in1=sg[j][:, :wdt], op=ALU.mult), reads=[PSR[2 + j], "sg%d" % j], writes=["hh%d" % hb])

                    def emit_dn(st, si, NT=NT):
                        b, s_, fst = st
                        hb = si % 2
                        ss_ = slice(s_ * 512, min(NT, (s_ + 1) * 512))
                        wdt = ss_.stop - ss_.start
                        for n in range(8):
                            bank = 4 + (n % 2 if gated else n % 4)
                            for j in range(2):
                                mm(bank, ps[bank][:, :wdt], wdb[b][:, j, n * 128:(n + 1) * 128], hh[hb][:, j, :wdt], j == 0, j == 1, ["wdb%d" % b, "hh%d" % hb])
                            if fst:
                                P.op("dve", lambda e: e.tensor_copy(out=acc[:, n, ss_], in_=ps[bank][:, :wdt]), reads=[PSR[bank]], writes=["acc"])
                            else:
                                P.op("dve", lambda e: e.tensor_tensor(out=acc[:, n, ss_], in0=acc[:, n, ss_], in1=ps[bank][:, :wdt], op=ALU.add), reads=[PSR[bank], "acc"], writes=["acc"])
                    for e_ in range(E):
                        if gated:
                            for s in range(nsub):
                                ss_ = slice(s * 512, min(NT, (s + 1) * 512))
                                wdt = ss_.stop - ss_.start
                                mm(6, ps[6][:, :wdt], selt[:, e_, :], gT[:, ss_], True, True, ["selt", "gT"])
                                P.op("act", lambda e: e.copy(out=Gbc[:, ss_], in_=ps[6][:, :wdt]), reads=[PSR[6]], writes=["Gbc"])
                        wgv = wg[e_].rearrange("(k p) f -> p k f", p=128)
                        wuv = wu[e_].rearrange("(k p) f -> p k f", p=128)
                        for fg in range(NFG):
                            b = wi % 2
                            wi += 1
                            fs = slice(fg * FG, (fg + 1) * FG)
                            P.dma("sp", stg[0][:], wgv[:, :, fs], writes=["stg0"])
                            P.op("act", lambda e: e.copy(out=wgb[b][:], in_=stg[0][:]), reads=["stg0"], writes=["wgb%d" % b])
                            P.dma("sp", stg[1][:], wuv[:, :, fs], writes=["stg1"])
                            P.op("pool", lambda e: e.tensor_copy(out=wub[b][:], in_=stg[1][:]), reads=["stg1"], writes=["wub%d" % b])
                            P.dma("sp", stgd[0][:], wd[e_][fg * FG:(fg + 1) * FG, :].rearrange("(j p) n -> p j n", p=128), writes=["stgd0"])
                            P.op("act", lambda e: e.copy(out=wdb[b][:], in_=stgd[0][:]), reads=["stgd0"], writes=["wdb%d" % b])
                            for s_ in range(nsub):
                                steps.append((b, s_, first))
                            first = False
                            while gu_done[0] < len(steps):
                                emit_gu(steps[gu_done[0]], gu_done[0])
                                gu_done[0] += 1
                                if dn_done[0] < gu_done[0] - 1:
                                    emit_dn(steps[dn_done[0]], dn_done[0])
                                    dn_done[0] += 1
                    while dn_done[0] < len(steps):
                        emit_dn(steps[dn_done[0]], dn_done[0])
                        dn_done[0] += 1
                    off = 0
                    for (it, t0, n) in segs:
                        W = min(512, LS[it])
                        for q in range(n // W):
                            blk = (t0 + q * W) // W
                            tt = t0 + q * W
                            P.dma("sp", x1[:, :, :W], xTd[it].rearrange("c p t -> p c t")[:, :, tt:tt + W], reads=["xTd%d_%d" % (it, blk)], writes=["x1f"])
                            for c in range(8):
                                P.op("dve", lambda e: e.scalar_tensor_tensor(out=x2[:, c, :W], in0=acc[:, c, off + q * W:off + (q + 1) * W], scalar=GATE(l, 1, c, COL[it]), in1=x1[:, c, :W], op0=ALU.mult, op1=ALU.add),
                                     reads=["acc", "x1f", "mod"], writes=["x2"])
                            if not final:
                                P.dma("pool", xTd[it].rearrange("c p t -> p c t")[:, :, tt:tt + W], x2[:, :, :W], reads=["x2"], writes=["xTd%d_%d" % (it, blk)])
                            else:
                                ob = oi % 2
                                oi += 1
                                for t in range(W // 128):
                                    for c in range(8):
                                        bank = 6 + (c // 4) % 2
                                        P.op("pe", lambda e: e.transpose(out=ps[bank][:, (c % 4) * 128:(c % 4 + 1) * 128], in_=x2[:, c, t * 128:(t + 1) * 128], identity=ident_f[:]),
                                             reads=["x2", "const"], writes=[PSR[bank]])
                                        if c % 4 == 3:
                                            h0 = (c // 4) * 512
                                            if c // 4:
                                                P.op("act", lambda e: e.copy(out=xo[ob][:, t, h0:h0 + 512], in_=ps[bank][:, :]), reads=[PSR[bank]], writes=["xo%d" % ob])
                                            else:
                                                P.op("dve", lambda e: e.tensor_copy(out=xo[ob][:, t, h0:h0 + 512], in_=ps[bank][:, :]), reads=[PSR[bank]], writes=["xo%d" % ob])
                                P.dma("pool", out[it][tt:tt + W, :].rearrange("(t p) d -> p t d", p=128), xo[ob][:, 0:W // 128, :], reads=["xo%d" % ob], writes=["out"])
                        off += n
                P.barrier()


        def moe_sparse(l):
            AXX = mybir.AxisListType.X
            with ExitStack() as es:
                Mb = sbt(es, [128, 64, 8], BF16)
                utb = sbt(es, [128, 128], BF16)
                iota = sbt(es, [128, 1], F32)
                bvals = sbt(es, [128, NBLK], F32)
                cntt = sbt(es, [128, 8], F32)
                qq = sbt(es, [128, 8], F32)
                padded = sbt(es, [128, 8], F32)
                pend = sbt(es, [128, 8], F32)
                pstart = sbt(es, [128, 8], F32)
                be = sbt(es, [128, NBLK], F32)
                idxf = sbt(es, [128, NBLK, NFG], F32)
                base = sbt(es, [128, 64, 8], F32)
                slot = sbt(es, [128, 64, 8], F32)
                tsel = sbt(es, [128, 64, 8], F32)
                dsf = sbt(es, [128, 64, 2], F32)
                P.dma("sp", utb[:], cst["utb"][:, :], writes=["plc"])
                P.dma("sp", iota[:], cst["iota_p"][:, :], writes=["plc"])
                P.dma("sp", bvals[:], cst["bvals"][:, :], writes=["plc"])
                P.op("dve", lambda e: e.tensor_tensor(out=Mb[:], in0=MK[:, :, 0:8], in1=MK[:, :, 8:16], op=ALU.add), reads=["MK"], writes=["Mb"])
                for ti in range(64):
                    mm(0, ps[0][:, 0:8], ones_b[:], Mb[:, ti, :], ti == 0, ti == 63, ["Mb", "const"])
                for ti in range(64):
                    mm(1, ps[1][:, ti * 8:(ti + 1) * 8], ones_b[:], Mb[:, ti, :], True, True, ["Mb", "const"])
                for ti in range(64):
                    mm(2, ps[2][:, ti * 8:(ti + 1) * 8], utb[:], Mb[:, ti, :], True, True, ["Mb", "plc"])
                P.op("dve", lambda e: e.tensor_copy(out=cntt[:], in_=ps[0][:, 0:8]), reads=[PSR[0]], writes=["cntt"])
                P.op("dve", lambda e: e.tensor_scalar(out=qq[:], in0=cntt[:], scalar1=0.0, scalar2=None, op0=ALU.is_gt), reads=["cntt"], writes=["qq"])
                for m_ in range(1, 8):
                    P.op("dve", lambda e: e.scalar_tensor_tensor(out=qq[:], in0=cntt[:], scalar=float(m_ * SBLK), in1=qq[:], op0=ALU.is_gt, op1=ALU.add), reads=["cntt", "qq"], writes=["qq"])
                P.op("dve", lambda e: e.tensor_scalar(out=padded[:], in0=qq[:], scalar1=float(SBLK), scalar2=None, op0=ALU.mult), reads=["qq"], writes=["padded"])
                P.op("dve", lambda e: e.tensor_copy(out=pend[:, 0:1], in_=padded[:, 0:1]), reads=["padded"], writes=["pend"])
                for e_ in range(1, 8):
                    P.op("dve", lambda e: e.tensor_tensor(out=pend[:, e_:e_ + 1], in0=pend[:, e_ - 1:e_], in1=padded[:, e_:e_ + 1], op=ALU.add), reads=["pend", "padded"], writes=["pend"])
                P.op("dve", lambda e: e.tensor_tensor(out=pstart[:], in0=pend[:], in1=padded[:], op=ALU.subtract), reads=["pend", "padded"], writes=["pstart"])
                P.op("dve", lambda e: e.tensor_scalar(out=be[:], in0=bvals[:], scalar1=pend[:, 0:1], scalar2=None, op0=ALU.is_ge), reads=["plc", "pend"], writes=["be"])
                for e_ in range(1, 8):
                    P.op("dve", lambda e: e.scalar_tensor_tensor(out=be[:], in0=bvals[:], scalar=pend[:, e_:e_ + 1], in1=be[:], op0=ALU.is_ge, op1=ALU.add), reads=["plc", "pend", "be"], writes=["be"])
                P.op("dve", lambda e: e.tensor_scalar(out=be[:], in0=be[:], scalar1=7.0, scalar2=None, op0=ALU.min), reads=["be"], writes=["be"])
                for fg in range(NFG):
                    P.op("dve", lambda e: e.tensor_scalar(out=idxf[:, :, fg], in0=be[:], scalar1=float(NFG * 128), scalar2=float(fg * 128), op0=ALU.mult, op1=ALU.add), reads=["be"], writes=["idxf"])
                P.op("dve", lambda e: e.tensor_scalar(out=idxf[:], in0=idxf[:], scalar1=iota[:, 0:1], scalar2=None, op0=ALU.add), reads=["idxf", "plc"], writes=["idxf"])
                P.op("dve", lambda e: e.tensor_copy(out=idxw[:], in_=idxf[:].rearrange("p a b -> p (a b)")), reads=["idxf"], writes=["idxw"])
                P.op("dve", lambda e: e.tensor_copy(out=base[:, 0, :], in_=pstart[:]), reads=["pstart"], writes=["base"])
                for ti in range(1, 64):
                    P.op("dve", lambda e: e.tensor_tensor(out=base[:, ti, :], in0=base[:, ti - 1, :], in1=ps[1][:, (ti - 1) * 8:ti * 8], op=ALU.add), reads=["base", PSR[1]], writes=["base"])
                P.op("dve", lambda e: e.tensor_tensor(out=slot[:], in0=base[:], in1=ps[2][:, :].rearrange("p (a b) -> p a b", b=8), op=ALU.add), reads=["base", PSR[2]], writes=["slot"])
                for k_ in range(2):
                    P.op("dve", lambda e: e.tensor_tensor(out=tsel[:], in0=slot[:], in1=MK[:, :, k_ * 8:(k_ + 1) * 8], op=ALU.mult), reads=["slot", "MK"], writes=["tsel"])
                    P.op("dve", lambda e: e.reduce_sum(out=dsf[:, :, k_], in_=tsel[:], axis=AXX), reads=["tsel"], writes=["dsf"])
                P.op("dve", lambda e: e.tensor_scalar(out=dsf[:], in0=dsf[:], scalar1=float(NSLOT - 1), scalar2=None, op0=ALU.min), reads=["dsf"], writes=["dsf"])
                P.op("dve", lambda e: e.tensor_copy(out=dsl[:], in_=dsf[:].rearrange("p a b -> p (a b)")), reads=["dsf"], writes=["dsl"])
                if "dbg_plan" in debug:
                    P.dma("sp", dbg_plan[:, 0:128], dsf[:].rearrange("p a b -> p (a b)"), reads=["dsf"], writes=["dbgp"])
                    P.dma("sp", dbg_plan[:, 128:128 + NBLK], be[:], reads=["be"], writes=["dbgp"])
                    P.dma("sp", dbg_plan[:, 160:168], cntt[:], reads=["cntt"], writes=["dbgp"])
                P.barrier()
            with ExitStack() as es:
                ht = [sbt(es, [128, D], BF16) for _ in range(4)]
                for ti in range(64):
                    b = ti % 4
                    P.dma("sp", ht[b][:], Htok[ti * 128:(ti + 1) * 128, :], reads=["Htok%d" % ti], writes=["ht%d" % b])
                    for k_ in range(2):
                        P.idma(Xs[:, :], ht[b][:], dsl[:, 2 * ti + k_:2 * ti + k_ + 1], None, NSLOT - 1, reads=["ht%d" % b, "dsl"], writes=["Xs"])
                P.barrier()
            with ExitStack() as es:
                xtok = sbt(es, [128, 8, D], BF16)
                hT = sbt(es, [128, 8, SBLK], BF16)
                accs = [sbt(es, [128, 8, SBLK], F32) for _ in range(2)]
                ybs = [sbt(es, [128, 8, 128], BF16) for _ in range(2)]
                ytok = [sbt(es, [128, D], BF16) for _ in range(2)]
                stg = [[sbt(es, [128, 2048], F32) for _ in range(2)] for _ in range(3)]
                wgb = [sbt(es, [128, 8, 256], BF16) for _ in range(2)]
                wub = [sbt(es, [128, 8, 256], BF16) for _ in range(2)]
                wdb = [sbt(es, [128, 2, D], BF16) for _ in range(2)]
                sg = [sbt(es, [128, 512], F32) for _ in range(2)]
                hh = [sbt(es, [128, 2, 512], BF16) for _ in range(2)]
                wi = 0
                yi = [0]
                ps7b = ps[7][:, :].bitcast(BF16)

                def emit_out_tile(blk_o, t):
                    acc_o = accs[blk_o % 2]
                    racc_o = "acc%d" % (blk_o % 2)
                    yb_ = yi[0] % 2
                    yi[0] += 1
                    yb = ybs[yb_]
                    if yb_:
                        P.op("pool", lambda e: e.tensor_copy(out=yb[:], in_=acc_o[:, :, t * 128:(t + 1) * 128]), reads=[racc_o], writes=["yb%d" % yb_])
                    else:
                        P.op("act", lambda e: e.copy(out=yb[:], in_=acc_o[:, :, t * 128:(t + 1) * 128]), reads=[racc_o], writes=["yb%d" % yb_])
                    for c in range(8):
                        P.op("pe", lambda e: e.transpose(out=ps7b[:, c * 128:(c + 1) * 128], in_=yb[:, c, :], identity=ident_b[:]), reads=["yb%d" % yb_, "const"], writes=[PSR[7]])
                    P.op("act", lambda e: e.copy(out=ytok[yb_][:], in_=ps7b[:, :]), reads=[PSR[7]], writes=["ytok%d" % yb_])
                    r0 = blk_o * SBLK + t * 128
                    P.dma("sp", Ys[r0:r0 + 128, :], ytok[yb_][:], reads=["ytok%d" % yb_], writes=["Ys"])

                for blk in range(NBLK):
                    acc = accs[blk % 2]
                    racc = "acc%d" % (blk % 2)
                    P.dma("sp", xtok[:], Xs[blk * SBLK:(blk + 1) * SBLK, :].rearrange("(t p) d -> p t d", p=128), reads=["Xs"], writes=["xtok"])
                    for c in range(8):
                        for t4 in range(2):
                            bank = 6 + (c * 2 + t4) % 2
                            for q in range(4):
                                t = t4 * 4 + q
                                mm(bank, ps[bank][:, q * 128:(q + 1) * 128], xtok[:, t, c * 128:(c + 1) * 128], ident_b[:], True, True, ["xtok", "const"])
                            if (c * 2 + t4) % 2:
                                P.op("act", lambda e: e.copy(out=hT[:, c, t4 * 512:(t4 + 1) * 512], in_=ps[bank][:, :]), reads=[PSR[bank]], writes=["hT"])
                            else:
                                P.op("dve", lambda e: e.tensor_copy(out=hT[:, c, t4 * 512:(t4 + 1) * 512], in_=ps[bank][:, :]), reads=[PSR[bank]], writes=["hT"])
                    steps = []
                    gu_done = 0
                    dn_done = 0

                    def emit_gu(st, si):
                        b, s_, fst = st
                        hb = si % 2
                        ss_ = slice(s_ * 512, (s_ + 1) * 512)
                        for j in range(2):
                            for k in range(8):
                                mm(0 + j, ps[0 + j][:, :], wgb[b][:, k, j * 128:(j + 1) * 128], hT[:, k, ss_], k == 0, k == 7, ["wgb%d" % b, "hT"])
                            for k in range(8):
                                mm(2 + j, ps[2 + j][:, :], wub[b][:, k, j * 128:(j + 1) * 128], hT[:, k, ss_], k == 0, k == 7, ["wub%d" % b, "hT"])
                            P.op("act", lambda e: e.activation(out=sg[j][:], in_=ps[0 + j][:, :], func=AF.Silu), reads=[PSR[0 + j]], writes=["sg%d" % j])
                            P.op("dve", lambda e: e.tensor_tensor(out=hh[hb][:, j, :], in0=ps[2 + j][:, :], in1=sg[j][:], op=ALU.mult), reads=[PSR[2 + j], "sg%d" % j], writes=["hh%d" % hb])

                    def emit_dn(st, si):
                        b, s_, fst = st
                        hb = si % 2
                        ss_ = slice(s_ * 512, (s_ + 1) * 512)
                        for n in range(8):
                            bank = 4 + n % 3
                            for j in range(2):
                                mm(bank, ps[bank][:, :], wdb[b][:, j, n * 128:(n + 1) * 128], hh[hb][:, j, :], j == 0, j == 1, ["wdb%d" % b, "hh%d" % hb])
                            if fst:
                                P.op("dve", lambda e: e.tensor_copy(out=acc[:, n, ss_], in_=ps[bank][:, :]), reads=[PSR[bank]], writes=[racc])
                            else:
                                P.op("dve", lambda e: e.tensor_tensor(out=acc[:, n, ss_], in0=acc[:, n, ss_], in1=ps[bank][:, :], op=ALU.add), reads=[PSR[bank], racc], writes=[racc])

                    def gather_w(blk_, fg_, b_):
                        ic = blk_ * NFG + fg_
                        for wsrc, wk in ((moe_wg, 0), (moe_wu, 1), (moe_wd, 2)):
                            P.idma(stg[wk][b_][:], wsrc[:, :], None, idxw[:, ic:ic + 1], NE * NFG * 128 - 1, reads=["idxw"], writes=["stg%d_%d" % (wk, b_)])

                    if blk == 0:
                        gather_w(0, 0, wi % 2)
                    for fg in range(NFG):
                        b = wi % 2
                        wi += 1
                        if fg + 1 < NFG:
                            gather_w(blk, fg + 1, wi % 2)
                        elif blk + 1 < NBLK:
                            gather_w(blk + 1, 0, wi % 2)
                        P.op("act", lambda e: e.copy(out=wgb[b][:].rearrange("p k f -> p (k f)"), in_=stg[0][b][:]), reads=["stg0_%d" % b], writes=["wgb%d" % b])
                        P.op("act", lambda e: e.copy(out=wub[b][:].rearrange("p k f -> p (k f)"), in_=stg[1][b][:]), reads=["stg1_%d" % b], writes=["wub%d" % b])
                        P.op("act", lambda e: e.copy(out=wdb[b][:].rearrange("p k f -> p (k f)"), in_=stg[2][b][:]), reads=["stg2_%d" % b], writes=["wdb%d" % b])
                        for s_ in range(SBLK // 512):
                            steps.append((b, s_, fg == 0))
                        while gu_done < len(steps):
                            emit_gu(steps[gu_done], gu_done)
                            gu_done += 1
                            if dn_done < gu_done - 1:
                                emit_dn(steps[dn_done], dn_done)
                                dn_done += 1
                        if blk > 0 and 2 <= fg < 10:
                            emit_out_tile(blk - 1, fg - 2)
                    while dn_done < len(steps):
                        emit_dn(steps[dn_done], dn_done)
                        dn_done += 1
                for t in range(8):
                    emit_out_tile(NBLK - 1, t)
                P.barrier()
            with ExitStack() as es:
                g2rep = sbt(es, [128, 128], F32)
                g2bc = [sbt(es, [128, D], F32) for _ in range(2)]
                o1 = [sbt(es, [128, D], BF16) for _ in range(4)]
                o2 = [sbt(es, [128, D], BF16) for _ in range(4)]
                yf = [sbt(es, [128, D], F32) for _ in range(4)]
                x1 = [sbt(es, [128, 8, 128], F32) for _ in range(4)]
                ot = [sbt(es, [128, D], F32) for _ in range(4)]
                for it in range(2):
                    for c in range(8):
                        P.op("dve", lambda e: e.tensor_scalar(out=g2rep[:], in0=ones_f[:], scalar1=GATE(l, 1, c, COL[it]), scalar2=None, op0=ALU.mult), reads=["const", "mod"], writes=["g2rep"])
                        bank = c // 4
                        P.op("pe", lambda e: e.transpose(out=ps[bank][:, (c % 4) * 128:(c % 4 + 1) * 128], in_=g2rep[:], identity=ident_f[:]), reads=["g2rep", "const"], writes=[PSR[bank]])
                        if c % 4 == 3:
                            P.op("act", lambda e: e.copy(out=g2bc[it][:, bank * 512:(bank + 1) * 512], in_=ps[bank][:, :]), reads=[PSR[bank]], writes=["g2bc"])
                for ti in range(64):
                    it = ti // 32
                    tt = (ti % 32) * 128
                    b = ti % 4
                    P.idma(o1[b][:], Ys[:, :], None, dsl[:, 2 * ti:2 * ti + 1], NSLOT - 1, reads=["Ys", "dsl"], writes=["o1_%d" % b])
                    P.idma(o2[b][:], Ys[:, :], None, dsl[:, 2 * ti + 1:2 * ti + 2], NSLOT - 1, reads=["Ys", "dsl"], writes=["o2_%d" % b])
                    P.dma("sp", x1[b][:], xTd[it].rearrange("c p t -> p c t")[:, :, tt:tt + 128], reads=["xTd%d_%d" % (it, tt // 512)], writes=["x1_%d" % b])
                    P.op("dve", lambda e: e.tensor_scalar(out=yf[b][:], in0=o1[b][:], scalar1=GV[:, ti, 0:1], scalar2=None, op0=ALU.mult), reads=["o1_%d" % b, "GV"], writes=["yf%d" % b])
                    P.op("dve", lambda e: e.scalar_tensor_tensor(out=yf[b][:], in0=o2[b][:], scalar=GV[:, ti, 1:2], in1=yf[b][:], op0=ALU.mult, op1=ALU.add), reads=["o2_%d" % b, "GV", "yf%d" % b], writes=["yf%d" % b])
                    P.op("dve", lambda e: e.tensor_tensor(out=yf[b][:], in0=yf[b][:], in1=g2bc[it][:], op=ALU.mult), reads=["yf%d" % b, "g2bc"], writes=["yf%d" % b])
                    for c in range(8):
                        bank = 2 * b + c // 4
                        P.op("pe", lambda e: e.transpose(out=ps[bank][:, (c % 4) * 128:(c % 4 + 1) * 128], in_=x1[b][:, c, :], identity=ident_f[:]), reads=["x1_%d" % b, "const"], writes=[PSR[bank]])
                    for h_ in range(2):
                        bank = 2 * b + h_
                        P.op("dve", lambda e: e.tensor_tensor(out=ot[b][:, h_ * 512:(h_ + 1) * 512], in0=ps[bank][:, :], in1=yf[b][:, h_ * 512:(h_ + 1) * 512], op=ALU.add), reads=[PSR[bank], "yf%d" % b], writes=["ot%d" % b])
                    P.dma("sp", out[it][tt:tt + 128, :], ot[b][:], reads=["ot%d" % b], writes=["out"])
                P.barrier()

        if upto >= 5:
            blocks0 = [[(0, 0, 2048)], [(0, 2048, 2048)], [(1, 0, 2048)], [(1, 2048, 2048)], [(2, 0, 256), (3, 0, 256)]]
            ffn_phase(0, blocks0, ffn_wg, ffn_wu, ffn_wd, 1, False, False)


        if upto >= 6:
            with ExitStack() as es:
                xt = sbt(es, [128, 8, 512], F32)
                sq = sbt(es, [128, 8, 512], BF16)
                rs = sbt(es, [128, 512], F32)
                tmp = sbt(es, [128, 8, 512], F32)
                hx = sbt(es, [128, 8, 512], BF16)
                for it in items0:
                    L = LS[it]
                    W = min(512, L)
                    for blk in range(L // W):
                        t0 = blk * W
                        P.dma("sp", xt[:, :, :W], xTd[it].rearrange("c p t -> p c t")[:, :, t0:t0 + W], reads=["xTd%d_%d" % (it, blk)], writes=["xt"])
                        norm_block(es, xt, "xt", W, 1, 0, COL[it], hx, "hx", (sq, rs, tmp))
                        P.dma("pool", hTd[it].rearrange("c p t -> p c t")[:, :, t0:t0 + W], hx[:, :, :W], reads=["hx"], writes=["hTd%d_%d" % (it, blk)])
                P.barrier()
            esQ = ExitStack()
            wq_b = load_cast_w(esQ, w_qkv.rearrange("(k p) n -> p k n", p=128), 8, 1536, "wq_b")
            for it in (0, 1):
                ci_ = it + 2
                L = SEQ
                nK = L // 128
                with ExitStack() as esI:
                    hxT = sbt(esI, [128, 8, L], BF16)
                    hcT = sbt(esI, [128, 8, CTX], BF16)
                    cosT = sbt(esI, [64, L], BF16)
                    sinT = sbt(esI, [64, L], BF16)
                    RT = sbt(esI, [64, 128], BF16)
                    qkgs = sbt(esI, [64, 2], F32)
                    esink = sbt(esI, [64, 16], F32)
                    mkp = sbt(esI, [128, 512], BF16)
                    mkn = sbt(esI, [128, 512], BF16)
                    mstage = sbt(esI, [128, 512], F32)
                    kT = sbt(esI, [64, L + CTX], BF16)
                    Vt = sbt(esI, [128, nK + 2, 128], BF16)
                    qT = sbt(esI, [64, 4, L], BF16)
                    ET = [[sbt(esI, [128, 512], BF16) for _ in range(5)] for _ in range(2)]
                    dns = [sbt(esI, [64, 512], F32) for _ in range(2)]
                    oT = [sbt(esI, [64, 512], BF16) for _ in range(2)]
                    P.dma("sp", hxT[:], hTd[it].rearrange("c p t -> p c t"), reads=["hTd%d_%d" % (it, b_) for b_ in range(8)], writes=["hxT"])
                    P.dma("sp", hcT[:], hTd[ci_].rearrange("c p t -> p c t"), reads=["hTd%d_0" % ci_], writes=["hcT"])
                    P.dma("sp", cosT[:], cst["rope_cos"][:, :], writes=["ropec"])
                    P.dma("sp", sinT[:], cst["rope_sin"][:, :], writes=["ropec"])
                    P.dma("sp", RT[:], cst["ropeRT"][:, :], writes=["ropec"])
                    P.dma("sp", qkgs[:], qkg[:, :], writes=["ropec"])
                    P.dma("sp", esink[:], sinkr[:, :], writes=["esink"])
                    P.op("act", lambda e: e.activation(out=esink[:], in_=esink[:], func=AF.Exp), reads=["esink"], writes=["esink"])
                    P.dma("sp", mstage[:], cst["maskp"].rearrange("p a b -> p (a b)"), writes=["mstage"])
                    P.op("dve", lambda e: e.tensor_copy(out=mkp[:], in_=mstage[:]), reads=["mstage"], writes=["mkp"])
                    P.dma("sp", mstage[:], cst["maskn"].rearrange("p a b -> p (a b)"), reads=[], writes=["mstage"])
                    P.op("dve", lambda e: e.tensor_copy(out=mkn[:], in_=mstage[:]), reads=["mstage"], writes=["mkn"])

                    sqqs = [sbt(esI, [64, 512], BF16) for _ in range(3)]
                    rsqs = [sbt(esI, [64, 512], F32) for _ in range(2)]
                    qns = [sbt(esI, [64, 512], BF16) for _ in range(2)]
                    r1s = [sbt(esI, [64, 512], F32) for _ in range(2)]
                    r2s = [sbt(esI, [64, 512], F32) for _ in range(2)]

                    def stA(u, ui):
                        (src, rsrc, c0, W, col0, gcol, rope, dest, rdest, vinfo) = u
                        pa = ui % 3
                        for k in range(8):
                            mm(pa, ps[pa][:, :W], wq_b[:, k, col0:col0 + 128], src[:, k, c0:c0 + W], k == 0, k == 7, ["wq_b", rsrc])
                        P.op("act", lambda e: e.activation(out=sqqs[pa][:, :W], in_=ps[pa][0:64, :W], func=AF.Square), reads=[PSR[pa]], writes=["sqq%d" % pa])
                        if vinfo is not None:
                            g_, vch0 = vinfo
                            nt = W // 128
                            for t in range(nt):
                                for k in range(8):
                                    mm(7, ps[7][:, t * 64:(t + 1) * 64], src[:, k, c0 + t * 128:c0 + (t + 1) * 128], wq_b[:, k, 1280 + g_ * 64:1280 + (g_ + 1) * 64], k == 0, k == 7, ["wq_b", rsrc])
                            P.op("act", lambda e: e.copy(out=Vt[:, vch0:vch0 + nt, 0:64], in_=ps[7][:, 0:nt * 64].rearrange("p (a b) -> p a b", b=64)), reads=[PSR[7]], writes=["Vt"])
                            P.op("dve", lambda e: e.tensor_copy(out=Vt[:, vch0:vch0 + nt, 64:128], in_=ps[7][:, 0:nt * 64].rearrange("p (a b) -> p a b", b=64)), reads=[PSR[7]], writes=["Vt"])

                    def stB(u, ui):
                        (src, rsrc, c0, W, col0, gcol, rope, dest, rdest, vinfo) = u
                        pa = ui % 3
                        p_ = ui % 2
                        bs = 3 + p_
                        mm(bs, ps[bs][:, :W], ones_b[0:64, :], sqqs[pa][:, :W], True, True, ["sqq%d" % pa, "const"])
                        P.op("act", lambda e: e.activation(out=rsqs[p_][:, :W], in_=ps[bs][0:64, :W], func=AF.Ln, bias=epst[0:64, 0:1], scale=1.0 / 64), reads=[PSR[bs], "const"], writes=["rsq%d" % p_])
                        P.op("act", lambda e: e.activation(out=rsqs[p_][:, :W], in_=rsqs[p_][:, :W], func=AF.Exp, scale=-0.5), reads=["rsq%d" % p_], writes=["rsq%d" % p_])
                        P.op("dve", lambda e: e.scalar_tensor_tensor(out=qns[p_][:, :W], in0=ps[pa][0:64, :W], scalar=qkgs[:, gcol:gcol + 1], in1=rsqs[p_][:, :W], op0=ALU.mult, op1=ALU.mult),
                             reads=[PSR[pa], "rsq%d" % p_, "ropec"], writes=["qn%d" % p_])

                    def stC(u, ui):
                        (src, rsrc, c0, W, col0, gcol, rope, dest, rdest, vinfo) = u
                        p_ = ui % 2
                        br = 5 + p_
                        if rope:
                            mm(br, ps[br][:, :W], RT[:], qns[p_][:, :W], True, True, ["qn%d" % p_, "ropec"])
                            P.op("pool", lambda e: e.tensor_tensor(out=r1s[p_][:, :W], in0=qns[p_][:, :W], in1=cosT[:, c0:c0 + W], op=ALU.mult), reads=["qn%d" % p_, "ropec"], writes=["r1%d" % p_])
                            P.op("dve", lambda e: e.tensor_tensor(out=r2s[p_][:, :W], in0=ps[br][0:64, :W], in1=sinT[:, c0:c0 + W], op=ALU.mult), reads=[PSR[br], "ropec"], writes=["r2%d" % p_])
                            P.op("pool", lambda e: e.tensor_tensor(out=dest, in0=r1s[p_][:, :W], in1=r2s[p_][:, :W], op=ALU.add), reads=["r1%d" % p_, "r2%d" % p_], writes=[rdest])
                        else:
                            P.op("pool", lambda e: e.tensor_copy(out=dest, in_=qns[p_][:, :W]), reads=["qn%d" % p_], writes=[rdest])

                    for g in range(4):
                        units = []
                        for b_ in range(L // 512):
                            c0 = b_ * 512
                            units.append((hxT, "hxT", c0, 512, 1024 + g * 64, 1, True, kT[:, c0:c0 + 512], "kT", (g, c0 // 128)))
                            for j in range(4):
                                units.append((hxT, "hxT", c0, 512, (4 * g + j) * 64, 0, True, qT[:, j, c0:c0 + 512], "qT", None))
                        units.append((hcT, "hcT", 0, CTX, 1024 + g * 64, 1, False, kT[:, L:L + CTX], "kT", (g, L // 128)))
                        nu = len(units)
                        for t_ in range(nu + 2):
                            if t_ < nu:
                                stA(units[t_], t_)
                            if 0 <= t_ - 1 < nu:
                                stB(units[t_ - 1], t_ - 1)
                            if 0 <= t_ - 2 < nu:
                                stC(units[t_ - 2], t_ - 2)

                        def chunks_of(i):
                            ch = []
                            if i > 0:
                                ch.append((i - 1, mkp, "mkp"))
                            ch.append((i, None, None))
                            if i < nK - 1:
                                ch.append((i + 1, mkn, "mkn"))
                            ch.append((nK, None, None))
                            ch.append((nK + 1, None, None))
                            return ch

                        def stS(i):
                            eb = i % 2
                            for ci, (kc, mk, rmk) in enumerate(chunks_of(i)):
                                sb_ = ci % 4
                                mm(sb_, ps[sb_][:, :].rearrange("p (a b) -> p a b", b=128), kT[:, kc * 128:(kc + 1) * 128], qT[:, :, i * 128:(i + 1) * 128], True, True, ["kT", "qT"])
                                P.op("act", lambda e: e.activation(out=ET[eb][ci][:], in_=ps[sb_][:, :], func=AF.Exp, scale=0.125), reads=[PSR[sb_]], writes=["ET%d_%d" % (eb, ci)])
                                if mk is not None:
                                    P.op("pool", lambda e: e.tensor_tensor(out=ET[eb][ci][:], in0=ET[eb][ci][:], in1=mk[:], op=ALU.mult), reads=["ET%d_%d" % (eb, ci), rmk], writes=["ET%d_%d" % (eb, ci)])

                        def stR(i):
                            eb = i % 2
                            chunks = chunks_of(i)
                            nch = len(chunks)
                            bo, bd = (4, 5) if eb == 0 else (6, 7)
                            dn = dns[eb]
                            for ci, (kc, mk, rmk) in enumerate(chunks):
                                mm(bo, ps[bo][:, :], Vt[:, kc, :], ET[eb][ci][:], ci == 0, ci == nch - 1, ["Vt", "ET%d_%d" % (eb, ci)])
                            for ci, (kc, mk, rmk) in enumerate(chunks):
                                mm(bd, ps[bd][:, :], ones_b[:, :], ET[eb][ci][:], ci == 0, ci == nch - 1, ["const", "ET%d_%d" % (eb, ci)])
                            for j in range(4):
                                h = 4 * g + j
                                P.op("act", lambda e: e.activation(out=dn[:, j * 128:(j + 1) * 128], in_=ps[bd][0:64, j * 128:(j + 1) * 128], func=AF.Ln, bias=esink[:, h:h + 1], scale=1.0),
                                     reads=[PSR[bd], "esink"], writes=["dn%d" % eb])
                            P.op("act", lambda e: e.activation(out=dn[:], in_=dn[:], func=AF.Exp, scale=-1.0), reads=["dn%d" % eb], writes=["dn%d" % eb])
                            P.op("dve", lambda e: e.tensor_tensor(out=oT[eb][:], in0=ps[bo][0:64, :], in1=dn[:], op=ALU.mult), reads=[PSR[bo], "dn%d" % eb], writes=["oT%d" % eb])
                            P.dma("pool", attTd[it].rearrange("h p t -> p h t")[:, 4 * g:4 * g + 4, i * 128:(i + 1) * 128], oT[eb][:].rearrange("p (a b) -> p a b", b=128),
                                  reads=["oT%d" % eb], writes=["attTd%d" % it])

                        stS(0)
                        for i in range(nK):
                            if i + 1 < nK:
                                stS(i + 1)
                            stR(i)
                    P.barrier()
            esQ.close()
            holder1 = {}

            def cl1(es):
                att = sbt(es, [64, 16, 512], BF16)
                holder1["att"] = att

                def load(it, t0, W):
                    P.dma("sp", att[:, :, :W], attTd[it].rearrange("h p t -> p h t")[:, :, t0:t0 + W], reads=["attTd%d" % it], writes=["att"])
                    return "att"
                return load
            kparts1 = [((lambda wb, n, h=h: wb[0:64, h, n * 128:(n + 1) * 128]), (lambda W, h=h: holder1["att"][:, h, :W])) for h in range(16)]
            outproj_phase(1, [0, 1], w_out1.rearrange("(h p) n -> p h n", p=64), kparts1, cl1, router=True, npart=64)
            if upto >= 7:
                moe_sparse(1)

        P.barrier()
        print("instructions:", P.ninst, flush=True)
    return nc


def _core_inputs(inp, core):
    b0 = 2 * core
    f32 = np.float32
    m = {}
    m["x"] = np.ascontiguousarray(inp["x"][b0:b0 + 2])
    m["ctx"] = np.ascontiguousarray(inp["ctx"][b0:b0 + 2])
    cc = np.stack([inp["c"][b0], inp["c"][b0 + 1], inp["c_ctx"]], axis=-1)
    m["cT"] = np.ascontiguousarray(cc.reshape(8, 128, 3).transpose(1, 0, 2)).astype(f32)
    m["ada_w"] = inp["ada_w"]
    m["ada_bT"] = np.ascontiguousarray(inp["ada_b"].reshape(2, 48, 128).transpose(2, 0, 1))
    m["n1g"] = np.ascontiguousarray(inp["norm1_g"].reshape(2, 8, 128).transpose(2, 0, 1))
    m["n2g"] = np.ascontiguousarray(inp["norm2_g"].reshape(2, 8, 128).transpose(2, 0, 1))
    m["ev_w_in"] = inp["ev_w_in"][0]
    m["ev_w_out"] = inp["ev_w_out"][0]
    m["cw"] = np.ascontiguousarray(inp["hy_conv_w"][0].reshape(3, 12, 128).transpose(2, 1, 0))
    m["cb"] = np.ascontiguousarray(inp["hy_conv_b"][0].reshape(12, 128).T)
    m["hyb"] = np.ascontiguousarray(inp["hy_bias"][0].reshape(4, 128).T)
    m["hf_w0"] = inp["hf_w0"][0]
    m["hf_w1"] = inp["hf_w1"][0]
    m["hf_w2"] = inp["hf_w2"][0]
    m["hf_w3"] = inp["hf_w3"][0]
    m["hf_vec"] = np.ascontiguousarray(np.stack([inp["hf_b0"][0], inp["hf_b1"][0], inp["hf_b2"][0], inp["hf_freq"][0]], axis=-1))
    m["ffn_w_gate"] = inp["ffn_w_gate"]
    m["ffn_w_up"] = inp["ffn_w_up"]
    m["ffn_w_down"] = inp["ffn_w_down"]
    m["od_w_qkv"] = inp["od_w_qkv"][0]
    m["od_w_out"] = inp["od_w_out"][0]
    m["qkg"] = np.ascontiguousarray(np.stack([inp["q_norm_g"][0], inp["k_norm_g"][0]], axis=-1))
    m["sinkr"] = np.ascontiguousarray(np.broadcast_to(inp["attn_sink"][0][None, :], (64, 16)))
    m["routerT"] = np.ascontiguousarray(inp["moe_router"][0].reshape(8, 128, 8).transpose(1, 0, 2))
    m["moe_w_gate"] = inp["_moe_wg_t"]
    m["moe_w_up"] = inp["_moe_wu_t"]
    m["moe_w_down"] = inp["_moe_wd_t"]
    for k, v in _CONSTS.items():
        m["k_" + k] = v
    return {k: np.ascontiguousarray(v) for k, v in m.items()}


def kernel(**inputs):
    global _CONSTS
    if _CONSTS is None:
        _CONSTS = host_consts()
    inp = {k: np.asarray(v) for k, v in inputs.items()}
    inp["_moe_wg_t"] = np.ascontiguousarray(inp["moe_w_gate"][0].reshape(NE, 8, 128, NFG, 256).transpose(0, 3, 2, 1, 4)).reshape(NE * NFG * 128, 2048)
    inp["_moe_wu_t"] = np.ascontiguousarray(inp["moe_w_up"][0].reshape(NE, 8, 128, NFG, 256).transpose(0, 3, 2, 1, 4)).reshape(NE * NFG * 128, 2048)
    inp["_moe_wd_t"] = np.ascontiguousarray(inp["moe_w_down"][0].reshape(NE, NFG, 2, 128, D).transpose(0, 1, 3, 2, 4)).reshape(NE * NFG * 128, 2048)
    nc = build()
    in_maps = [_core_inputs(inp, c) for c in range(8)]
    res = run_bass_kernel_spmd(nc, in_maps, core_ids=list(range(8)))
    return np.concatenate([r["out"] for r in res.results], axis=0).astype(np.float32)
```

```python
import math
import numpy as np
from contextlib import ExitStack
import ml_dtypes
import concourse.bass as bass
import concourse.mybir as mybir
from concourse.bass_utils import run_bass_kernel_spmd

F32 = mybir.dt.float32
BF16 = mybir.dt.bfloat16
AF = mybir.ActivationFunctionType
ALU = mybir.AluOpType
NPBF = ml_dtypes.bfloat16

D = 1024
SEQ = 4096
CTX = 256
DFF = 3584
NE = 8
EPS = 1e-6
PI = math.pi
SBLK = 1024
NBLK = (2 * 2 * SEQ) // SBLK + NE - 1
NSLOT = NBLK * SBLK
NFG = 14
SPARSE = True


class Prog:
    COMPUTE = ("pe", "dve", "act", "pool")

    def __init__(self, nc, es, kq=8):
        self.nc = nc
        self.engs = {"pe": nc.tensor, "dve": nc.vector, "act": nc.scalar, "pool": nc.gpsimd, "sp": nc.sync}
        self.sem = {}
        self.cnt = {}
        for e in self.COMPUTE:
            self.sem[e] = es.enter_context(nc.semaphore("s_" + e))
            self.cnt[e] = 0
        self.kq = kq
        self.dsem = {}
        self.dn = {}
        for q in ("sp", "pool", "act"):
            self.dsem[q] = [es.enter_context(nc.semaphore("d_%s%d" % (q, i))) for i in range(kq)]
            self.dn[q] = 0
        self.known = {e: {} for e in self.engs}
        self.res = {}
        self.ninst = 0

    def _deps(self, reads, writes):
        ev = {}
        for r in reads:
            st = self.res.get(r)
            if st:
                for k, v in st["w"].items():
                    if ev.get(k, 0) < v:
                        ev[k] = v
        for w in writes:
            st = self.res.get(w)
            if st:
                for k, v in st["w"].items():
                    if ev.get(k, 0) < v:
                        ev[k] = v
                for k, v in st["r"].items():
                    if ev.get(k, 0) < v:
                        ev[k] = v
        return ev

    def _semobj(self, k):
        return self.sem[k[1]] if k[0] == "c" else self.dsem[k[1]][k[2]]

    def _wait(self, e, ev, skip_key=None):
        eng = self.engs[e]
        kn = self.known[e]
        for k, v in ev.items():
            if k == skip_key or kn.get(k, 0) >= v:
                continue
            eng.wait_ge(self._semobj(k), v)
            kn[k] = v

    def _commit(self, key, val, reads, writes):
        for r in reads:
            st = self.res.setdefault(r, {"w": {}, "r": {}})
            st["r"][key] = val
        for w in writes:
            st = self.res.setdefault(w, {"w": {}, "r": {}})
            st["w"][key] = val

    def op(self, e, fn, reads=(), writes=()):
        ev = self._deps(reads, writes)
        key = ("c", e)
        self._wait(e, ev, skip_key=key if e == "pe" else None)
        ins = fn(self.engs[e])
        self.cnt[e] += 1
        ins.then_inc(self.sem[e], 1)
        self._commit(key, self.cnt[e], reads, writes)
        self.ninst += 1

    def dma(self, q, out, in_, reads=(), writes=(), **kw):
        ev = self._deps(reads, writes)
        n = self.dn[q]
        j = n % self.kq
        val = 16 * (n // self.kq + 1)
        key = ("d", q, j)
        if val > 16 and ev.get(key, 0) < val - 16:
            ev[key] = val - 16
        self._wait(q, ev)
        ins = self.engs[q].dma_start(out=out, in_=in_, **kw)
        ins.then_inc(self.dsem[q][j], 16)
        self.dn[q] = n + 1
        self._commit(key, val, reads, writes)
        self.ninst += 1

    def idma(self, out, in_, out_off, in_off, bound, reads=(), writes=()):
        q = "pool"
        ev = self._deps(reads, writes)
        n = self.dn[q]
        j = n % self.kq
        val = 16 * (n // self.kq + 1)
        key = ("d", q, j)
        if val > 16 and ev.get(key, 0) < val - 16:
            ev[key] = val - 16
        self._wait(q, ev)
        oo = bass.IndirectOffsetOnAxis(ap=out_off, axis=0) if out_off is not None else None
        io = bass.IndirectOffsetOnAxis(ap=in_off, axis=0) if in_off is not None else None
        ins = self.nc.gpsimd.indirect_dma_start(out=out, out_offset=oo, in_=in_, in_offset=io)
        ins.then_inc(self.dsem[q][j], 16)
        self.dn[q] = n + 1
        self._commit(key, val, reads, writes)
        self.ninst += 1

    def barrier(self):
        ev = {}
        for e in self.COMPUTE:
            if self.cnt[e]:
                ev[("c", e)] = self.cnt[e]
        for q in self.dsem:
            n = self.dn[q]
            for j in range(self.kq):
                cntj = (n - j + self.kq - 1) // self.kq if n > j else 0
                if cntj:
                    ev[("d", q, j)] = 16 * cntj
        for e in self.engs:
            self._wait(e, dict(ev), skip_key=None)


def _tiled_table(fn, L):
    nk = L // 128
    a = np.arange(L, dtype=np.float64)
    full = fn(a[:, None], a[None, :])
    t = full.reshape(nk, 128, nk, 128).transpose(2, 1, 0, 3)
    return np.ascontiguousarray(t).astype(NPBF)


def host_consts():
    c = {}
    c["ident_f"] = np.eye(128, dtype=np.float32)
    c["ident_b"] = np.eye(128).astype(NPBF)
    c["ones_b"] = np.ones((128, 128)).astype(NPBF)
    c["ones_f"] = np.ones((128, 128), np.float32)
    for L, tag in ((SEQ, "4096"), (CTX, "256")):
        c["thc" + tag] = _tiled_table(lambda a, b: np.cos(PI * (2 * a + 1) * (2 * b + 1) / (4 * L)), L)
        c["ths" + tag] = _tiled_table(lambda a, b: np.sin(PI * (2 * a + 1) * (2 * b + 1) / (4 * L)), L)
        c["tfc" + tag] = _tiled_table(lambda a, b: np.cos(2 * PI * ((a * b) % L) / L), L)
        c["tfs" + tag] = _tiled_table(lambda a, b: np.sin(2 * PI * ((a * b) % L) / L), L)
        nk = L // 128
        f = np.arange(L, dtype=np.float64)
        al = PI * (2 * f + 1) / (4 * L)
        c["ca" + tag] = np.ascontiguousarray((np.cos(al) / L).reshape(nk, 128).T).astype(np.float32)
        c["sa" + tag] = np.ascontiguousarray((np.sin(al) / L).reshape(nk, 128).T).astype(np.float32)
        pos = np.arange(L, dtype=np.float32)
        t = pos / np.float32(max(L - 1, 1))
        w = np.float32(2.0 * PI / L) * pos
        bands = np.linspace(1e-4, 15, 16, dtype=np.float32)
        ang = w[:, None] * bands[None, :]
        emb = np.concatenate([t[:, None], np.cos(ang), -np.sin(ang)], axis=-1).astype(np.float32)
        c["emb" + tag] = np.ascontiguousarray(emb.T)
        c["negt" + tag] = np.ascontiguousarray((-t).reshape(nk, 128).T).astype(np.float32)
        t1 = (np.arange(L, dtype=np.float32) + 1) / np.float32(max(L - 1, 1))
        c["negt1" + tag] = np.ascontiguousarray((-t1).reshape(nk, 128).T).astype(np.float32)
    deltas = np.abs(np.linspace(math.log(1e-2) / 1.5, math.log(1e-2) / 0.3, 512, dtype=np.float32))
    c["delta_bc"] = np.ascontiguousarray(np.broadcast_to(deltas[None, :], (128, 512))).astype(np.float32)
    dd = np.arange(128, dtype=np.float64)
    ang = 2 * PI * ((dd[:, None] * dd[None, :]) % 128) / 128
    c["c128"] = np.cos(ang).astype(NPBF)
    c["ns128"] = (-np.sin(ang)).astype(NPBF)
    inv = (10000.0 ** (-np.arange(0, 32, 2, dtype=np.float32) / 32)).astype(np.float32)
    row = np.repeat(np.arange(SEQ // 64, dtype=np.float32), 64)
    col = np.tile(np.arange(64, dtype=np.float32), SEQ // 64)
    ar = (row[None, :] * inv[:, None]).astype(np.float32)
    ac = (col[None, :] * inv[:, None]).astype(np.float32)
    c["rope_cos"] = np.concatenate([np.cos(ar), np.cos(ar), np.cos(ac), np.cos(ac)], 0).astype(NPBF)
    c["rope_sin"] = np.concatenate([np.sin(ar), np.sin(ar), np.sin(ac), np.sin(ac)], 0).astype(NPBF)
    Rm = np.zeros((64, 64), np.float32)
    for base in (0, 32):
        for i in range(16):
            Rm[base + i, base + 16 + i] = -1.0
            Rm[base + 16 + i, base + i] = 1.0
    c["ropeRT"] = np.ascontiguousarray(np.concatenate([Rm.T, np.zeros((64, 64), np.float32)], 1)).astype(NPBF)
    j = np.arange(128)[:, None]
    r = np.arange(128)[None, :]
    mp = (j >= r).astype(np.float32)
    mn = (j <= r).astype(np.float32)
    c["maskp"] = np.ascontiguousarray(np.broadcast_to(mp[:, None, :], (128, 4, 128))).astype(np.float32)
    c["maskn"] = np.ascontiguousarray(np.broadcast_to(mn[:, None, :], (128, 4, 128))).astype(np.float32)
    sel = np.zeros((8, 8, 128), np.float32)
    for e in range(8):
        sel[e, e, :] = 1.0
    c["sel"] = sel
    ut = (np.arange(128)[:, None] < np.arange(128)[None, :]).astype(np.float32)
    c["utb"] = ut.astype(NPBF)
    c["iota_p"] = np.arange(128, dtype=np.float32).reshape(128, 1)
    c["bvals"] = np.ascontiguousarray(np.broadcast_to((np.arange(NBLK, dtype=np.float32) * SBLK)[None, :], (128, NBLK)))
    return c


_CONSTS = None


def build(debug=None, upto=99):
    debug = debug or set()
    nc = bass.Bass("TRN2", target_bir_lowering=False)
    ins = {}

    def inp(name, shape, dt=F32):
        ins[name] = nc.dram_tensor(name, list(shape), dt, kind="ExternalInput").ap()
        return ins[name]

    def scratch(name, shape, dt):
        kind = "ExternalOutput" if name in debug else "Internal"
        return nc.dram_tensor(name, list(shape), dt, kind=kind).ap()

    x_in = inp("x", [2, SEQ, D])
    ctx_in = inp("ctx", [2, CTX, D])
    cT = inp("cT", [128, 8, 3])
    ada_w = inp("ada_w", [2, D, 6 * D])
    ada_bT = inp("ada_bT", [128, 2, 48])
    n1g = inp("n1g", [128, 2, 8])
    n2g = inp("n2g", [128, 2, 8])
    w_in = inp("ev_w_in", [D, 2048])
    w_out0 = inp("ev_w_out", [D, D])
    cw = inp("cw", [128, 12, 3])
    cb = inp("cb", [128, 12])
    hyb = inp("hyb", [128, 4])
    hf_w0 = inp("hf_w0", [33, 64])
    hf_w1 = inp("hf_w1", [64, 64])
    hf_w2 = inp("hf_w2", [64, 64])
    hf_w3 = inp("hf_w3", [64, 1024])
    hf_vec = inp("hf_vec", [64, 4])
    ffn_wg = inp("ffn_w_gate", [1, D, DFF])
    ffn_wu = inp("ffn_w_up", [1, D, DFF])
    ffn_wd = inp("ffn_w_down", [1, DFF, D])
    w_qkv = inp("od_w_qkv", [D, 1536])
    w_out1 = inp("od_w_out", [D, D])
    qkg = inp("qkg", [64, 2])
    sinkr = inp("sinkr", [64, 16])
    routerT = inp("routerT", [128, 8, 8])
    moe_wg = inp("moe_w_gate", [NE * NFG * 128, 2048])
    moe_wu = inp("moe_w_up", [NE * NFG * 128, 2048])
    moe_wd = inp("moe_w_down", [NE * NFG * 128, 2048])
    cst = {}
    for k, v in _CONSTS.items():
        cst[k] = inp("k_" + k, v.shape, BF16 if v.dtype == NPBF else F32)
    out = nc.dram_tensor("out", [2, SEQ, D], F32, kind="ExternalOutput").ap()

    LS = [SEQ, SEQ, CTX, CTX]
    TAG = ["4096", "4096", "256", "256"]
    COL = [0, 1, 2, 2]
    xTd = [scratch("xTd%d" % i, [8, 128, LS[i]], F32) for i in range(4)]
    uTd = [scratch("uTd%d" % i, [16, 128, LS[i]], BF16) for i in range(4)]
    zTd = [scratch("zTd%d" % i, [4, 128, LS[i]], BF16) for i in range(4)]
    x0Td = [scratch("x0Td%d" % i, [4, 128, LS[i]], BF16) for i in range(4)]
    catTd = [scratch("catTd%d" % i, [8, 128, LS[i]], BF16) for i in range(4)]
    hTd = [scratch("hTd%d" % i, [8, 128, LS[i]], BF16) for i in range(4)]
    Md = {"4096": scratch("Md4096", [32, 128, 2, 512], F32), "256": scratch("Md256", [2, 128, 2, 512], F32)}
    attTd = [scratch("attTd%d" % i, [16, 64, SEQ], BF16) for i in range(2)]
    gTd = [scratch("gTd%d" % i, [8, SEQ], F32) for i in range(2)]
    Htok = scratch("Htok", [2 * SEQ, D], BF16)
    Xs = scratch("Xs", [NSLOT, D], BF16)
    Ys = scratch("Ys", [NSLOT, D], BF16)
    dbg_mod = scratch("dbg_mod", [128, 2 * 48 * 3], F32) if "dbg_mod" in debug else None
    dbg_plan = scratch("dbg_plan", [128, 168], F32) if "dbg_plan" in debug else None

    with ExitStack() as es0:
        P = Prog(nc, es0)
        cnt = [0]

        def sbt(es, shape, dt, name=None):
            cnt[0] += 1
            return es.enter_context(nc.sbuf_tensor(name or ("t%d" % cnt[0]), list(shape), dt))

        ps = [es0.enter_context(nc.psum_tensor("ps%d" % i, [128, 512], F32)) for i in range(8)]
        PSR = ["ps%d" % i for i in range(8)]

        def mm(bank, out_ap, lhsT, rhs, start, stop, reads):
            P.op("pe", lambda e: e.matmul(out_ap, lhsT=lhsT, rhs=rhs, start=start, stop=stop), reads=reads, writes=[PSR[bank]])

        ident_f = sbt(es0, [128, 128], F32)
        ident_b = sbt(es0, [128, 128], BF16)
        ones_b = sbt(es0, [128, 128], BF16)
        ones_f = sbt(es0, [128, 128], F32)
        epst = sbt(es0, [128, 1], F32)
        mod = sbt(es0, [128, 2, 48, 3], F32)
        gs = sbt(es0, [128, 2, 2, 8, 3], F32)
        for t, k in ((ident_f, "ident_f"), (ident_b, "ident_b"), (ones_b, "ones_b"), (ones_f, "ones_f")):
            P.dma("sp", t[:], cst[k][:, :], writes=["const"])
        P.op("dve", lambda e: e.memset(epst[:], EPS), writes=["const"])

        I32 = mybir.dt.int32
        MK = sbt(es0, [128, 64, 16], F32)
        GV = sbt(es0, [128, 64, 2], F32)
        dsl = sbt(es0, [128, 128], I32)
        idxw = sbt(es0, [128, NBLK * NFG], I32)
        with ExitStack() as es:
            cT_sb = sbt(es, [128, 8, 3], F32)
            sT = sbt(es, [128, 8, 3], F32)
            abT = sbt(es, [128, 2, 48], F32)
            g1 = sbt(es, [128, 2, 8], F32)
            g2 = sbt(es, [128, 2, 8], F32)
            tmpm = sbt(es, [128, 8, 3], F32)
            aw = [sbt(es, [128, 8, 768], F32) for _ in range(2)]
            zt0 = sbt(es, [128, 8, D], BF16)
            P.op("pool", lambda e: e.memset(zt0[:], 0.0), writes=["zt0"])
            for r_ in range(NSLOT // 1024):
                P.dma("pool", Xs[r_ * 1024:(r_ + 1) * 1024, :].rearrange("(t p) d -> p t d", p=128), zt0[:], reads=["zt0"], writes=["Xs"])
            P.dma("sp", cT_sb[:], cT[:, :, :], writes=["cT"])
            P.dma("sp", abT[:], ada_bT[:, :, :], writes=["abT"])
            P.dma("sp", g1[:], n1g[:, :, :], writes=["g12"])
            P.dma("sp", g2[:], n2g[:, :, :], writes=["g12"])
            P.op("act", lambda e: e.activation(out=sT[:], in_=cT_sb[:], func=AF.Silu), reads=["cT"], writes=["sT"])
            it = 0
            for l in range(2):
                awv = ada_w[l].rearrange("(k p) n -> p k n", p=128)
                for nb in range(8):
                    b = it % 2
                    it += 1
                    P.dma("sp", aw[b][:], awv[:, :, nb * 768:(nb + 1) * 768], writes=["aw%d" % b])
                    for j in range(6):
                        n = nb * 6 + j
                        bank = n % 2
                        for k in range(8):
                            mm(bank, ps[bank][:, 0:3], aw[b][:, k, j * 128:(j + 1) * 128], sT[:, k, :], k == 0, k == 7, ["aw%d" % b, "sT"])
                        P.op("dve", lambda e: e.tensor_scalar(out=mod[:, l, n, :], in0=ps[bank][:, 0:3], scalar1=abT[:, l, n:n + 1], scalar2=None, op0=ALU.add),
                             reads=[PSR[bank], "abT"], writes=["mod"])
            for l in range(2):
                for w, gt in ((0, g1), (1, g2)):
                    P.op("dve", lambda e: e.tensor_scalar(out=tmpm[:], in0=mod[:, l, 8 + 24 * w:16 + 24 * w, :], scalar1=1.0, scalar2=None, op0=ALU.add),
                         reads=["mod"], writes=["tmpm"])
                    for col in range(3):
                        P.op("dve", lambda e: e.tensor_tensor(out=gs[:, l, w, :, col], in0=tmpm[:, :, col], in1=gt[:, l, :], op=ALU.mult),
                             reads=["tmpm", "g12"], writes=["mod"])
            if dbg_mod is not None:
                P.dma("sp", dbg_mod[:, :], mod[:].rearrange("p a b c -> p (a b c)"), reads=["mod"], writes=["dbg_mod"])
            P.barrier()

        def SH(l, w, c, col):
            return mod[:, l, 24 * w + c, col:col + 1]

        def GATE(l, w, c, col):
            return mod[:, l, 16 + 24 * w + c, col:col + 1]

        def GS(l, w, c, col):
            return gs[:, l, w, c, col:col + 1]

        def norm_block(es_t, xt, rx, W, l, w, col, outb, rout, tiles, outf=None):
            sq, rs, tmp = tiles
            P.op("act", lambda e: e.activation(out=sq[:, :, :W], in_=xt[:, :, :W], func=AF.Square), reads=[rx], writes=["n_sq"])
            for c in range(8):
                mm(7, ps[7][:, :W], ones_b[:], sq[:, c, :W], c == 0, c == 7, ["n_sq", "const"])
            P.op("act", lambda e: e.activation(out=rs[:, :W], in_=ps[7][:, :W], func=AF.Ln, bias=epst[:, 0:1], scale=1.0 / D),
                 reads=[PSR[7], "const"], writes=["n_rs"])
            P.op("act", lambda e: e.activation(out=rs[:, :W], in_=rs[:, :W], func=AF.Exp, scale=-0.5), reads=["n_rs"], writes=["n_rs"])
            for c in range(8):
                P.op("dve", lambda e: e.scalar_tensor_tensor(out=tmp[:, c, :W], in0=xt[:, c, :W], scalar=GS(l, w, c, col), in1=rs[:, :W], op0=ALU.mult, op1=ALU.mult),
                     reads=[rx, "n_rs", "mod"], writes=["n_tmp"])
                P.op("act", lambda e: e.activation(out=outb[:, c, :W], in_=tmp[:, c, :W], func=AF.Identity, bias=SH(l, w, c, col), scale=1.0),
                     reads=["n_tmp", "mod"], writes=[rout])
                if outf is not None:
                    P.op("act", lambda e: e.activation(out=outf[:, c, :W], in_=tmp[:, c, :W], func=AF.Identity, bias=SH(l, w, c, col), scale=1.0),
                         reads=["n_tmp", "mod"], writes=[rout + "f"])

        def load_cast_w(es, src_view, kch, ncols, name, npart=128):
            wb = sbt(es, [npart, kch, ncols], BF16)
            with ExitStack() as e2:
                st = sbt(e2, [npart, kch, 512], F32)
                for j in range(0, ncols, 512):
                    P.dma("sp", st[:], src_view[:, :, j:j + 512], writes=["wstage"])
                    P.op("act", lambda e: e.copy(out=wb[:, :, j:j + 512], in_=st[:]), reads=["wstage"], writes=[name])
                P.barrier()
            return wb

        items0 = [0, 1, 2, 3]

        def xsrc(it):
            return x_in[it] if it < 2 else ctx_in[it - 2]

        if upto >= 1:
            with ExitStack() as es:
                win_b = load_cast_w(es, w_in.rearrange("(k p) n -> p k n", p=128), 8, 2048, "win_b")
                xin = [sbt(es, [128, 4, D], F32) for _ in range(2)]
                xts = [sbt(es, [128, 8, 512], F32) for _ in range(2)]
                sq = sbt(es, [128, 8, 512], BF16)
                rs = sbt(es, [128, 512], F32)
                tmp = sbt(es, [128, 8, 512], F32)
                hxs = [sbt(es, [128, 8, 512], BF16) for _ in range(2)]
                uts = [sbt(es, [128, 16, 512], BF16) for _ in range(2)]
                ablocks = []
                for it in items0:
                    L = LS[it]
                    W = min(512, L)
                    for blk in range(L // W):
                        ablocks.append((it, blk, W))

                def stT(i):
                    it, blk, W = ablocks[i]
                    b = i % 2
                    xt = xts[b]
                    t0 = blk * W
                    nt = W // 128
                    P.dma("sp", xin[b][:, 0:nt, :], xsrc(it)[t0:t0 + W, :].rearrange("(t p) d -> p t d", p=128), writes=["xin%d" % b])
                    for c in range(8):
                        bank = c % 4
                        for t in range(nt):
                            P.op("pe", lambda e: e.transpose(out=ps[bank][:, t * 128:(t + 1) * 128], in_=xin[b][:, t, c * 128:(c + 1) * 128], identity=ident_f[:]),
                                 reads=["xin%d" % b, "const"], writes=[PSR[bank]])
                        if c % 2:
                            P.op("act", lambda e: e.copy(out=xt[:, c, :W], in_=ps[bank][:, :W]), reads=[PSR[bank]], writes=["xt%d" % b])
                        else:
                            P.op("dve", lambda e: e.tensor_copy(out=xt[:, c, :W], in_=ps[bank][:, :W]), reads=[PSR[bank]], writes=["xt%d" % b])
                    P.dma("pool", xTd[it].rearrange("c p t -> p c t")[:, :, t0:t0 + W], xt[:, :, :W], reads=["xt%d" % b], writes=["xTd%d_%d" % (it, blk)])

                def stN(i):
                    it, blk, W = ablocks[i]
                    b = i % 2
                    norm_block(es, xts[b], "xt%d" % b, W, 0, 0, COL[it], hxs[b], "hx%d" % b, (sq, rs, tmp))

                def stU(i):
                    it, blk, W = ablocks[i]
                    b = i % 2
                    hx = hxs[b]
                    ut = uts[b]
                    t0 = blk * W
                    for n in range(16):
                        bank = 4 + n % 2
                        for k in range(8):
                            mm(bank, ps[bank][:, :W], win_b[:, k, n * 128:(n + 1) * 128], hx[:, k, :W], k == 0, k == 7, ["win_b", "hx%d" % b])
                        if n % 2:
                            P.op("act", lambda e: e.copy(out=ut[:, n, :W], in_=ps[bank][:, :W]), reads=[PSR[bank]], writes=["ut%d" % b])
                        else:
                            P.op("dve", lambda e: e.tensor_copy(out=ut[:, n, :W], in_=ps[bank][:, :W]), reads=[PSR[bank]], writes=["ut%d" % b])
                    P.dma("pool", uTd[it].rearrange("c p t -> p c t")[:, :, t0:t0 + W], ut[:, :, :W], reads=["ut%d" % b], writes=["uTd%d_%d" % (it, blk)])

                nab = len(ablocks)
                stT(0)
                for i in range(nab):
                    if i + 1 < nab:
                        stT(i + 1)
                    stN(i)
                    if i >= 1:
                        stU(i - 1)
                stU(nab - 1)
                P.barrier()

        def blocks_of(it):
            L = LS[it]
            return range(L // min(512, L))

        if upto >= 2:
            for tag, L in (("4096", SEQ), ("256", CTX)):
                nK = L // 128
                W = min(512, L)
                with ExitStack() as es:
                    emb = sbt(es, [33, L], F32)
                    hA = sbt(es, [64, L + 1], F32)
                    hB = sbt(es, [64, L + 1], F32)
                    w0s = sbt(es, [33, 64], F32)
                    w1s = sbt(es, [64, 64], F32)
                    w2s = sbt(es, [64, 64], F32)
                    w3s = sbt(es, [64, 1024], F32)
                    vec = sbt(es, [64, 4], F32)
                    fb = sbt(es, [64, 3], F32)
                    arg = sbt(es, [64, 512], F32)
                    msk = sbt(es, [64, 512], F32)
                    negt = sbt(es, [128, nK], F32)
                    negt1 = sbt(es, [128, nK], F32)
                    ca = sbt(es, [128, nK], F32)
                    sa = sbt(es, [128, nK], F32)
                    dbc = sbt(es, [128, 512], F32)
                    A0 = sbt(es, [128, nK, 512], BF16)
                    B0 = sbt(es, [128, nK, 512], BF16)
                    dec = sbt(es, [128, 512], F32)
                    dec1 = sbt(es, [128, 512], F32)
                    hf = sbt(es, [128, 512], F32)
                    hb = sbt(es, [128, 512], F32)
                    sqf = sbt(es, [128, 512], F32)
                    sqb = sbt(es, [128, 512], F32)
                    rn = sbt(es, [128, 512], F32)
                    tC = [sbt(es, [128, nK, 128], BF16) for _ in range(2)]
                    tS = [sbt(es, [128, nK, 128], BF16) for _ in range(2)]
                    t1 = sbt(es, [128, 512], F32)
                    t2 = sbt(es, [128, 512], F32)
                    t3 = sbt(es, [128, 512], F32)
                    Mt = [sbt(es, [128, 2, 512], F32) for _ in range(2)]
                    for t, src in ((emb, cst["emb" + tag]), (w0s, hf_w0), (w1s, hf_w1), (w2s, hf_w2), (w3s, hf_w3), (vec, hf_vec),
                                   (negt, cst["negt" + tag]), (negt1, cst["negt1" + tag]), (ca, cst["ca" + tag]), (sa, cst["sa" + tag]),
                                   (dbc, cst["delta_bc"])):
                        P.dma("sp", t[:], src[:, :], writes=["fconst"])
                    P.op("dve", lambda e: e.memset(hA[:], 0.0), writes=["hA"])
                    for i in range(3):
                        P.op("dve", lambda e: e.tensor_tensor(out=fb[:, i:i + 1], in0=vec[:, i:i + 1], in1=vec[:, 3:4], op=ALU.mult), reads=["fconst"], writes=["fb"])
                    layers = ((w0s, emb, "fconst", hA, "hA", 0), (w1s, hA, "hA", hB, "hB", 1), (w2s, hB, "hB", hA, "hA", 2))
                    for (ws, src, rsrc, dst, rdst, li) in layers:
                        for cbk in range(L // W):
                            cs = slice(cbk * W, (cbk + 1) * W)
                            mm(0, ps[0][0:64, :W], ws[:], src[:, cs], True, True, ["fconst", rsrc])
                            P.op("dve", lambda e: e.tensor_scalar(out=arg[:, :W], in0=ps[0][0:64, :W], scalar1=vec[:, 3:4], scalar2=fb[:, li:li + 1], op0=ALU.mult, op1=ALU.add),
                                 reads=[PSR[0], "fconst", "fb"], writes=["arg"])
                            for _rep in range(2):
                                P.op("dve", lambda e: e.tensor_scalar(out=msk[:, :W], in0=arg[:, :W], scalar1=PI, scalar2=2 * PI, op0=ALU.is_gt, op1=ALU.mult), reads=["arg"], writes=["msk"])
                                P.op("dve", lambda e: e.tensor_tensor(out=arg[:, :W], in0=arg[:, :W], in1=msk[:, :W], op=ALU.subtract), reads=["arg", "msk"], writes=["arg"])
                                P.op("dve", lambda e: e.tensor_scalar(out=msk[:, :W], in0=arg[:, :W], scalar1=-PI, scalar2=2 * PI, op0=ALU.is_lt, op1=ALU.mult), reads=["arg"], writes=["msk"])
                                P.op("dve", lambda e: e.tensor_tensor(out=arg[:, :W], in0=arg[:, :W], in1=msk[:, :W], op=ALU.add), reads=["arg", "msk"], writes=["arg"])
                            P.op("act", lambda e: e.activation(out=dst[:, cs], in_=arg[:, :W], func=AF.Sin), reads=["arg"], writes=[rdst])
                    for jc in range(nK):
                        mm(0, ps[0][:, :], hA[:, jc * 128:(jc + 1) * 128], w3s[:, 0:512], True, True, ["hA", "fconst"])
                        mm(1, ps[1][:, :], hA[:, jc * 128 + 1:(jc + 1) * 128 + 1], w3s[:, 512:1024], True, True, ["hA", "fconst"])
                        P.op("act", lambda e: e.activation(out=dec[:], in_=dbc[:], func=AF.Exp, scale=negt[:, jc:jc + 1]), reads=["fconst"], writes=["dec"])
                        P.op("act", lambda e: e.activation(out=dec1[:], in_=dbc[:], func=AF.Exp, scale=negt1[:, jc:jc + 1]), reads=["fconst"], writes=["dec1"])
                        P.op("dve", lambda e: e.tensor_tensor(out=hf[:], in0=ps[0][:, :], in1=dec[:], op=ALU.mult), reads=[PSR[0], "dec"], writes=["hf"])
                        P.op("dve", lambda e: e.tensor_tensor(out=hb[:], in0=ps[1][:, :], in1=dec1[:], op=ALU.mult), reads=[PSR[1], "dec1"], writes=["hb"])
                        P.op("pool", lambda e: e.tensor_tensor(out=A0[:, jc, :], in0=hf[:], in1=hb[:], op=ALU.add), reads=["hf", "hb"], writes=["A0"])
                        P.op("pool", lambda e: e.tensor_tensor(out=B0[:, jc, :], in0=hb[:], in1=hf[:], op=ALU.subtract), reads=["hf", "hb"], writes=["B0"])
                        P.op("dve", lambda e: e.tensor_tensor(out=sqf[:], in0=hf[:], in1=hf[:], op=ALU.mult), reads=["hf"], writes=["sqf"])
                        P.op("dve", lambda e: e.tensor_tensor(out=sqb[:], in0=hb[:], in1=hb[:], op=ALU.mult), reads=["hb"], writes=["sqb"])
                        mm(7, ps[7][:, :], ones_f[:], sqf[:], jc == 0, False, ["sqf", "const"])
                        mm(7, ps[7][:, :], ones_f[:], sqb[:], False, jc == nK - 1, ["sqb", "const"])
                    P.op("act", lambda e: e.activation(out=rn[:], in_=ps[7][:, :], func=AF.Sqrt, bias=epst[:, 0:1], scale=1.0), reads=[PSR[7], "const"], writes=["rn"])
                    P.op("dve", lambda e: e.reciprocal(out=rn[:], in_=rn[:]), reads=["rn"], writes=["rn"])
                    for m in range(nK):
                        b = m % 2
                        P.dma("sp", tC[b][:], cst["thc" + tag][m], writes=["tC%d" % b])
                        P.dma("sp", tS[b][:], cst["ths" + tag][m], writes=["tS%d" % b])
                        for k in range(nK):
                            mm(2 * b, ps[2 * b][:, :], tC[b][:, k, :], A0[:, k, :], k == 0, k == nK - 1, ["tC%d" % b, "A0"])
                        for k in range(nK):
                            mm(2 * b + 1, ps[2 * b + 1][:, :], tS[b][:, k, :], B0[:, k, :], k == 0, k == nK - 1, ["tS%d" % b, "B0"])
                        P.op("dve", lambda e: e.tensor_tensor(out=t1[:], in0=ps[2 * b][:, :], in1=rn[:], op=ALU.mult), reads=[PSR[2 * b], "rn"], writes=["t1"])
                        P.op("dve", lambda e: e.tensor_tensor(out=t2[:], in0=ps[2 * b + 1][:, :], in1=rn[:], op=ALU.mult), reads=[PSR[2 * b + 1], "rn"], writes=["t2"])
                        P.op("pool", lambda e: e.tensor_scalar(out=t3[:], in0=t2[:], scalar1=sa[:, m:m + 1], scalar2=None, op0=ALU.mult), reads=["t2", "fconst"], writes=["t3"])
                        P.op("dve", lambda e: e.scalar_tensor_tensor(out=Mt[b][:, 0, :], in0=t1[:], scalar=ca[:, m:m + 1], in1=t3[:], op0=ALU.mult, op1=ALU.subtract),
                             reads=["t1", "t3", "fconst"], writes=["Mt%d" % b])
                        P.op("pool", lambda e: e.tensor_scalar(out=t3[:], in0=t2[:], scalar1=ca[:, m:m + 1], scalar2=None, op0=ALU.mult), reads=["t2", "fconst"], writes=["t3"])
                        P.op("dve", lambda e: e.scalar_tensor_tensor(out=Mt[b][:, 1, :], in0=t1[:], scalar=sa[:, m:m + 1], in1=t3[:], op0=ALU.mult, op1=ALU.add),
                             reads=["t1", "t3", "fconst"], writes=["Mt%d" % b])
                        P.dma("act", Md[tag][m], Mt[b][:], reads=["Mt%d" % b], writes=["Md" + tag])
                    P.barrier()

        if upto >= 3:
            for it in items0:
                L = LS[it]
                tag = TAG[it]
                nK = L // 128
                NB = list(blocks_of(it))
                with ExitStack() as esI:
                    z_tok = sbt(esI, [128, nK, 512], BF16)
                    tC = [sbt(esI, [128, nK, 128], BF16) for _ in range(2)]
                    tS = [sbt(esI, [128, nK, 128], BF16) for _ in range(2)]
                    with ExitStack() as es:
                        cws = sbt(es, [128, 12, 3], F32)
                        cbs = sbt(es, [128, 12], F32)
                        dg = sbt(es, [128, 36, 128], BF16)
                        pad = [sbt(es, [128, L + 2], BF16) for _ in range(3)]
                        v32 = [sbt(es, [128, 512], F32) for _ in range(2)]
                        zT = sbt(es, [128, L], BF16)
                        x0T = sbt(es, [128, L], BF16)
                        P.dma("sp", cws[:], cw[:, :, :], writes=["cws"])
                        P.dma("sp", cbs[:], cb[:, :], writes=["cws"])
                        for p_ in range(3):
                            P.op("pool", lambda e: e.memset(pad[p_][:], 0.0), writes=["pad%d" % p_])
                        for pi in range(12):
                            for tap in range(3):
                                P.op("dve", lambda e: e.tensor_scalar(out=dg[:, pi * 3 + tap, :], in0=ident_b[:], scalar1=cws[:, pi, tap:tap + 1], scalar2=None, op0=ALU.mult),
                                     reads=["const", "cws"], writes=["dg"])
                        Wc = min(512, L)
                        vi = 0
                        for c in range(4):
                            for part in range(3):
                                ch = 4 + part * 4 + c
                                P.dma("sp", pad[part][:, 1:L + 1], uTd[it][ch], reads=["uTd%d_%d" % (it, b_) for b_ in NB], writes=["pad%d" % part])
                            for blk in range(L // Wc):
                                t0 = blk * Wc
                                for part in range(3):
                                    pi = part * 4 + c
                                    for tap in range(3):
                                        mm(part, ps[part][:, :Wc], dg[:, pi * 3 + tap, :], pad[part][:, t0 + tap:t0 + tap + Wc], tap == 0, tap == 2, ["dg", "pad%d" % part])
                                vb = vi % 2
                                vi += 1
                                P.op("act", lambda e: e.activation(out=v32[vb][:, :Wc], in_=ps[0][:, :Wc], func=AF.Identity, bias=cbs[:, c:c + 1], scale=1.0), reads=[PSR[0], "cws"], writes=["v32_%d" % vb])
                                P.op("dve", lambda e: e.scalar_tensor_tensor(out=zT[:, t0:t0 + Wc], in0=ps[1][:, :Wc], scalar=cbs[:, 4 + c:5 + c], in1=v32[vb][:, :Wc], op0=ALU.add, op1=ALU.mult),
                                     reads=[PSR[1], "cws", "v32_%d" % vb], writes=["zT"])
                                P.op("act", lambda e: e.activation(out=x0T[:, t0:t0 + Wc], in_=ps[2][:, :Wc], func=AF.Identity, bias=cbs[:, 8 + c:9 + c], scale=1.0), reads=[PSR[2], "cws"], writes=["x0T"])
                            P.dma("pool", zTd[it][c], zT[:], reads=["zT"], writes=["zTd%d" % it])
                            P.dma("pool", x0Td[it][c], x0T[:], reads=["x0T"], writes=["x0Td%d" % it])
                            for l4 in range(0, nK, 4):
                                nn = min(4, nK - l4)
                                bank = 4 + (l4 // 4) % 2
                                for q in range(nn):
                                    lc = l4 + q
                                    mm(bank, ps[bank][:, q * 128:(q + 1) * 128], zT[:, lc * 128:(lc + 1) * 128], ident_b[:], True, True, ["zT", "const"])
                                P.op("act", lambda e: e.copy(out=z_tok[:, l4:l4 + nn, c * 128:(c + 1) * 128], in_=ps[bank][:, 0:nn * 128].rearrange("p (a b) -> p a b", b=128)),
                                     reads=[PSR[bank]], writes=["z_tok"])
                        P.barrier()
                    Wre = sbt(esI, [128, nK, 512], BF16)
                    nWim = sbt(esI, [128, nK, 512], BF16)
                    with ExitStack() as es:
                        Mt = [sbt(es, [128, 2, 512], F32) for _ in range(2)]
                        p1 = sbt(es, [128, 512], F32)
                        p2 = sbt(es, [128, 512], F32)
                        p3 = sbt(es, [128, 512], F32)
                        p4 = sbt(es, [128, 512], F32)
                        for m in range(nK):
                            b = m % 2
                            P.dma("sp", tC[b][:], cst["thc" + tag][m], writes=["tC%d" % b])
                            P.dma("sp", tS[b][:], cst["ths" + tag][m], writes=["tS%d" % b])
                            P.dma("sp", Mt[b][:], Md[tag][m], reads=["Md" + tag], writes=["Mt%d" % b])
                            ba, bb = 2 * b, 2 * b + 1
                            for k in range(nK):
                                mm(ba, ps[ba][:, :], tC[b][:, k, :], z_tok[:, k, :], k == 0, k == nK - 1, ["tC%d" % b, "z_tok"])
                            for k in range(nK):
                                mm(bb, ps[bb][:, :], tS[b][:, k, :], z_tok[:, k, :], k == 0, k == nK - 1, ["tS%d" % b, "z_tok"])
                            P.op("dve", lambda e: e.tensor_tensor(out=p1[:], in0=ps[ba][:, :], in1=Mt[b][:, 0, :], op=ALU.mult), reads=[PSR[ba], "Mt%d" % b], writes=["p1"])
                            P.op("dve", lambda e: e.tensor_tensor(out=p2[:], in0=ps[bb][:, :], in1=Mt[b][:, 1, :], op=ALU.mult), reads=[PSR[bb], "Mt%d" % b], writes=["p2"])
                            P.op("pool", lambda e: e.tensor_tensor(out=Wre[:, m, :], in0=p1[:], in1=p2[:], op=ALU.add), reads=["p1", "p2"], writes=["Wre"])
                            P.op("dve", lambda e: e.tensor_tensor(out=p3[:], in0=ps[bb][:, :], in1=Mt[b][:, 0, :], op=ALU.mult), reads=[PSR[bb], "Mt%d" % b], writes=["p3"])
                            P.op("dve", lambda e: e.tensor_tensor(out=p4[:], in0=ps[ba][:, :], in1=Mt[b][:, 1, :], op=ALU.mult), reads=[PSR[ba], "Mt%d" % b], writes=["p4"])
                            P.op("pool", lambda e: e.tensor_tensor(out=nWim[:, m, :], in0=p3[:], in1=p4[:], op=ALU.subtract), reads=["p3", "p4"], writes=["nWim"])
                        P.barrier()
                    with ExitStack() as es:
                        hybs = sbt(es, [128, 4], F32)
                        ysb = sbt(es, [128, 512], BF16)
                        zs = [sbt(es, [128, 4, 128], BF16) for _ in range(2)]
                        xs = [sbt(es, [128, 4, 128], BF16) for _ in range(2)]
                        tq = sbt(es, [128, 4, 128], F32)
                        bT = [sbt(es, [128, 4, 128], BF16) for _ in range(2)]
                        P.dma("sp", hybs[:], hyb[:, :], writes=["hybs"])
                        for m in range(nK):
                            b = m % 2
                            P.dma("sp", tC[b][:], cst["thc" + tag][m], writes=["tC%d" % b])
                            P.dma("sp", tS[b][:], cst["ths" + tag][m], writes=["tS%d" % b])
                            P.dma("sp", zs[b][:], zTd[it].rearrange("c p t -> p c t")[:, :, m * 128:(m + 1) * 128], reads=["zTd%d" % it], writes=["zs%d" % b])
                            P.dma("sp", xs[b][:], x0Td[it].rearrange("c p t -> p c t")[:, :, m * 128:(m + 1) * 128], reads=["x0Td%d" % it], writes=["xs%d" % b])
                            for k in range(nK):
                                mm(b, ps[b][:, :], tC[b][:, k, :], Wre[:, k, :], k == 0, False, ["tC%d" % b, "Wre"])
                            for k in range(nK):
                                mm(b, ps[b][:, :], tS[b][:, k, :], nWim[:, k, :], False, k == nK - 1, ["tS%d" % b, "nWim"])
                            P.op("act", lambda e: e.copy(out=ysb[:], in_=ps[b][:, :]), reads=[PSR[b]], writes=["ysb"])
                            for c in range(4):
                                mm(2 + b, ps[2 + b][:, c * 128:(c + 1) * 128], ysb[:, c * 128:(c + 1) * 128], ident_b[:], True, True, ["ysb", "const"])
                            for c in range(4):
                                P.op("dve", lambda e: e.scalar_tensor_tensor(out=tq[:, c, :], in0=zs[b][:, c, :], scalar=hybs[:, c:c + 1], in1=ps[2 + b][:, c * 128:(c + 1) * 128], op0=ALU.mult, op1=ALU.add),
                                     reads=["zs%d" % b, "hybs", PSR[2 + b]], writes=["tq"])
                            P.op("pool", lambda e: e.tensor_tensor(out=bT[b][:], in0=tq[:], in1=xs[b][:], op=ALU.mult), reads=["tq", "xs%d" % b], writes=["bT%d" % b])
                            P.dma("pool", catTd[it].rearrange("c p t -> p c t")[:, 4:8, m * 128:(m + 1) * 128], bT[b][:], reads=["bT%d" % b], writes=["catTd%d" % it])
                        P.barrier()
                    with ExitStack() as es:
                        UfT = sbt(es, [128, 4, L], BF16)
                        c128 = sbt(es, [128, 128], BF16)
                        ns128 = sbt(es, [128, 128], BF16)
                        asb = sbt(es, [128, 512], BF16)
                        aT = [sbt(es, [128, 4, 128], BF16) for _ in range(2)]
                        P_tok, Q_tok = Wre, nWim
                        P.dma("sp", c128[:], cst["c128"][:, :], writes=["c128"])
                        P.dma("sp", ns128[:], cst["ns128"][:, :], writes=["c128"])
                        P.dma("sp", UfT[:], uTd[it].rearrange("c p t -> p c t")[:, 0:4, :], reads=["uTd%d_%d" % (it, b_) for b_ in NB], writes=["UfT"])
                        for lc in range(nK):
                            b = lc % 2
                            for g in range(4):
                                mm(b, ps[b][:, g * 128:(g + 1) * 128], UfT[:, g, lc * 128:(lc + 1) * 128], c128[:], True, True, ["UfT", "c128"])
                            for g in range(4):
                                mm(2 + b, ps[2 + b][:, g * 128:(g + 1) * 128], UfT[:, g, lc * 128:(lc + 1) * 128], ns128[:], True, True, ["UfT", "c128"])
                            P.op("act", lambda e: e.copy(out=P_tok[:, lc, :], in_=ps[b][:, :]), reads=[PSR[b]], writes=["Wre"])
                            P.op("dve", lambda e: e.tensor_copy(out=Q_tok[:, lc, :], in_=ps[2 + b][:, :]), reads=[PSR[2 + b]], writes=["nWim"])
                        sc = 1.0 / math.sqrt(128.0 * L)
                        for m in range(nK):
                            b = m % 2
                            P.dma("sp", tC[b][:], cst["tfc" + tag][m], writes=["tC%d" % b])
                            P.dma("sp", tS[b][:], cst["tfs" + tag][m], writes=["tS%d" % b])
                            for k in range(nK):
                                mm(4 + b, ps[4 + b][:, :], tC[b][:, k, :], P_tok[:, k, :], k == 0, False, ["tC%d" % b, "Wre"])
                            for k in range(nK):
                                mm(4 + b, ps[4 + b][:, :], tS[b][:, k, :], Q_tok[:, k, :], False, k == nK - 1, ["tS%d" % b, "nWim"])
                            P.op("act", lambda e: e.activation(out=asb[:], in_=ps[4 + b][:, :], func=AF.Copy, scale=sc), reads=[PSR[4 + b]], writes=["asb"])
                            for g in range(4):
                                mm(6 + b, ps[6 + b][:, g * 128:(g + 1) * 128], asb[:, g * 128:(g + 1) * 128], ident_b[:], True, True, ["asb", "const"])
                            P.op("dve", lambda e: e.tensor_copy(out=aT[b][:], in_=ps[6 + b][:, :].rearrange("p (a b) -> p a b", b=128)), reads=[PSR[6 + b]], writes=["aT%d" % b])
                            P.dma("pool", catTd[it].rearrange("c p t -> p c t")[:, 0:4, m * 128:(m + 1) * 128], aT[b][:], reads=["aT%d" % b], writes=["catTd%d" % it])
                    P.barrier()

        def outproj_phase(l, items, wout_view, kparts, cat_loader, router, npart=128):
            with ExitStack() as es:
                nkc = len(kparts)
                wb = load_cast_w(es, wout_view, nkc, D, "wout_b", npart=npart)
                xt = sbt(es, [128, 8, 512], F32)
                x1 = sbt(es, [128, 8, 512], F32)
                sq = sbt(es, [128, 8, 512], BF16)
                rs = sbt(es, [128, 512], F32)
                tmp = sbt(es, [128, 8, 512], F32)
                hx = sbt(es, [128, 8, 512], BF16)
                hxf = sbt(es, [128, 8, 512], F32) if router else None
                cat = cat_loader(es)
                if router:
                    rw = sbt(es, [128, 8, 8], F32)
                    lg = sbt(es, [128, 8], F32)
                    m1 = sbt(es, [128, 1], F32)
                    m2 = sbt(es, [128, 1], F32)
                    mk1 = sbt(es, [128, 8], F32)
                    mk2 = sbt(es, [128, 8], F32)
                    l2 = sbt(es, [128, 8], F32)
                    dd = sbt(es, [128, 1], F32)
                    g1t = sbt(es, [128, 1], F32)
                    g2t = sbt(es, [128, 1], F32)
                    gt = sbt(es, [128, 8], F32)
                    htok = [sbt(es, [128, D], BF16) for _ in range(2)]
                    P.dma("sp", rw[:], routerT[:, :, :], writes=["rw"])
                for it in items:
                    L = LS[it]
                    W = min(512, L)
                    for blk in range(L // W):
                        t0 = blk * W
                        rcat = cat(it, t0, W)
                        P.dma("sp", xt[:, :, :W], xTd[it].rearrange("c p t -> p c t")[:, :, t0:t0 + W], reads=["xTd%d_%d" % (it, blk)], writes=["xt"])
                        for n in range(8):
                            bank = n % 4
                            for ki, (lh, rh) in enumerate(kparts):
                                mm(bank, ps[bank][:, :W], lh(wb, n), rh(W), ki == 0, ki == nkc - 1, ["wout_b", rcat])
                            P.op("dve", lambda e: e.scalar_tensor_tensor(out=x1[:, n, :W], in0=ps[bank][:, :W], scalar=GATE(l, 0, n, COL[it]), in1=xt[:, n, :W], op0=ALU.mult, op1=ALU.add),
                                 reads=[PSR[bank], "xt", "mod"], writes=["x1"])
                        P.dma("pool", xTd[it].rearrange("c p t -> p c t")[:, :, t0:t0 + W], x1[:, :, :W], reads=["x1"], writes=["xTd%d_%d" % (it, blk)])
                        norm_block(es, x1, "x1", W, l, 1, COL[it], hx, "hx", (sq, rs, tmp), outf=hxf)
                        P.dma("pool", hTd[it].rearrange("c p t -> p c t")[:, :, t0:t0 + W], hx[:, :, :W], reads=["hx"], writes=["hTd%d_%d" % (it, blk)])
                        if router:
                            for t in range(W // 128):
                                ts_ = slice(t * 128, (t + 1) * 128)
                                for k in range(8):
                                    mm(4, ps[4][:, 0:8], hxf[:, k, ts_], rw[:, k, :], k == 0, k == 7, ["hxf", "rw"])
                                P.op("dve", lambda e: e.tensor_copy(out=lg[:], in_=ps[4][:, 0:8]), reads=[PSR[4]], writes=["lg"])
                                P.op("dve", lambda e: e.reduce_max(out=m1[:], in_=lg[:], axis=mybir.AxisListType.X), reads=["lg"], writes=["m1"])
                                P.op("dve", lambda e: e.tensor_scalar(out=mk1[:], in0=lg[:], scalar1=m1[:, 0:1], scalar2=None, op0=ALU.is_ge), reads=["lg", "m1"], writes=["mk1"])
                                P.op("dve", lambda e: e.scalar_tensor_tensor(out=l2[:], in0=mk1[:], scalar=-1e30, in1=lg[:], op0=ALU.mult, op1=ALU.add), reads=["mk1", "lg"], writes=["l2"])
                                P.op("dve", lambda e: e.reduce_max(out=m2[:], in_=l2[:], axis=mybir.AxisListType.X), reads=["l2"], writes=["m2"])
                                P.op("dve", lambda e: e.tensor_scalar(out=mk2[:], in0=l2[:], scalar1=m2[:, 0:1], scalar2=None, op0=ALU.is_ge), reads=["l2", "m2"], writes=["mk2"])
                                P.op("dve", lambda e: e.tensor_tensor(out=dd[:], in0=m2[:], in1=m1[:], op=ALU.subtract), reads=["m1", "m2"], writes=["dd"])
                                P.op("act", lambda e: e.activation(out=g2t[:], in_=dd[:], func=AF.Exp), reads=["dd"], writes=["g2t"])
                                P.op("dve", lambda e: e.tensor_scalar(out=g1t[:], in0=g2t[:], scalar1=1.0, scalar2=None, op0=ALU.add), reads=["g2t"], writes=["g1t"])
                                P.op("dve", lambda e: e.reciprocal(out=g1t[:], in_=g1t[:]), reads=["g1t"], writes=["g1t"])
                                P.op("dve", lambda e: e.tensor_tensor(out=g2t[:], in0=g2t[:], in1=g1t[:], op=ALU.mult), reads=["g2t", "g1t"], writes=["g2t"])
                                P.op("dve", lambda e: e.tensor_scalar(out=gt[:], in0=mk1[:], scalar1=g1t[:, 0:1], scalar2=None, op0=ALU.mult), reads=["mk1", "g1t"], writes=["gt"])
                                P.op("dve", lambda e: e.scalar_tensor_tensor(out=gt[:], in0=mk2[:], scalar=g2t[:, 0:1], in1=gt[:], op0=ALU.mult, op1=ALU.add), reads=["mk2", "g2t", "gt"], writes=["gt"])
                                ti = it * 32 + blk * 4 + t
                                P.op("pool", lambda e: e.tensor_copy(out=MK[:, ti, 0:8], in_=mk1[:]), reads=["mk1"], writes=["MK"])
                                P.op("pool", lambda e: e.tensor_copy(out=MK[:, ti, 8:16], in_=mk2[:]), reads=["mk2"], writes=["MK"])
                                P.op("pool", lambda e: e.tensor_copy(out=GV[:, ti, 0:1], in_=g1t[:]), reads=["g1t"], writes=["GV"])
                                P.op("pool", lambda e: e.tensor_copy(out=GV[:, ti, 1:2], in_=g2t[:]), reads=["g2t"], writes=["GV"])
                                hb_ = ti % 2
                                for c in range(8):
                                    bank = 5 + c // 4
                                    mm(bank, ps[bank][:, (c % 4) * 128:(c % 4 + 1) * 128], hx[:, c, ts_], ident_b[:], True, True, ["hx", "const"])
                                P.op("act", lambda e: e.copy(out=htok[hb_][:, 0:512], in_=ps[5][:, :]), reads=[PSR[5]], writes=["htok%d" % hb_])
                                P.op("dve", lambda e: e.tensor_copy(out=htok[hb_][:, 512:1024], in_=ps[6][:, :]), reads=[PSR[6]], writes=["htok%d" % hb_])
                                P.dma("pool", Htok[ti * 128:(ti + 1) * 128, :], htok[hb_][:], reads=["htok%d" % hb_], writes=["Htok%d" % ti])
                P.barrier()

        def cat_loader0(es):
            cat = sbt(es, [128, 8, 512], BF16)

            def load(it, t0, W):
                P.dma("sp", cat[:, :, :W], catTd[it].rearrange("c p t -> p c t")[:, :, t0:t0 + W], reads=["catTd%d" % it], writes=["cat"])
                return "cat"
            load.tile = cat
            return load

        if upto >= 4:
            holder = {}

            def cl0(es):
                f = cat_loader0(es)
                holder["cat"] = f.tile
                return f
            kparts0 = [((lambda wb, n, k=k: wb[:, k, n * 128:(n + 1) * 128]), (lambda W, k=k: holder["cat"][:, k, :W])) for k in range(8)]
            outproj_phase(0, items0, w_out0.rearrange("(k p) n -> p k n", p=128), kparts0, cl0, router=False)

        def ffn_phase(l, blocks, wg, wu, wd, E, gated, final):
            FG = 256
            NFG = DFF // FG
            with ExitStack() as es:
                NTmax = max(sum(s[2] for s in segs) for segs in blocks)
                hT = sbt(es, [128, 8, NTmax], BF16)
                acc = sbt(es, [128, 8, NTmax], F32)
                if gated:
                    gT = sbt(es, [8, NTmax], F32)
                    Gbc = sbt(es, [128, NTmax], F32)
                    selt = sbt(es, [8, 8, 128], F32)
                    P.dma("sp", selt[:], cst["sel"][:, :, :], writes=["selt"])
                wi = 0
                oi = 0
                assert not final and not gated
                esW = es
                stg = [sbt(esW, [128, 8, FG], F32) for _ in range(2)]
                stgd = [sbt(esW, [128, 2, D], F32) for _ in range(2)]
                wgb = [sbt(esW, [128, 8, FG], BF16) for _ in range(2)]
                wub = [sbt(esW, [128, 8, FG], BF16) for _ in range(2)]
                wdb = [sbt(esW, [128, 2, D], BF16) for _ in range(2)]
                sg = [sbt(esW, [128, 512], F32) for _ in range(2)]
                tu = [sbt(esW, [128, 512], F32) for _ in range(2)]
                hh = [sbt(esW, [128, 2, 512], BF16) for _ in range(2)]
                x1 = sbt(esW, [128, 8, 512], F32)
                x2 = sbt(esW, [128, 8, 512], F32)
                for segs in blocks:
                    NT = sum(s[2] for s in segs)
                    off = 0
                    for (it, t0, n) in segs:
                        rds = ["hTd%d_%d" % (it, b_) for b_ in range(t0 // min(512, LS[it]), (t0 + n + min(512, LS[it]) - 1) // min(512, LS[it]))]
                        P.dma("sp", hT[:, :, off:off + n], hTd[it].rearrange("c p t -> p c t")[:, :, t0:t0 + n], reads=rds, writes=["hT"])
                        if gated:
                            rdg = ["gTd%d_%d" % (it, b_) for b_ in range(t0 // 512, (t0 + n + 511) // 512)]
                            P.dma("sp", gT[:, off:off + n], gTd[it][:, t0:t0 + n], reads=rdg, writes=["gT"])
                        off += n
                    nsub = (NT + 511) // 512
                    first = True
                    steps = []
                    gu_done = [0]
                    dn_done = [0]

                    def emit_gu(st, si, NT=NT):
                        b, s_, fst = st
                        hb = si % 2
                        ss_ = slice(s_ * 512, min(NT, (s_ + 1) * 512))
                        wdt = ss_.stop - ss_.start
                        for j in range(2):
                            for k in range(8):
                                mm(0 + j, ps[0 + j][:, :wdt], wgb[b][:, k, j * 128:(j + 1) * 128], hT[:, k, ss_], k == 0, k == 7, ["wgb%d" % b, "hT"])
                            for k in range(8):
                                mm(2 + j, ps[2 + j][:, :wdt], wub[b][:, k, j * 128:(j + 1) * 128], hT[:, k, ss_], k == 0, k == 7, ["wub%d" % b, "hT"])
                            P.op("act", lambda e: e.activation(out=sg[j][:, :wdt], in_=ps[0 + j][:, :wdt], func=AF.Silu), reads=[PSR[0 + j]], writes=["sg%d" % j])
                            if gated:
                                P.op("dve", lambda e: e.tensor_tensor(out=tu[j][:, :wdt], in0=ps[2 + j][:, :wdt], in1=Gbc[:, ss_], op=ALU.mult), reads=[PSR[2 + j], "Gbc"], writes=["tu%d" % j])
                                P.op("pool", lambda e: e.tensor_tensor(out=hh[hb][:, j, :wdt], in0=sg[j][:, :wdt], in1=tu[j][:, :wdt], op=ALU.mult), reads=["sg%d" % j, "tu%d" % j], writes=["hh%d" % hb])
                            else:
                                P.op("dve", lambda e: e.tensor_tensor(out=hh[hb][:, j, :wdt], in0=ps[2 + j][:, :wdt], in1=sg[j][:, :wdt], op=ALU.mult), reads=[PSR[2 + j], "sg%d" % j], writes=["hh%d" % hb])

                    def emit_dn(st, si, NT=NT):
                        b, s_, fst = st
                        hb = si % 2
                        ss_ = slice(s_ * 512, min(NT, (s_ + 1) * 512))
                        wdt = ss_.stop - ss_.start
                        for n in range(8):
                            bank = 4 + (n % 2 if gated else n % 4)
                            for j in range(2):
                                mm(bank, ps[bank][:, :wdt], wdb[b][:, j, n * 128:(n + 1) * 128], hh[hb][:, j, :wdt], j == 0, j == 1, ["wdb%d" % b, "hh%d" % hb])
                            if fst:
                                P.op("dve", lambda e: e.tensor_copy(out=acc[:, n, ss_], in_=ps[bank][:, :wdt]), reads=[PSR[bank]], writes=["acc"])
                            else:
                                P.op("dve", lambda e: e.tensor_tensor(out=acc[:, n, ss_], in0=acc[:, n, ss_], in1=ps[bank][:, :wdt], op=ALU.add), reads=[PSR[bank], "acc"], writes=["acc"])
                    for e_ in range(E):
                        if gated:
                            for s in range(nsub):
                                ss_ = slice(s * 512, min(NT, (s + 1) * 512))
                                wdt = ss_.stop - ss_.start
                                mm(6, ps[6][:, :wdt], selt[:, e_, :], gT[:, ss_], True, True, ["selt", "gT"])
                                P.op("act", lambda e: e.copy(out=Gbc[:, ss_], in_=ps[6][:, :wdt]), reads=[PSR[6]], writes=["Gbc"])
                        wgv = wg[e_].rearrange("(k p) f -> p k f", p=128)
                        wuv = wu[e_].rearrange("(k p) f -> p k f", p=128)
                        for fg in range(NFG):
                            b = wi % 2
                            wi += 1
                            fs = slice(fg * FG, (fg + 1) * FG)
                            P.dma("sp", stg[0][:], wgv[:, :, fs], writes=["stg0"])
                            P.op("act", lambda e: e.copy(out=wgb[b][:], in_=stg[0][:]), reads=["stg0"], writes=["wgb%d" % b])
                            P.dma("sp", stg[1][:], wuv[:, :, fs], writes=["stg1"])
                            P.op("pool", lambda e: e.tensor_copy(out=wub[b][:], in_=stg[1][:]), reads=["stg1"], writes=["wub%d" % b])
                            P.dma("sp", stgd[0][:], wd[e_][fg * FG:(fg + 1) * FG, :].rearrange("(j p) n -> p j n", p=128), writes=["stgd0"])
                            P.op("act", lambda e: e.copy(out=wdb[b][:], in_=stgd[0][:]), reads=["stgd0"], writes=["wdb%d" % b])
                            for s_ in range(nsub):
                                steps.append((b, s_, first))
                            first = False
                            while gu_done[0] < len(steps):
                                emit_gu(steps[gu_done[0]], gu_done[0])
                                gu_done[0] += 1
                                if dn_done[0] < gu_done[0] - 1:
                                    emit_dn(steps[dn_done[0]], dn_done[0])
                                    dn_done[0] += 1
                    while dn_done[0] < len(steps):
                        emit_dn(steps[dn_done[0]], dn_done[0])
                        dn_done[0] += 1
                    off = 0
                    for (it, t0, n) in segs:
                        W = min(512, LS[it])
                        for q in range(n // W):
                            blk = (t0 + q * W) // W
                            tt = t0 + q * W
                            P.dma("sp", x1[:, :, :W], xTd[it].rearrange("c p t -> p c t")[:, :, tt:tt + W], reads=["xTd%d_%d" % (it, blk)], writes=["x1f"])
                            for c in range(8):
                                P.op("dve", lambda e: e.scalar_tensor_tensor(out=x2[:, c, :W], in0=acc[:, c, off + q * W:off + (q + 1) * W], scalar=GATE(l, 1, c, COL[it]), in1=x1[:, c, :W], op0=ALU.mult, op1=ALU.add),
                                     reads=["acc", "x1f", "mod"], writes=["x2"])
                            if not final:
                                P.dma("pool", xTd[it].rearrange("c p t -> p c t")[:, :, tt:tt + W], x2[:, :, :W], reads=["x2"], writes=["xTd%d_%d" % (it, blk)])
                            else:
                                ob = oi % 2
                                oi += 1
                                for t in range(W // 128):
                                    for c in range(8):
                                        bank = 6 + (c // 4) % 2
                                        P.op("pe", lambda e: e.transpose(out=ps[bank][:, (c % 4) * 128:(c % 4 + 1) * 128], in_=x2[:, c, t * 128:(t + 1) * 128], identity=ident_f[:]),
                                             reads=["x2", "const"], writes=[PSR[bank]])
                                        if c % 4 == 3:
                                            h0 = (c // 4) * 512
                                            if c // 4:
                                                P.op("act", lambda e: e.copy(out=xo[ob][:, t, h0:h0 + 512], in_=ps[bank][:, :]), reads=[PSR[bank]], writes=["xo%d" % ob])
                                            else:
                                                P.op("dve", lambda e: e.tensor_copy(out=xo[ob][:, t, h0:h0 + 512], in_=ps[bank][:, :]), reads=[PSR[bank]], writes=["xo%d" % ob])
                                P.dma("pool", out[it][tt:tt + W, :].rearrange("(t p) d -> p t d", p=128), xo[ob][:, 0:W // 128, :], reads=["xo%d" % ob], writes=["out"])
                        off += n
                P.barrier()


        def moe_sparse(l):
            AXX = mybir.AxisListType.X
            with ExitStack() as es:
                Mb = sbt(es, [128, 64, 8], BF16)
                utb = sbt(es, [128, 128], BF16)
                iota = sbt(es, [128, 1], F32)
                bvals = sbt(es, [128, NBLK], F32)
                cntt = sbt(es, [128, 8], F32)
                qq = sbt(es, [128, 8], F32)
                padded = sbt(es, [128, 8], F32)
                pend = sbt(es, [128, 8], F32)
                pstart = sbt(es, [128, 8], F32)
                be = sbt(es, [128, NBLK], F32)
                idxf = sbt(es, [128, NBLK, NFG], F32)
                base = sbt(es, [128, 64, 8], F32)
                slot = sbt(es, [128, 64, 8], F32)
                tsel = sbt(es, [128, 64, 8], F32)
                dsf = sbt(es, [128, 64, 2], F32)
                P.dma("sp", utb[:], cst["utb"][:, :], writes=["plc"])
                P.dma("sp", iota[:], cst["iota_p"][:, :], writes=["plc"])
                P.dma("sp", bvals[:], cst["bvals"][:, :], writes=["plc"])
                P.op("dve", lambda e: e.tensor_tensor(out=Mb[:], in0=MK[:, :, 0:8], in1=MK[:, :, 8:16], op=ALU.add), reads=["MK"], writes=["Mb"])
                for ti in range(64):
                    mm(0, ps[0][:, 0:8], ones_b[:], Mb[:, ti, :], ti == 0, ti == 63, ["Mb", "const"])
                for ti in range(64):
                    mm(1, ps[1][:, ti * 8:(ti + 1) * 8], ones_b[:], Mb[:, ti, :], True, True, ["Mb", "const"])
                for ti in range(64):
                    mm(2, ps[2][:, ti * 8:(ti + 1) * 8], utb[:], Mb[:, ti, :], True, True, ["Mb", "plc"])
                P.op("dve", lambda e: e.tensor_copy(out=cntt[:], in_=ps[0][:, 0:8]), reads=[PSR[0]], writes=["cntt"])
                P.op("dve", lambda e: e.tensor_scalar(out=qq[:], in0=cntt[:], scalar1=0.0, scalar2=None, op0=ALU.is_gt), reads=["cntt"], writes=["qq"])
                for m_ in range(1, 8):
                    P.op("dve", lambda e: e.scalar_tensor_tensor(out=qq[:], in0=cntt[:], scalar=float(m_ * SBLK), in1=qq[:], op0=ALU.is_gt, op1=ALU.add), reads=["cntt", "qq"], writes=["qq"])
                P.op("dve", lambda e: e.tensor_scalar(out=padded[:], in0=qq[:], scalar1=float(SBLK), scalar2=None, op0=ALU.mult), reads=["qq"], writes=["padded"])
                P.op("dve", lambda e: e.tensor_copy(out=pend[:, 0:1], in_=padded[:, 0:1]), reads=["padded"], writes=["pend"])
                for e_ in range(1, 8):
                    P.op("dve", lambda e: e.tensor_tensor(out=pend[:, e_:e_ + 1], in0=pend[:, e_ - 1:e_], in1=padded[:, e_:e_ + 1], op=ALU.add), reads=["pend", "padded"], writes=["pend"])
                P.op("dve", lambda e: e.tensor_tensor(out=pstart[:], in0=pend[:], in1=padded[:], op=ALU.subtract), reads=["pend", "padded"], writes=["pstart"])
                P.op("dve", lambda e: e.tensor_scalar(out=be[:], in0=bvals[:], scalar1=pend[:, 0:1], scalar2=None, op0=ALU.is_ge), reads=["plc", "pend"], writes=["be"])
                for e_ in range(1, 8):
                    P.op("dve", lambda e: e.scalar_tensor_tensor(out=be[:], in0=bvals[:], scalar=pend[:, e_:e_ + 1], in1=be[:], op0=ALU.is_ge, op1=ALU.add), reads=["plc", "pend", "be"], writes=["be"])
                P.op("dve", lambda e: e.tensor_scalar(out=be[:], in0=be[:], scalar1=7.0, scalar2=None, op0=ALU.min), reads=["be"], writes=["be"])
                for fg in range(NFG):
                    P.op("dve", lambda e: e.tensor_scalar(out=idxf[:, :, fg], in0=be[:], scalar1=float(NFG * 128), scalar2=float(fg * 128), op0=ALU.mult, op1=ALU.add), reads=["be"], writes=["idxf"])
                P.op("dve", lambda e: e.tensor_scalar(out=idxf[:], in0=idxf[:], scalar1=iota[:, 0:1], scalar2=None, op0=ALU.add), reads=["idxf", "plc"], writes=["idxf"])
                P.op("dve", lambda e: e.tensor_copy(out=idxw[:], in_=idxf[:].rearrange("p a b -> p (a b)")), reads=["idxf"], writes=["idxw"])
                P.op("dve", lambda e: e.tensor_copy(out=base[:, 0, :], in_=pstart[:]), reads=["pstart"], writes=["base"])
                for ti in range(1, 64):
                    P.op("dve", lambda e: e.tensor_tensor(out=base[:, ti, :], in0=base[:, ti - 1, :], in1=ps[1][:, (ti - 1) * 8:ti * 8], op=ALU.add), reads=["base", PSR[1]], writes=["base"])
                P.op("dve", lambda e: e.tensor_tensor(out=slot[:], in0=base[:], in1=ps[2][:, :].rearrange("p (a b) -> p a b", b=8), op=ALU.add), reads=["base", PSR[2]], writes=["slot"])
                for k_ in range(2):
                    P.op("dve", lambda e: e.tensor_tensor(out=tsel[:], in0=slot[:], in1=MK[:, :, k_ * 8:(k_ + 1) * 8], op=ALU.mult), reads=["slot", "MK"], writes=["tsel"])
                    P.op("dve", lambda e: e.reduce_sum(out=dsf[:, :, k_], in_=tsel[:], axis=AXX), reads=["tsel"], writes=["dsf"])
                P.op("dve", lambda e: e.tensor_scalar(out=dsf[:], in0=dsf[:], scalar1=float(NSLOT - 1), scalar2=None, op0=ALU.min), reads=["dsf"], writes=["dsf"])
                P.op("dve", lambda e: e.tensor_copy(out=dsl[:], in_=dsf[:].rearrange("p a b -> p (a b)")), reads=["dsf"], writes=["dsl"])
                if "dbg_plan" in debug:
                    P.dma("sp", dbg_plan[:, 0:128], dsf[:].rearrange("p a b -> p (a b)"), reads=["dsf"], writes=["dbgp"])
                    P.dma("sp", dbg_plan[:, 128:128 + NBLK], be[:], reads=["be"], writes=["dbgp"])
                    P.dma("sp", dbg_plan[:, 160:168], cntt[:], reads=["cntt"], writes=["dbgp"])
                P.barrier()
            with ExitStack() as es:
                ht = [sbt(es, [128, D], BF16) for _ in range(4)]
                for ti in range(64):
                    b = ti % 4
                    P.dma("sp", ht[b][:], Htok[ti * 128:(ti + 1) * 128, :], reads=["Htok%d" % ti], writes=["ht%d" % b])
                    for k_ in range(2):
                        P.idma(Xs[:, :], ht[b][:], dsl[:, 2 * ti + k_:2 * ti + k_ + 1], None, NSLOT - 1, reads=["ht%d" % b, "dsl"], writes=["Xs"])
                P.barrier()
            with ExitStack() as es:
                xtok = sbt(es, [128, 8, D], BF16)
                hT = sbt(es, [128, 8, SBLK], BF16)
                acc = sbt(es, [128, 8, SBLK], F32)
                ybs = [sbt(es, [128, 8, 128], BF16) for _ in range(2)]
                ytok = [sbt(es, [128, D], BF16) for _ in range(2)]
                stg = [[sbt(es, [128, 2048], F32) for _ in range(2)] for _ in range(3)]
                wgb = [sbt(es, [128, 8, 256], BF16) for _ in range(2)]
                wub = [sbt(es, [128, 8, 256], BF16) for _ in range(2)]
                wdb = [sbt(es, [128, 2, D], BF16) for _ in range(2)]
                sg = [sbt(es, [128, 512], F32) for _ in range(2)]
                hh = [sbt(es, [128, 2, 512], BF16) for _ in range(2)]
                wi = 0
                yi = 0
                for blk in range(NBLK):
                    P.dma("sp", xtok[:], Xs[blk * SBLK:(blk + 1) * SBLK, :].rearrange("(t p) d -> p t d", p=128), reads=["Xs"], writes=["xtok"])
                    for c in range(8):
                        for t4 in range(2):
                            bank = 6 + (c * 2 + t4) % 2
                            for q in range(4):
                                t = t4 * 4 + q
                                mm(bank, ps[bank][:, q * 128:(q + 1) * 128], xtok[:, t, c * 128:(c + 1) * 128], ident_b[:], True, True, ["xtok", "const"])
                            if (c * 2 + t4) % 2:
                                P.op("act", lambda e: e.copy(out=hT[:, c, t4 * 512:(t4 + 1) * 512], in_=ps[bank][:, :]), reads=[PSR[bank]], writes=["hT"])
                            else:
                                P.op("dve", lambda e: e.tensor_copy(out=hT[:, c, t4 * 512:(t4 + 1) * 512], in_=ps[bank][:, :]), reads=[PSR[bank]], writes=["hT"])
                    steps = []
                    gu_done = 0
                    dn_done = 0

                    def emit_gu(st, si):
                        b, s_, fst = st
                        hb = si % 2
                        ss_ = slice(s_ * 512, (s_ + 1) * 512)
                        for j in range(2):
                            for k in range(8):
                                mm(0 + j, ps[0 + j][:, :], wgb[b][:, k, j * 128:(j + 1) * 128], hT[:, k, ss_], k == 0, k == 7, ["wgb%d" % b, "hT"])
                            for k in range(8):
                                mm(2 + j, ps[2 + j][:, :], wub[b][:, k, j * 128:(j + 1) * 128], hT[:, k, ss_], k == 0, k == 7, ["wub%d" % b, "hT"])
                            P.op("act", lambda e: e.activation(out=sg[j][:], in_=ps[0 + j][:, :], func=AF.Silu), reads=[PSR[0 + j]], writes=["sg%d" % j])
                            P.op("dve", lambda e: e.tensor_tensor(out=hh[hb][:, j, :], in0=ps[2 + j][:, :], in1=sg[j][:], op=ALU.mult), reads=[PSR[2 + j], "sg%d" % j], writes=["hh%d" % hb])

                    def emit_dn(st, si):
                        b, s_, fst = st
                        hb = si % 2
                        ss_ = slice(s_ * 512, (s_ + 1) * 512)
                        for n in range(8):
                            bank = 4 + n % 4
                            for j in range(2):
                                mm(bank, ps[bank][:, :], wdb[b][:, j, n * 128:(n + 1) * 128], hh[hb][:, j, :], j == 0, j == 1, ["wdb%d" % b, "hh%d" % hb])
                            if fst:
                                P.op("dve", lambda e: e.tensor_copy(out=acc[:, n, ss_], in_=ps[bank][:, :]), reads=[PSR[bank]], writes=["acc"])
                            else:
                                P.op("dve", lambda e: e.tensor_tensor(out=acc[:, n, ss_], in0=acc[:, n, ss_], in1=ps[bank][:, :], op=ALU.add), reads=[PSR[bank], "acc"], writes=["acc"])

                    def gather_w(blk_, fg_, b_):
                        ic = blk_ * NFG + fg_
                        for wsrc, wk in ((moe_wg, 0), (moe_wu, 1), (moe_wd, 2)):
                            P.idma(stg[wk][b_][:], wsrc[:, :], None, idxw[:, ic:ic + 1], NE * NFG * 128 - 1, reads=["idxw"], writes=["stg%d_%d" % (wk, b_)])

                    if blk == 0:
                        gather_w(0, 0, wi % 2)
                    for fg in range(NFG):
                        b = wi % 2
                        wi += 1
                        if fg + 1 < NFG:
                            gather_w(blk, fg + 1, wi % 2)
                        elif blk + 1 < NBLK:
                            gather_w(blk + 1, 0, wi % 2)
                        P.op("act", lambda e: e.copy(out=wgb[b][:].rearrange("p k f -> p (k f)"), in_=stg[0][b][:]), reads=["stg0_%d" % b], writes=["wgb%d" % b])
                        P.op("act", lambda e: e.copy(out=wub[b][:].rearrange("p k f -> p (k f)"), in_=stg[1][b][:]), reads=["stg1_%d" % b], writes=["wub%d" % b])
                        P.op("act", lambda e: e.copy(out=wdb[b][:].rearrange("p k f -> p (k f)"), in_=stg[2][b][:]), reads=["stg2_%d" % b], writes=["wdb%d" % b])
                        for s_ in range(SBLK // 512):
                            steps.append((b, s_, fg == 0))
                        while gu_done < len(steps):
                            emit_gu(steps[gu_done], gu_done)
                            gu_done += 1
                            if dn_done < gu_done - 1:
                                emit_dn(steps[dn_done], dn_done)
                                dn_done += 1
                    while dn_done < len(steps):
                        emit_dn(steps[dn_done], dn_done)
                        dn_done += 1
                    for t in range(8):
                        yb_ = yi % 2
                        yi += 1
                        yb = ybs[yb_]
                        if yb_:
                            P.op("pool", lambda e: e.tensor_copy(out=yb[:], in_=acc[:, :, t * 128:(t + 1) * 128]), reads=["acc"], writes=["yb%d" % yb_])
                        else:
                            P.op("act", lambda e: e.copy(out=yb[:], in_=acc[:, :, t * 128:(t + 1) * 128]), reads=["acc"], writes=["yb%d" % yb_])
                        for c in range(8):
                            bank = 6 + c // 4
                            mm(bank, ps[bank][:, (c % 4) * 128:(c % 4 + 1) * 128], yb[:, c, :], ident_b[:], True, True, ["yb%d" % yb_, "const"])
                        P.op("act", lambda e: e.copy(out=ytok[yb_][:, 0:512], in_=ps[6][:, :]), reads=[PSR[6]], writes=["ytok%d" % yb_])
                        P.op("dve", lambda e: e.tensor_copy(out=ytok[yb_][:, 512:1024], in_=ps[7][:, :]), reads=[PSR[7]], writes=["ytok%d" % yb_])
                        r0 = blk * SBLK + t * 128
                        P.dma("sp", Ys[r0:r0 + 128, :], ytok[yb_][:], reads=["ytok%d" % yb_], writes=["Ys"])
                P.barrier()
            with ExitStack() as es:
                g2rep = sbt(es, [128, 128], F32)
                g2bc = [sbt(es, [128, D], F32) for _ in range(2)]
                o1 = [sbt(es, [128, D], BF16) for _ in range(4)]
                o2 = [sbt(es, [128, D], BF16) for _ in range(4)]
                yf = [sbt(es, [128, D], F32) for _ in range(4)]
                x1 = [sbt(es, [128, 8, 128], F32) for _ in range(4)]
                ot = [sbt(es, [128, D], F32) for _ in range(4)]
                for it in range(2):
                    for c in range(8):
                        P.op("dve", lambda e: e.tensor_scalar(out=g2rep[:], in0=ones_f[:], scalar1=GATE(l, 1, c, COL[it]), scalar2=None, op0=ALU.mult), reads=["const", "mod"], writes=["g2rep"])
                        bank = c // 4
                        P.op("pe", lambda e: e.transpose(out=ps[bank][:, (c % 4) * 128:(c % 4 + 1) * 128], in_=g2rep[:], identity=ident_f[:]), reads=["g2rep", "const"], writes=[PSR[bank]])
                        if c % 4 == 3:
                            P.op("act", lambda e: e.copy(out=g2bc[it][:, bank * 512:(bank + 1) * 512], in_=ps[bank][:, :]), reads=[PSR[bank]], writes=["g2bc"])
                for ti in range(64):
                    it = ti // 32
                    tt = (ti % 32) * 128
                    b = ti % 4
                    P.idma(o1[b][:], Ys[:, :], None, dsl[:, 2 * ti:2 * ti + 1], NSLOT - 1, reads=["Ys", "dsl"], writes=["o1_%d" % b])
                    P.idma(o2[b][:], Ys[:, :], None, dsl[:, 2 * ti + 1:2 * ti + 2], NSLOT - 1, reads=["Ys", "dsl"], writes=["o2_%d" % b])
                    P.dma("sp", x1[b][:], xTd[it].rearrange("c p t -> p c t")[:, :, tt:tt + 128], reads=["xTd%d_%d" % (it, tt // 512)], writes=["x1_%d" % b])
                    P.op("dve", lambda e: e.tensor_scalar(out=yf[b][:], in0=o1[b][:], scalar1=GV[:, ti, 0:1], scalar2=None, op0=ALU.mult), reads=["o1_%d" % b, "GV"], writes=["yf%d" % b])
                    P.op("dve", lambda e: e.scalar_tensor_tensor(out=yf[b][:], in0=o2[b][:], scalar=GV[:, ti, 1:2], in1=yf[b][:], op0=ALU.mult, op1=ALU.add), reads=["o2_%d" % b, "GV", "yf%d" % b], writes=["yf%d" % b])
                    P.op("dve", lambda e: e.tensor_tensor(out=yf[b][:], in0=yf[b][:], in1=g2bc[it][:], op=ALU.mult), reads=["yf%d" % b, "g2bc"], writes=["yf%d" % b])
                    for c in range(8):
                        bank = 2 * b + c // 4
                        P.op("pe", lambda e: e.transpose(out=ps[bank][:, (c % 4) * 128:(c % 4 + 1) * 128], in_=x1[b][:, c, :], identity=ident_f[:]), reads=["x1_%d" % b, "const"], writes=[PSR[bank]])
                    for h_ in range(2):
                        bank = 2 * b + h_
                        P.op("dve", lambda e: e.tensor_tensor(out=ot[b][:, h_ * 512:(h_ + 1) * 512], in0=ps[bank][:, :], in1=yf[b][:, h_ * 512:(h_ + 1) * 512], op=ALU.add), reads=[PSR[bank], "yf%d" % b], writes=["ot%d" % b])
                    P.dma("sp", out[it][tt:tt + 128, :], ot[b][:], reads=["ot%d" % b], writes=["out"])
                P.barrier()

        if upto >= 5:
            blocks0 = [[(0, 0, 2048)], [(0, 2048, 2048)], [(1, 0, 2048)], [(1, 2048, 2048)], [(2, 0, 256), (3, 0, 256)]]
            ffn_phase(0, blocks0, ffn_wg, ffn_wu, ffn_wd, 1, False, False)


        if upto >= 6:
            with ExitStack() as es:
                xts = [sbt(es, [128, 8, 512], F32) for _ in range(2)]
                sq = sbt(es, [128, 8, 512], BF16)
                rs = sbt(es, [128, 512], F32)
                tmp = sbt(es, [128, 8, 512], F32)
                hxs = [sbt(es, [128, 8, 512], BF16) for _ in range(2)]
                hi = 0
                for it in items0:
                    L = LS[it]
                    W = min(512, L)
                    for blk in range(L // W):
                        t0 = blk * W
                        b = hi % 2
                        hi += 1
                        P.dma("sp", xts[b][:, :, :W], xTd[it].rearrange("c p t -> p c t")[:, :, t0:t0 + W], reads=["xTd%d_%d" % (it, blk)], writes=["xt%d" % b])
                        norm_block(es, xts[b], "xt%d" % b, W, 1, 0, COL[it], hxs[b], "hx%d" % b, (sq, rs, tmp))
                        P.dma("pool", hTd[it].rearrange("c p t -> p c t")[:, :, t0:t0 + W], hxs[b][:, :, :W], reads=["hx%d" % b], writes=["hTd%d_%d" % (it, blk)])
                P.barrier()
            esQ = ExitStack()
            wq_b = load_cast_w(esQ, w_qkv.rearrange("(k p) n -> p k n", p=128), 8, 1536, "wq_b")
            for it in (0, 1):
                ci_ = it + 2
                L = SEQ
                nK = L // 128
                with ExitStack() as esI:
                    hxT = sbt(esI, [128, 8, L], BF16)
                    hcT = sbt(esI, [128, 8, CTX], BF16)
                    cosT = sbt(esI, [64, L], BF16)
                    sinT = sbt(esI, [64, L], BF16)
                    RT = sbt(esI, [64, 128], BF16)
                    qkgs = sbt(esI, [64, 2], F32)
                    esink = sbt(esI, [64, 16], F32)
                    mkp = sbt(esI, [128, 512], BF16)
                    mkn = sbt(esI, [128, 512], BF16)
                    mstage = sbt(esI, [128, 512], F32)
                    kT = sbt(esI, [64, L + CTX], BF16)
                    Vt = sbt(esI, [128, nK + 2, 128], BF16)
                    qT = sbt(esI, [64, 4, L], BF16)
                    ET = [[sbt(esI, [128, 512], BF16) for _ in range(5)] for _ in range(2)]
                    dns = [sbt(esI, [64, 512], F32) for _ in range(2)]
                    oT = [sbt(esI, [64, 512], BF16) for _ in range(2)]
                    P.dma("sp", hxT[:], hTd[it].rearrange("c p t -> p c t"), reads=["hTd%d_%d" % (it, b_) for b_ in range(8)], writes=["hxT"])
                    P.dma("sp", hcT[:], hTd[ci_].rearrange("c p t -> p c t"), reads=["hTd%d_0" % ci_], writes=["hcT"])
                    P.dma("sp", cosT[:], cst["rope_cos"][:, :], writes=["ropec"])
                    P.dma("sp", sinT[:], cst["rope_sin"][:, :], writes=["ropec"])
                    P.dma("sp", RT[:], cst["ropeRT"][:, :], writes=["ropec"])
                    P.dma("sp", qkgs[:], qkg[:, :], writes=["ropec"])
                    P.dma("sp", esink[:], sinkr[:, :], writes=["esink"])
                    P.op("act", lambda e: e.activation(out=esink[:], in_=esink[:], func=AF.Exp), reads=["esink"], writes=["esink"])
                    P.dma("sp", mstage[:], cst["maskp"].rearrange("p a b -> p (a b)"), writes=["mstage"])
                    P.op("dve", lambda e: e.tensor_copy(out=mkp[:], in_=mstage[:]), reads=["mstage"], writes=["mkp"])
                    P.dma("sp", mstage[:], cst["maskn"].rearrange("p a b -> p (a b)"), reads=[], writes=["mstage"])
                    P.op("dve", lambda e: e.tensor_copy(out=mkn[:], in_=mstage[:]), reads=["mstage"], writes=["mkn"])

                    sqqs = [sbt(esI, [64, 512], BF16) for _ in range(3)]
                    rsqs = [sbt(esI, [64, 512], F32) for _ in range(2)]
                    qns = [sbt(esI, [64, 512], BF16) for _ in range(2)]
                    r1s = [sbt(esI, [64, 512], F32) for _ in range(2)]
                    r2s = [sbt(esI, [64, 512], F32) for _ in range(2)]

                    def stA(u, ui):
                        (src, rsrc, c0, W, col0, gcol, rope, dest, rdest, vinfo) = u
                        pa = ui % 3
                        for k in range(8):
                            mm(pa, ps[pa][:, :W], wq_b[:, k, col0:col0 + 128], src[:, k, c0:c0 + W], k == 0, k == 7, ["wq_b", rsrc])
                        P.op("act", lambda e: e.activation(out=sqqs[pa][:, :W], in_=ps[pa][0:64, :W], func=AF.Square), reads=[PSR[pa]], writes=["sqq%d" % pa])
                        if vinfo is not None:
                            g_, vch0 = vinfo
                            nt = W // 128
                            for t in range(nt):
                                for k in range(8):
                                    mm(7, ps[7][:, t * 64:(t + 1) * 64], src[:, k, c0 + t * 128:c0 + (t + 1) * 128], wq_b[:, k, 1280 + g_ * 64:1280 + (g_ + 1) * 64], k == 0, k == 7, ["wq_b", rsrc])
                            P.op("act", lambda e: e.copy(out=Vt[:, vch0:vch0 + nt, 0:64], in_=ps[7][:, 0:nt * 64].rearrange("p (a b) -> p a b", b=64)), reads=[PSR[7]], writes=["Vt"])
                            P.op("dve", lambda e: e.tensor_copy(out=Vt[:, vch0:vch0 + nt, 64:128], in_=ps[7][:, 0:nt * 64].rearrange("p (a b) -> p a b", b=64)), reads=[PSR[7]], writes=["Vt"])

                    def stB(u, ui):
                        (src, rsrc, c0, W, col0, gcol, rope, dest, rdest, vinfo) = u
                        pa = ui % 3
                        p_ = ui % 2
                        bs = 3 + p_
                        mm(bs, ps[bs][:, :W], ones_b[0:64, :], sqqs[pa][:, :W], True, True, ["sqq%d" % pa, "const"])
                        P.op("act", lambda e: e.activation(out=rsqs[p_][:, :W], in_=ps[bs][0:64, :W], func=AF.Ln, bias=epst[0:64, 0:1], scale=1.0 / 64), reads=[PSR[bs], "const"], writes=["rsq%d" % p_])
                        P.op("act", lambda e: e.activation(out=rsqs[p_][:, :W], in_=rsqs[p_][:, :W], func=AF.Exp, scale=-0.5), reads=["rsq%d" % p_], writes=["rsq%d" % p_])
                        P.op("dve", lambda e: e.scalar_tensor_tensor(out=qns[p_][:, :W], in0=ps[pa][0:64, :W], scalar=qkgs[:, gcol:gcol + 1], in1=rsqs[p_][:, :W], op0=ALU.mult, op1=ALU.mult),
                             reads=[PSR[pa], "rsq%d" % p_, "ropec"], writes=["qn%d" % p_])

                    def stC(u, ui):
                        (src, rsrc, c0, W, col0, gcol, rope, dest, rdest, vinfo) = u
                        p_ = ui % 2
                        br = 5 + p_
                        if rope:
                            mm(br, ps[br][:, :W], RT[:], qns[p_][:, :W], True, True, ["qn%d" % p_, "ropec"])
                            P.op("pool", lambda e: e.tensor_tensor(out=r1s[p_][:, :W], in0=qns[p_][:, :W], in1=cosT[:, c0:c0 + W], op=ALU.mult), reads=["qn%d" % p_, "ropec"], writes=["r1%d" % p_])
                            P.op("dve", lambda e: e.tensor_tensor(out=r2s[p_][:, :W], in0=ps[br][0:64, :W], in1=sinT[:, c0:c0 + W], op=ALU.mult), reads=[PSR[br], "ropec"], writes=["r2%d" % p_])
                            P.op("pool", lambda e: e.tensor_tensor(out=dest, in0=r1s[p_][:, :W], in1=r2s[p_][:, :W], op=ALU.add), reads=["r1%d" % p_, "r2%d" % p_], writes=[rdest])
                        else:
                            P.op("pool", lambda e: e.tensor_copy(out=dest, in_=qns[p_][:, :W]), reads=["qn%d" % p_], writes=[rdest])

                    for g in range(4):
                        units = []
                        for b_ in range(L // 512):
                            c0 = b_ * 512
                            units.append((hxT, "hxT", c0, 512, 1024 + g * 64, 1, True, kT[:, c0:c0 + 512], "kT", (g, c0 // 128)))
                            for j in range(4):
                                units.append((hxT, "hxT", c0, 512, (4 * g + j) * 64, 0, True, qT[:, j, c0:c0 + 512], "qT", None))
                        units.append((hcT, "hcT", 0, CTX, 1024 + g * 64, 1, False, kT[:, L:L + CTX], "kT", (g, L // 128)))
                        nu = len(units)
                        for t_ in range(nu + 2):
                            if t_ < nu:
                                stA(units[t_], t_)
                            if 0 <= t_ - 1 < nu:
                                stB(units[t_ - 1], t_ - 1)
                            if 0 <= t_ - 2 < nu:
                                stC(units[t_ - 2], t_ - 2)

                        def chunks_of(i):
                            ch = []
                            if i > 0:
                                ch.append((i - 1, mkp, "mkp"))
                            ch.append((i, None, None))
                            if i < nK - 1:
                                ch.append((i + 1, mkn, "mkn"))
                            ch.append((nK, None, None))
                            ch.append((nK + 1, None, None))
                            return ch

                        def stS(i):
                            eb = i % 2
                            for ci, (kc, mk, rmk) in enumerate(chunks_of(i)):
                                sb_ = ci % 4
                                mm(sb_, ps[sb_][:, :].rearrange("p (a b) -> p a b", b=128), kT[:, kc * 128:(kc + 1) * 128], qT[:, :, i * 128:(i + 1) * 128], True, True, ["kT", "qT"])
                                P.op("act", lambda e: e.activation(out=ET[eb][ci][:], in_=ps[sb_][:, :], func=AF.Exp, scale=0.125), reads=[PSR[sb_]], writes=["ET%d_%d" % (eb, ci)])
                                if mk is not None:
                                    P.op("pool", lambda e: e.tensor_tensor(out=ET[eb][ci][:], in0=ET[eb][ci][:], in1=mk[:], op=ALU.mult), reads=["ET%d_%d" % (eb, ci), rmk], writes=["ET%d_%d" % (eb, ci)])

                        def stR(i):
                            eb = i % 2
                            chunks = chunks_of(i)
                            nch = len(chunks)
                            bo, bd = (4, 5) if eb == 0 else (6, 7)
                            dn = dns[eb]
                            for ci, (kc, mk, rmk) in enumerate(chunks):
                                mm(bo, ps[bo][:, :], Vt[:, kc, :], ET[eb][ci][:], ci == 0, ci == nch - 1, ["Vt", "ET%d_%d" % (eb, ci)])
                            for ci, (kc, mk, rmk) in enumerate(chunks):
                                mm(bd, ps[bd][:, :], ones_b[:, :], ET[eb][ci][:], ci == 0, ci == nch - 1, ["const", "ET%d_%d" % (eb, ci)])
                            for j in range(4):
                                h = 4 * g + j
                                P.op("act", lambda e: e.activation(out=dn[:, j * 128:(j + 1) * 128], in_=ps[bd][0:64, j * 128:(j + 1) * 128], func=AF.Ln, bias=esink[:, h:h + 1], scale=1.0),
                                     reads=[PSR[bd], "esink"], writes=["dn%d" % eb])
                            P.op("act", lambda e: e.activation(out=dn[:], in_=dn[:], func=AF.Exp, scale=-1.0), reads=["dn%d" % eb], writes=["dn%d" % eb])
                            P.op("dve", lambda e: e.tensor_tensor(out=oT[eb][:], in0=ps[bo][0:64, :], in1=dn[:], op=ALU.mult), reads=[PSR[bo], "dn%d" % eb], writes=["oT%d" % eb])
                            P.dma("pool", attTd[it].rearrange("h p t -> p h t")[:, 4 * g:4 * g + 4, i * 128:(i + 1) * 128], oT[eb][:].rearrange("p (a b) -> p a b", b=128),
                                  reads=["oT%d" % eb], writes=["attTd%d" % it])

                        stS(0)
                        for i in range(nK):
                            if i + 1 < nK:
                                stS(i + 1)
                            stR(i)
                    P.barrier()
            esQ.close()
            holder1 = {}

            def cl1(es):
                att = sbt(es, [64, 16, 512], BF16)
                holder1["att"] = att

                def load(it, t0, W):
                    P.dma("sp", att[:, :, :W], attTd[it].rearrange("h p t -> p h t")[:, :, t0:t0 + W], reads=["attTd%d" % it], writes=["att"])
                    return "att"
                return load
            kparts1 = [((lambda wb, n, h=h: wb[0:64, h, n * 128:(n + 1) * 128]), (lambda W, h=h: holder1["att"][:, h, :W])) for h in range(16)]
            outproj_phase(1, [0, 1], w_out1.rearrange("(h p) n -> p h n", p=64), kparts1, cl1, router=True, npart=64)
            if upto >= 7:
                moe_sparse(1)

        P.barrier()
        print("instructions:", P.ninst, flush=True)
    return nc


def _core_inputs(inp, core):
    b0 = 2 * core
    f32 = np.float32
    m = {}
    m["x"] = np.ascontiguousarray(inp["x"][b0:b0 + 2])
    m["ctx"] = np.ascontiguousarray(inp["ctx"][b0:b0 + 2])
    cc = np.stack([inp["c"][b0], inp["c"][b0 + 1], inp["c_ctx"]], axis=-1)
    m["cT"] = np.ascontiguousarray(cc.reshape(8, 128, 3).transpose(1, 0, 2)).astype(f32)
    m["ada_w"] = inp["ada_w"]
    m["ada_bT"] = np.ascontiguousarray(inp["ada_b"].reshape(2, 48, 128).transpose(2, 0, 1))
    m["n1g"] = np.ascontiguousarray(inp["norm1_g"].reshape(2, 8, 128).transpose(2, 0, 1))
    m["n2g"] = np.ascontiguousarray(inp["norm2_g"].reshape(2, 8, 128).transpose(2, 0, 1))
    m["ev_w_in"] = inp["ev_w_in"][0]
    m["ev_w_out"] = inp["ev_w_out"][0]
    m["cw"] = np.ascontiguousarray(inp["hy_conv_w"][0].reshape(3, 12, 128).transpose(2, 1, 0))
    m["cb"] = np.ascontiguousarray(inp["hy_conv_b"][0].reshape(12, 128).T)
    m["hyb"] = np.ascontiguousarray(inp["hy_bias"][0].reshape(4, 128).T)
    m["hf_w0"] = inp["hf_w0"][0]
    m["hf_w1"] = inp["hf_w1"][0]
    m["hf_w2"] = inp["hf_w2"][0]
    m["hf_w3"] = inp["hf_w3"][0]
    m["hf_vec"] = np.ascontiguousarray(np.stack([inp["hf_b0"][0], inp["hf_b1"][0], inp["hf_b2"][0], inp["hf_freq"][0]], axis=-1))
    m["ffn_w_gate"] = inp["ffn_w_gate"]
    m["ffn_w_up"] = inp["ffn_w_up"]
    m["ffn_w_down"] = inp["ffn_w_down"]
    m["od_w_qkv"] = inp["od_w_qkv"][0]
    m["od_w_out"] = inp["od_w_out"][0]
    m["qkg"] = np.ascontiguousarray(np.stack([inp["q_norm_g"][0], inp["k_norm_g"][0]], axis=-1))
    m["sinkr"] = np.ascontiguousarray(np.broadcast_to(inp["attn_sink"][0][None, :], (64, 16)))
    m["routerT"] = np.ascontiguousarray(inp["moe_router"][0].reshape(8, 128, 8).transpose(1, 0, 2))
    m["moe_w_gate"] = inp["_moe_wg_t"]
    m["moe_w_up"] = inp["_moe_wu_t"]
    m["moe_w_down"] = inp["_moe_wd_t"]
    for k, v in _CONSTS.items():
        m["k_" + k] = v
    return {k: np.ascontiguousarray(v) for k, v in m.items()}


def kernel(**inputs):
    global _CONSTS
    if _CONSTS is None:
        _CONSTS = host_consts()
    inp = {k: np.asarray(v) for k, v in inputs.items()}
    inp["_moe_wg_t"] = np.ascontiguousarray(inp["moe_w_gate"][0].reshape(NE, 8, 128, NFG, 256).transpose(0, 3, 2, 1, 4)).reshape(NE * NFG * 128, 2048)
    inp["_moe_wu_t"] = np.ascontiguousarray(inp["moe_w_up"][0].reshape(NE, 8, 128, NFG, 256).transpose(0, 3, 2, 1, 4)).reshape(NE * NFG * 128, 2048)
    inp["_moe_wd_t"] = np.ascontiguousarray(inp["moe_w_down"][0].reshape(NE, NFG, 2, 128, D).transpose(0, 1, 3, 2, 4)).reshape(NE * NFG * 128, 2048)
    nc = build()
    in_maps = [_core_inputs(inp, c) for c in range(8)]
    res = run_bass_kernel_spmd(nc, in_maps, core_ids=list(range(8)))
    return np.concatenate([r["out"] for r in res.results], axis=0).astype(np.float32)
```

```python
import math
import numpy as np
from contextlib import ExitStack
import ml_dtypes
import concourse.bass as bass
import concourse.mybir as mybir
from concourse.bass_utils import run_bass_kernel_spmd

F32 = mybir.dt.float32
BF16 = mybir.dt.bfloat16
AF = mybir.ActivationFunctionType
ALU = mybir.AluOpType
NPBF = ml_dtypes.bfloat16

D = 1024
SEQ = 4096
CTX = 256
DFF = 3584
NE = 8
EPS = 1e-6
PI = math.pi
SBLK = 1024
NBLK = (2 * 2 * SEQ) // SBLK + NE - 1
NSLOT = NBLK * SBLK
NFG = 14
SPARSE = True


class Prog:
    COMPUTE = ("pe", "dve", "act", "pool")

    def __init__(self, nc, es, kq=8):
        self.nc = nc
        self.engs = {"pe": nc.tensor, "dve": nc.vector, "act": nc.scalar, "pool": nc.gpsimd, "sp": nc.sync}
        self.sem = {}
        self.cnt = {}
        for e in self.COMPUTE:
            self.sem[e] = es.enter_context(nc.semaphore("s_" + e))
            self.cnt[e] = 0
        self.kq = kq
        self.dsem = {}
        self.dn = {}
        for q in ("sp", "pool", "act"):
            self.dsem[q] = [es.enter_context(nc.semaphore("d_%s%d" % (q, i))) for i in range(kq)]
            self.dn[q] = 0
        self.known = {e: {} for e in self.engs}
        self.res = {}
        self.ninst = 0

    def _deps(self, reads, writes):
        ev = {}
        for r in reads:
            st = self.res.get(r)
            if st:
                for k, v in st["w"].items():
                    if ev.get(k, 0) < v:
                        ev[k] = v
        for w in writes:
            st = self.res.get(w)
            if st:
                for k, v in st["w"].items():
                    if ev.get(k, 0) < v:
                        ev[k] = v
                for k, v in st["r"].items():
                    if ev.get(k, 0) < v:
                        ev[k] = v
        return ev

    def _semobj(self, k):
        return self.sem[k[1]] if k[0] == "c" else self.dsem[k[1]][k[2]]

    def _wait(self, e, ev, skip_key=None):
        eng = self.engs[e]
        kn = self.known[e]
        for k, v in ev.items():
            if k == skip_key or kn.get(k, 0) >= v:
                continue
            eng.wait_ge(self._semobj(k), v)
            kn[k] = v

    def _commit(self, key, val, reads, writes):
        for r in reads:
            st = self.res.setdefault(r, {"w": {}, "r": {}})
            st["r"][key] = val
        for w in writes:
            st = self.res.setdefault(w, {"w": {}, "r": {}})
            st["w"][key] = val

    def op(self, e, fn, reads=(), writes=()):
        ev = self._deps(reads, writes)
        key = ("c", e)
        self._wait(e, ev, skip_key=key if e == "pe" else None)
        ins = fn(self.engs[e])
        self.cnt[e] += 1
        ins.then_inc(self.sem[e], 1)
        self._commit(key, self.cnt[e], reads, writes)
        self.ninst += 1

    def dma(self, q, out, in_, reads=(), writes=(), **kw):
        ev = self._deps(reads, writes)
        n = self.dn[q]
        j = n % self.kq
        val = 16 * (n // self.kq + 1)
        key = ("d", q, j)
        if val > 16 and ev.get(key, 0) < val - 16:
            ev[key] = val - 16
        self._wait(q, ev)
        ins = self.engs[q].dma_start(out=out, in_=in_, **kw)
        ins.then_inc(self.dsem[q][j], 16)
        self.dn[q] = n + 1
        self._commit(key, val, reads, writes)
        self.ninst += 1

    def idma(self, out, in_, out_off, in_off, bound, reads=(), writes=()):
        q = "pool"
        ev = self._deps(reads, writes)
        n = self.dn[q]
        j = n % self.kq
        val = 16 * (n // self.kq + 1)
        key = ("d", q, j)
        if val > 16 and ev.get(key, 0) < val - 16:
            ev[key] = val - 16
        self._wait(q, ev)
        oo = bass.IndirectOffsetOnAxis(ap=out_off, axis=0) if out_off is not None else None
        io = bass.IndirectOffsetOnAxis(ap=in_off, axis=0) if in_off is not None else None
        ins = self.nc.gpsimd.indirect_dma_start(out=out, out_offset=oo, in_=in_, in_offset=io)
        ins.then_inc(self.dsem[q][j], 16)
        self.dn[q] = n + 1
        self._commit(key, val, reads, writes)
        self.ninst += 1

    def barrier(self):
        ev = {}
        for e in self.COMPUTE:
            if self.cnt[e]:
                ev[("c", e)] = self.cnt[e]
        for q in self.dsem:
            n = self.dn[q]
            for j in range(self.kq):
                cntj = (n - j + self.kq - 1) // self.kq if n > j else 0
                if cntj:
                    ev[("d", q, j)] = 16 * cntj
        for e in self.engs:
            self._wait(e, dict(ev), skip_key=None)


def _tiled_table(fn, L):
    nk = L // 128
    a = np.arange(L, dtype=np.float64)
    full = fn(a[:, None], a[None, :])
    t = full.reshape(nk, 128, nk, 128).transpose(2, 1, 0, 3)
    return np.ascontiguousarray(t).astype(NPBF)


def host_consts():
    c = {}
    c["ident_f"] = np.eye(128, dtype=np.float32)
    c["ident_b"] = np.eye(128).astype(NPBF)
    c["ones_b"] = np.ones((128, 128)).astype(NPBF)
    c["ones_f"] = np.ones((128, 128), np.float32)
    for L, tag in ((SEQ, "4096"), (CTX, "256")):
        c["thc" + tag] = _tiled_table(lambda a, b: np.cos(PI * (2 * a + 1) * (2 * b + 1) / (4 * L)), L)
        c["ths" + tag] = _tiled_table(lambda a, b: np.sin(PI * (2 * a + 1) * (2 * b + 1) / (4 * L)), L)
        c["tfc" + tag] = _tiled_table(lambda a, b: np.cos(2 * PI * ((a * b) % L) / L), L)
        c["tfs" + tag] = _tiled_table(lambda a, b: np.sin(2 * PI * ((a * b) % L) / L), L)
        nk = L // 128
        f = np.arange(L, dtype=np.float64)
        al = PI * (2 * f + 1) / (4 * L)
        c["ca" + tag] = np.ascontiguousarray((np.cos(al) / L).reshape(nk, 128).T).astype(np.float32)
        c["sa" + tag] = np.ascontiguousarray((np.sin(al) / L).reshape(nk, 128).T).astype(np.float32)
        pos = np.arange(L, dtype=np.float32)
        t = pos / np.float32(max(L - 1, 1))
        w = np.float32(2.0 * PI / L) * pos
        bands = np.linspace(1e-4, 15, 16, dtype=np.float32)
        ang = w[:, None] * bands[None, :]
        emb = np.concatenate([t[:, None], np.cos(ang), -np.sin(ang)], axis=-1).astype(np.float32)
        c["emb" + tag] = np.ascontiguousarray(emb.T)
        c["negt" + tag] = np.ascontiguousarray((-t).reshape(nk, 128).T).astype(np.float32)
        t1 = (np.arange(L, dtype=np.float32) + 1) / np.float32(max(L - 1, 1))
        c["negt1" + tag] = np.ascontiguousarray((-t1).reshape(nk, 128).T).astype(np.float32)
    deltas = np.abs(np.linspace(math.log(1e-2) / 1.5, math.log(1e-2) / 0.3, 512, dtype=np.float32))
    c["delta_bc"] = np.ascontiguousarray(np.broadcast_to(deltas[None, :], (128, 512))).astype(np.float32)
    dd = np.arange(128, dtype=np.float64)
    ang = 2 * PI * ((dd[:, None] * dd[None, :]) % 128) / 128
    c["c128"] = np.cos(ang).astype(NPBF)
    c["ns128"] = (-np.sin(ang)).astype(NPBF)
    inv = (10000.0 ** (-np.arange(0, 32, 2, dtype=np.float32) / 32)).astype(np.float32)
    row = np.repeat(np.arange(SEQ // 64, dtype=np.float32), 64)
    col = np.tile(np.arange(64, dtype=np.float32), SEQ // 64)
    ar = (row[None, :] * inv[:, None]).astype(np.float32)
    ac = (col[None, :] * inv[:, None]).astype(np.float32)
    c["rope_cos"] = np.concatenate([np.cos(ar), np.cos(ar), np.cos(ac), np.cos(ac)], 0).astype(NPBF)
    c["rope_sin"] = np.concatenate([np.sin(ar), np.sin(ar), np.sin(ac), np.sin(ac)], 0).astype(NPBF)
    Rm = np.zeros((64, 64), np.float32)
    for base in (0, 32):
        for i in range(16):
            Rm[base + i, base + 16 + i] = -1.0
            Rm[base + 16 + i, base + i] = 1.0
    c["ropeRT"] = np.ascontiguousarray(np.concatenate([Rm.T, np.zeros((64, 64), np.float32)], 1)).astype(NPBF)
    j = np.arange(128)[:, None]
    r = np.arange(128)[None, :]
    mp = (j >= r).astype(np.float32)
    mn = (j <= r).astype(np.float32)
    c["maskp"] = np.ascontiguousarray(np.broadcast_to(mp[:, None, :], (128, 4, 128))).astype(np.float32)
    c["maskn"] = np.ascontiguousarray(np.broadcast_to(mn[:, None, :], (128, 4, 128))).astype(np.float32)
    sel = np.zeros((8, 8, 128), np.float32)
    for e in range(8):
        sel[e, e, :] = 1.0
    c["sel"] = sel
    ut = (np.arange(128)[:, None] < np.arange(128)[None, :]).astype(np.float32)
    c["utb"] = ut.astype(NPBF)
    c["iota_p"] = np.arange(128, dtype=np.float32).reshape(128, 1)
    c["bvals"] = np.ascontiguousarray(np.broadcast_to((np.arange(NBLK, dtype=np.float32) * SBLK)[None, :], (128, NBLK)))
    return c


_CONSTS = None


def build(debug=None, upto=99):
    debug = debug or set()
    nc = bass.Bass("TRN2", target_bir_lowering=False)
    ins = {}

    def inp(name, shape, dt=F32):
        ins[name] = nc.dram_tensor(name, list(shape), dt, kind="ExternalInput").ap()
        return ins[name]

    def scratch(name, shape, dt):
        kind = "ExternalOutput" if name in debug else "Internal"
        return nc.dram_tensor(name, list(shape), dt, kind=kind).ap()

    x_in = inp("x", [2, SEQ, D])
    ctx_in = inp("ctx", [2, CTX, D])
    cT = inp("cT", [128, 8, 3])
    ada_w = inp("ada_w", [2, D, 6 * D])
    ada_bT = inp("ada_bT", [128, 2, 48])
    n1g = inp("n1g", [128, 2, 8])
    n2g = inp("n2g", [128, 2, 8])
    w_in = inp("ev_w_in", [D, 2048])
    w_out0 = inp("ev_w_out", [D, D])
    cw = inp("cw", [128, 12, 3])
    cb = inp("cb", [128, 12])
    hyb = inp("hyb", [128, 4])
    hf_w0 = inp("hf_w0", [33, 64])
    hf_w1 = inp("hf_w1", [64, 64])
    hf_w2 = inp("hf_w2", [64, 64])
    hf_w3 = inp("hf_w3", [64, 1024])
    hf_vec = inp("hf_vec", [64, 4])
    ffn_wg = inp("ffn_w_gate", [1, D, DFF])
    ffn_wu = inp("ffn_w_up", [1, D, DFF])
    ffn_wd = inp("ffn_w_down", [1, DFF, D])
    w_qkv = inp("od_w_qkv", [D, 1536])
    w_out1 = inp("od_w_out", [D, D])
    qkg = inp("qkg", [64, 2])
    sinkr = inp("sinkr", [64, 16])
    routerT = inp("routerT", [128, 8, 8])
    moe_wg = inp("moe_w_gate", [NE * NFG * 128, 2048])
    moe_wu = inp("moe_w_up", [NE * NFG * 128, 2048])
    moe_wd = inp("moe_w_down", [NE * NFG * 128, 2048])
    cst = {}
    for k, v in _CONSTS.items():
        cst[k] = inp("k_" + k, v.shape, BF16 if v.dtype == NPBF else F32)
    out = nc.dram_tensor("out", [2, SEQ, D], F32, kind="ExternalOutput").ap()

    LS = [SEQ, SEQ, CTX, CTX]
    TAG = ["4096", "4096", "256", "256"]
    COL = [0, 1, 2, 2]
    xTd = [scratch("xTd%d" % i, [8, 128, LS[i]], F32) for i in range(4)]
    uTd = [scratch("uTd%d" % i, [16, 128, LS[i]], BF16) for i in range(4)]
    zTd = [scratch("zTd%d" % i, [4, 128, LS[i]], BF16) for i in range(4)]
    x0Td = [scratch("x0Td%d" % i, [4, 128, LS[i]], BF16) for i in range(4)]
    catTd = [scratch("catTd%d" % i, [8, 128, LS[i]], BF16) for i in range(4)]
    hTd = [scratch("hTd%d" % i, [8, 128, LS[i]], BF16) for i in range(4)]
    Md = {"4096": scratch("Md4096", [32, 128, 2, 512], F32), "256": scratch("Md256", [2, 128, 2, 512], F32)}
    attTd = [scratch("attTd%d" % i, [16, 64, SEQ], BF16) for i in range(2)]
    gTd = [scratch("gTd%d" % i, [8, SEQ], F32) for i in range(2)]
    Htok = scratch("Htok", [2 * SEQ, D], BF16)
    Xs = scratch("Xs", [NSLOT, D], BF16)
    Ys = scratch("Ys", [NSLOT, D], BF16)
    dbg_mod = scratch("dbg_mod", [128, 2 * 48 * 3], F32) if "dbg_mod" in debug else None
    dbg_plan = scratch("dbg_plan", [128, 168], F32) if "dbg_plan" in debug else None

    with ExitStack() as es0:
        P = Prog(nc, es0)
        cnt = [0]

        def sbt(es, shape, dt, name=None):
            cnt[0] += 1
            return es.enter_context(nc.sbuf_tensor(name or ("t%d" % cnt[0]), list(shape), dt))

        ps = [es0.enter_context(nc.psum_tensor("ps%d" % i, [128, 512], F32)) for i in range(8)]
        PSR = ["ps%d" % i for i in range(8)]

        def mm(bank, out_ap, lhsT, rhs, start, stop, reads):
            P.op("pe", lambda e: e.matmul(out_ap, lhsT=lhsT, rhs=rhs, start=start, stop=stop), reads=reads, writes=[PSR[bank]])

        ident_f = sbt(es0, [128, 128], F32)
        ident_b = sbt(es0, [128, 128], BF16)
        ones_b = sbt(es0, [128, 128], BF16)
        ones_f = sbt(es0, [128, 128], F32)
        epst = sbt(es0, [128, 1], F32)
        mod = sbt(es0, [128, 2, 48, 3], F32)
        gs = sbt(es0, [128, 2, 2, 8, 3], F32)
        for t, k in ((ident_f, "ident_f"), (ident_b, "ident_b"), (ones_b, "ones_b"), (ones_f, "ones_f")):
            P.dma("sp", t[:], cst[k][:, :], writes=["const"])
        P.op("dve", lambda e: e.memset(epst[:], EPS), writes=["const"])

        I32 = mybir.dt.int32
        MK = sbt(es0, [128, 64, 16], F32)
        GV = sbt(es0, [128, 64, 2], F32)
        dsl = sbt(es0, [128, 128], I32)
        idxw = sbt(es0, [128, NBLK * NFG], I32)
        with ExitStack() as es:
            cT_sb = sbt(es, [128, 8, 3], F32)
            sT = sbt(es, [128, 8, 3], F32)
            abT = sbt(es, [128, 2, 48], F32)
            g1 = sbt(es, [128, 2, 8], F32)
            g2 = sbt(es, [128, 2, 8], F32)
            tmpm = sbt(es, [128, 8, 3], F32)
            aw = [sbt(es, [128, 8, 768], F32) for _ in range(2)]
            zt0 = sbt(es, [128, 8, D], BF16)
            P.op("pool", lambda e: e.memset(zt0[:], 0.0), writes=["zt0"])
            for r_ in range(NSLOT // 1024):
                P.dma("pool", Xs[r_ * 1024:(r_ + 1) * 1024, :].rearrange("(t p) d -> p t d", p=128), zt0[:], reads=["zt0"], writes=["Xs"])
            P.dma("sp", cT_sb[:], cT[:, :, :], writes=["cT"])
            P.dma("sp", abT[:], ada_bT[:, :, :], writes=["abT"])
            P.dma("sp", g1[:], n1g[:, :, :], writes=["g12"])
            P.dma("sp", g2[:], n2g[:, :, :], writes=["g12"])
            P.op("act", lambda e: e.activation(out=sT[:], in_=cT_sb[:], func=AF.Silu), reads=["cT"], writes=["sT"])
            it = 0
            for l in range(2):
                awv = ada_w[l].rearrange("(k p) n -> p k n", p=128)
                for nb in range(8):
                    b = it % 2
                    it += 1
                    P.dma("sp", aw[b][:], awv[:, :, nb * 768:(nb + 1) * 768], writes=["aw%d" % b])
                    for j in range(6):
                        n = nb * 6 + j
                        bank = n % 2
                        for k in range(8):
                            mm(bank, ps[bank][:, 0:3], aw[b][:, k, j * 128:(j + 1) * 128], sT[:, k, :], k == 0, k == 7, ["aw%d" % b, "sT"])
                        P.op("dve", lambda e: e.tensor_scalar(out=mod[:, l, n, :], in0=ps[bank][:, 0:3], scalar1=abT[:, l, n:n + 1], scalar2=None, op0=ALU.add),
                             reads=[PSR[bank], "abT"], writes=["mod"])
            for l in range(2):
                for w, gt in ((0, g1), (1, g2)):
                    P.op("dve", lambda e: e.tensor_scalar(out=tmpm[:], in0=mod[:, l, 8 + 24 * w:16 + 24 * w, :], scalar1=1.0, scalar2=None, op0=ALU.add),
                         reads=["mod"], writes=["tmpm"])
                    for col in range(3):
                        P.op("dve", lambda e: e.tensor_tensor(out=gs[:, l, w, :, col], in0=tmpm[:, :, col], in1=gt[:, l, :], op=ALU.mult),
                             reads=["tmpm", "g12"], writes=["mod"])
            if dbg_mod is not None:
                P.dma("sp", dbg_mod[:, :], mod[:].rearrange("p a b c -> p (a b c)"), reads=["mod"], writes=["dbg_mod"])
            P.barrier()

        def SH(l, w, c, col):
            return mod[:, l, 24 * w + c, col:col + 1]

        def GATE(l, w, c, col):
            return mod[:, l, 16 + 24 * w + c, col:col + 1]

        def GS(l, w, c, col):
            return gs[:, l, w, c, col:col + 1]

        def norm_block(es_t, xt, rx, W, l, w, col, outb, rout, tiles, outf=None):
            sq, rs, tmp = tiles
            P.op("act", lambda e: e.activation(out=sq[:, :, :W], in_=xt[:, :, :W], func=AF.Square), reads=[rx], writes=["n_sq"])
            for c in range(8):
                mm(7, ps[7][:, :W], ones_b[:], sq[:, c, :W], c == 0, c == 7, ["n_sq", "const"])
            P.op("act", lambda e: e.activation(out=rs[:, :W], in_=ps[7][:, :W], func=AF.Ln, bias=epst[:, 0:1], scale=1.0 / D),
                 reads=[PSR[7], "const"], writes=["n_rs"])
            P.op("act", lambda e: e.activation(out=rs[:, :W], in_=rs[:, :W], func=AF.Exp, scale=-0.5), reads=["n_rs"], writes=["n_rs"])
            for c in range(8):
                P.op("dve", lambda e: e.scalar_tensor_tensor(out=tmp[:, c, :W], in0=xt[:, c, :W], scalar=GS(l, w, c, col), in1=rs[:, :W], op0=ALU.mult, op1=ALU.mult),
                     reads=[rx, "n_rs", "mod"], writes=["n_tmp"])
                P.op("act", lambda e: e.activation(out=outb[:, c, :W], in_=tmp[:, c, :W], func=AF.Identity, bias=SH(l, w, c, col), scale=1.0),
                     reads=["n_tmp", "mod"], writes=[rout])
                if outf is not None:
                    P.op("act", lambda e: e.activation(out=outf[:, c, :W], in_=tmp[:, c, :W], func=AF.Identity, bias=SH(l, w, c, col), scale=1.0),
                         reads=["n_tmp", "mod"], writes=[rout + "f"])

        def load_cast_w(es, src_view, kch, ncols, name, npart=128):
            wb = sbt(es, [npart, kch, ncols], BF16)
            with ExitStack() as e2:
                st = sbt(e2, [npart, kch, 512], F32)
                for j in range(0, ncols, 512):
                    P.dma("sp", st[:], src_view[:, :, j:j + 512], writes=["wstage"])
                    P.op("act", lambda e: e.copy(out=wb[:, :, j:j + 512], in_=st[:]), reads=["wstage"], writes=[name])
                P.barrier()
            return wb

        items0 = [0, 1, 2, 3]

        def xsrc(it):
            return x_in[it] if it < 2 else ctx_in[it - 2]

        if upto >= 1:
            with ExitStack() as es:
                win_b = load_cast_w(es, w_in.rearrange("(k p) n -> p k n", p=128), 8, 2048, "win_b")
                xin = [sbt(es, [128, 4, D], F32) for _ in range(2)]
                xts = [sbt(es, [128, 8, 512], F32) for _ in range(2)]
                sq = sbt(es, [128, 8, 512], BF16)
                rs = sbt(es, [128, 512], F32)
                tmp = sbt(es, [128, 8, 512], F32)
                hxs = [sbt(es, [128, 8, 512], BF16) for _ in range(2)]
                uts = [sbt(es, [128, 16, 512], BF16) for _ in range(2)]
                ablocks = []
                for it in items0:
                    L = LS[it]
                    W = min(512, L)
                    for blk in range(L // W):
                        ablocks.append((it, blk, W))

                def stT(i):
                    it, blk, W = ablocks[i]
                    b = i % 2
                    xt = xts[b]
                    t0 = blk * W
                    nt = W // 128
                    P.dma("sp", xin[b][:, 0:nt, :], xsrc(it)[t0:t0 + W, :].rearrange("(t p) d -> p t d", p=128), writes=["xin%d" % b])
                    for c in range(8):
                        bank = c % 4
                        for t in range(nt):
                            P.op("pe", lambda e: e.transpose(out=ps[bank][:, t * 128:(t + 1) * 128], in_=xin[b][:, t, c * 128:(c + 1) * 128], identity=ident_f[:]),
                                 reads=["xin%d" % b, "const"], writes=[PSR[bank]])
                        if c % 2:
                            P.op("act", lambda e: e.copy(out=xt[:, c, :W], in_=ps[bank][:, :W]), reads=[PSR[bank]], writes=["xt%d" % b])
                        else:
                            P.op("dve", lambda e: e.tensor_copy(out=xt[:, c, :W], in_=ps[bank][:, :W]), reads=[PSR[bank]], writes=["xt%d" % b])
                    P.dma("pool", xTd[it].rearrange("c p t -> p c t")[:, :, t0:t0 + W], xt[:, :, :W], reads=["xt%d" % b], writes=["xTd%d_%d" % (it, blk)])

                def stN(i):
                    it, blk, W = ablocks[i]
                    b = i % 2
                    norm_block(es, xts[b], "xt%d" % b, W, 0, 0, COL[it], hxs[b], "hx%d" % b, (sq, rs, tmp))

                def stU(i):
                    it, blk, W = ablocks[i]
                    b = i % 2
                    hx = hxs[b]
                    ut = uts[b]
                    t0 = blk * W
                    for n in range(16):
                        bank = 4 + n % 2
                        for k in range(8):
                            mm(bank, ps[bank][:, :W], win_b[:, k, n * 128:(n + 1) * 128], hx[:, k, :W], k == 0, k == 7, ["win_b", "hx%d" % b])
                        if n % 2:
                            P.op("act", lambda e: e.copy(out=ut[:, n, :W], in_=ps[bank][:, :W]), reads=[PSR[bank]], writes=["ut%d" % b])
                        else:
                            P.op("dve", lambda e: e.tensor_copy(out=ut[:, n, :W], in_=ps[bank][:, :W]), reads=[PSR[bank]], writes=["ut%d" % b])
                    P.dma("pool", uTd[it].rearrange("c p t -> p c t")[:, :, t0:t0 + W], ut[:, :, :W], reads=["ut%d" % b], writes=["uTd%d_%d" % (it, blk)])

                nab = len(ablocks)
                stT(0)
                for i in range(nab):
                    if i + 1 < nab:
                        stT(i + 1)
                    stN(i)
                    if i >= 1:
                        stU(i - 1)
                stU(nab - 1)
                P.barrier()

        def blocks_of(it):
            L = LS[it]
            return range(L // min(512, L))

        if upto >= 2:
            for tag, L in (("4096", SEQ), ("256", CTX)):
                nK = L // 128
                W = min(512, L)
                with ExitStack() as es:
                    emb = sbt(es, [33, L], F32)
                    hA = sbt(es, [64, L + 1], F32)
                    hB = sbt(es, [64, L + 1], F32)
                    w0s = sbt(es, [33, 64], F32)
                    w1s = sbt(es, [64, 64], F32)
                    w2s = sbt(es, [64, 64], F32)
                    w3s = sbt(es, [64, 1024], F32)
                    vec = sbt(es, [64, 4], F32)
                    fb = sbt(es, [64, 3], F32)
                    arg = sbt(es, [64, 512], F32)
                    msk = sbt(es, [64, 512], F32)
                    negt = sbt(es, [128, nK], F32)
                    negt1 = sbt(es, [128, nK], F32)
                    ca = sbt(es, [128, nK], F32)
                    sa = sbt(es, [128, nK], F32)
                    dbc = sbt(es, [128, 512], F32)
                    A0 = sbt(es, [128, nK, 512], BF16)
                    B0 = sbt(es, [128, nK, 512], BF16)
                    dec = sbt(es, [128, 512], F32)
                    dec1 = sbt(es, [128, 512], F32)
                    hf = sbt(es, [128, 512], F32)
                    hb = sbt(es, [128, 512], F32)
                    sqf = sbt(es, [128, 512], F32)
                    sqb = sbt(es, [128, 512], F32)
                    rn = sbt(es, [128, 512], F32)
                    tC = [sbt(es, [128, nK, 128], BF16) for _ in range(2)]
                    tS = [sbt(es, [128, nK, 128], BF16) for _ in range(2)]
                    t1 = sbt(es, [128, 512], F32)
                    t2 = sbt(es, [128, 512], F32)
                    t3 = sbt(es, [128, 512], F32)
                    Mt = [sbt(es, [128, 2, 512], F32) for _ in range(2)]
                    for t, src in ((emb, cst["emb" + tag]), (w0s, hf_w0), (w1s, hf_w1), (w2s, hf_w2), (w3s, hf_w3), (vec, hf_vec),
                                   (negt, cst["negt" + tag]), (negt1, cst["negt1" + tag]), (ca, cst["ca" + tag]), (sa, cst["sa" + tag]),
                                   (dbc, cst["delta_bc"])):
                        P.dma("sp", t[:], src[:, :], writes=["fconst"])
                    P.op("dve", lambda e: e.memset(hA[:], 0.0), writes=["hA"])
                    for i in range(3):
                        P.op("dve", lambda e: e.tensor_tensor(out=fb[:, i:i + 1], in0=vec[:, i:i + 1], in1=vec[:, 3:4], op=ALU.mult), reads=["fconst"], writes=["fb"])
                    layers = ((w0s, emb, "fconst", hA, "hA", 0), (w1s, hA, "hA", hB, "hB", 1), (w2s, hB, "hB", hA, "hA", 2))
                    for (ws, src, rsrc, dst, rdst, li) in layers:
                        for cbk in range(L // W):
                            cs = slice(cbk * W, (cbk + 1) * W)
                            mm(0, ps[0][0:64, :W], ws[:], src[:, cs], True, True, ["fconst", rsrc])
                            P.op("dve", lambda e: e.tensor_scalar(out=arg[:, :W], in0=ps[0][0:64, :W], scalar1=vec[:, 3:4], scalar2=fb[:, li:li + 1], op0=ALU.mult, op1=ALU.add),
                                 reads=[PSR[0], "fconst", "fb"], writes=["arg"])
                            for _rep in range(2):
                                P.op("dve", lambda e: e.tensor_scalar(out=msk[:, :W], in0=arg[:, :W], scalar1=PI, scalar2=2 * PI, op0=ALU.is_gt, op1=ALU.mult), reads=["arg"], writes=["msk"])
                                P.op("dve", lambda e: e.tensor_tensor(out=arg[:, :W], in0=arg[:, :W], in1=msk[:, :W], op=ALU.subtract), reads=["arg", "msk"], writes=["arg"])
                                P.op("dve", lambda e: e.tensor_scalar(out=msk[:, :W], in0=arg[:, :W], scalar1=-PI, scalar2=2 * PI, op0=ALU.is_lt, op1=ALU.mult), reads=["arg"], writes=["msk"])
                                P.op("dve", lambda e: e.tensor_tensor(out=arg[:, :W], in0=arg[:, :W], in1=msk[:, :W], op=ALU.add), reads=["arg", "msk"], writes=["arg"])
                            P.op("act", lambda e: e.activation(out=dst[:, cs], in_=arg[:, :W], func=AF.Sin), reads=["arg"], writes=[rdst])
                    for jc in range(nK):
                        mm(0, ps[0][:, :], hA[:, jc * 128:(jc + 1) * 128], w3s[:, 0:512], True, True, ["hA", "fconst"])
                        mm(1, ps[1][:, :], hA[:, jc * 128 + 1:(jc + 1) * 128 + 1], w3s[:, 512:1024], True, True, ["hA", "fconst"])
                        P.op("act", lambda e: e.activation(out=dec[:], in_=dbc[:], func=AF.Exp, scale=negt[:, jc:jc + 1]), reads=["fconst"], writes=["dec"])
                        P.op("act", lambda e: e.activation(out=dec1[:], in_=dbc[:], func=AF.Exp, scale=negt1[:, jc:jc + 1]), reads=["fconst"], writes=["dec1"])
                        P.op("dve", lambda e: e.tensor_tensor(out=hf[:], in0=ps[0][:, :], in1=dec[:], op=ALU.mult), reads=[PSR[0], "dec"], writes=["hf"])
                        P.op("dve", lambda e: e.tensor_tensor(out=hb[:], in0=ps[1][:, :], in1=dec1[:], op=ALU.mult), reads=[PSR[1], "dec1"], writes=["hb"])
                        P.op("pool", lambda e: e.tensor_tensor(out=A0[:, jc, :], in0=hf[:], in1=hb[:], op=ALU.add), reads=["hf", "hb"], writes=["A0"])
                        P.op("pool", lambda e: e.tensor_tensor(out=B0[:, jc, :], in0=hb[:], in1=hf[:], op=ALU.subtract), reads=["hf", "hb"], writes=["B0"])
                        P.op("dve", lambda e: e.tensor_tensor(out=sqf[:], in0=hf[:], in1=hf[:], op=ALU.mult), reads=["hf"], writes=["sqf"])
                        P.op("dve", lambda e: e.tensor_tensor(out=sqb[:], in0=hb[:], in1=hb[:], op=ALU.mult), reads=["hb"], writes=["sqb"])
                        mm(7, ps[7][:, :], ones_f[:], sqf[:], jc == 0, False, ["sqf", "const"])
                        mm(7, ps[7][:, :], ones_f[:], sqb[:], False, jc == nK - 1, ["sqb", "const"])
                    P.op("act", lambda e: e.activation(out=rn[:], in_=ps[7][:, :], func=AF.Sqrt, bias=epst[:, 0:1], scale=1.0), reads=[PSR[7], "const"], writes=["rn"])
                    P.op("dve", lambda e: e.reciprocal(out=rn[:], in_=rn[:]), reads=["rn"], writes=["rn"])
                    for m in range(nK):
                        b = m % 2
                        P.dma("sp", tC[b][:], cst["thc" + tag][m], writes=["tC%d" % b])
                        P.dma("sp", tS[b][:], cst["ths" + tag][m], writes=["tS%d" % b])
                        for k in range(nK):
                            mm(2 * b, ps[2 * b][:, :], tC[b][:, k, :], A0[:, k, :], k == 0, k == nK - 1, ["tC%d" % b, "A0"])
                        for k in range(nK):
                            mm(2 * b + 1, ps[2 * b + 1][:, :], tS[b][:, k, :], B0[:, k, :], k == 0, k == nK - 1, ["tS%d" % b, "B0"])
                        P.op("dve", lambda e: e.tensor_tensor(out=t1[:], in0=ps[2 * b][:, :], in1=rn[:], op=ALU.mult), reads=[PSR[2 * b], "rn"], writes=["t1"])
                        P.op("dve", lambda e: e.tensor_tensor(out=t2[:], in0=ps[2 * b + 1][:, :], in1=rn[:], op=ALU.mult), reads=[PSR[2 * b + 1], "rn"], writes=["t2"])
                        P.op("pool", lambda e: e.tensor_scalar(out=t3[:], in0=t2[:], scalar1=sa[:, m:m + 1], scalar2=None, op0=ALU.mult), reads=["t2", "fconst"], writes=["t3"])
                        P.op("dve", lambda e: e.scalar_tensor_tensor(out=Mt[b][:, 0, :], in0=t1[:], scalar=ca[:, m:m + 1], in1=t3[:], op0=ALU.mult, op1=ALU.subtract),
                             reads=["t1", "t3", "fconst"], writes=["Mt%d" % b])
                        P.op("pool", lambda e: e.tensor_scalar(out=t3[:], in0=t2[:], scalar1=ca[:, m:m + 1], scalar2=None, op0=ALU.mult), reads=["t2", "fconst"], writes=["t3"])
                        P.op("dve", lambda e: e.scalar_tensor_tensor(out=Mt[b][:, 1, :], in0=t1[:], scalar=sa[:, m:m + 1], in1=t3[:], op0=ALU.mult, op1=ALU.add),
                             reads=["t1", "t3", "fconst"], writes=["Mt%d" % b])
                        P.dma("act", Md[tag][m], Mt[b][:], reads=["Mt%d" % b], writes=["Md" + tag])
                    P.barrier()

        if upto >= 3:
            for it in items0:
                L = LS[it]
                tag = TAG[it]
                nK = L // 128
                NB = list(blocks_of(it))
                with ExitStack() as esI:
                    z_tok = sbt(esI, [128, nK, 512], BF16)
                    tC = [sbt(esI, [128, nK, 128], BF16) for _ in range(2)]
                    tS = [sbt(esI, [128, nK, 128], BF16) for _ in range(2)]
                    with ExitStack() as es:
                        cws = sbt(es, [128, 12, 3], F32)
                        cbs = sbt(es, [128, 12], F32)
                        dg = sbt(es, [128, 36, 128], BF16)
                        pad = [sbt(es, [128, L + 2], BF16) for _ in range(3)]
                        v32 = [sbt(es, [128, 512], F32) for _ in range(2)]
                        zT = sbt(es, [128, L], BF16)
                        x0T = sbt(es, [128, L], BF16)
                        P.dma("sp", cws[:], cw[:, :, :], writes=["cws"])
                        P.dma("sp", cbs[:], cb[:, :], writes=["cws"])
                        for p_ in range(3):
                            P.op("pool", lambda e: e.memset(pad[p_][:], 0.0), writes=["pad%d" % p_])
                        for pi in range(12):
                            for tap in range(3):
                                P.op("dve", lambda e: e.tensor_scalar(out=dg[:, pi * 3 + tap, :], in0=ident_b[:], scalar1=cws[:, pi, tap:tap + 1], scalar2=None, op0=ALU.mult),
                                     reads=["const", "cws"], writes=["dg"])
                        Wc = min(512, L)
                        vi = 0
                        for c in range(4):
                            for part in range(3):
                                ch = 4 + part * 4 + c
                                P.dma("sp", pad[part][:, 1:L + 1], uTd[it][ch], reads=["uTd%d_%d" % (it, b_) for b_ in NB], writes=["pad%d" % part])
                            for blk in range(L // Wc):
                                t0 = blk * Wc
                                for part in range(3):
                                    pi = part * 4 + c
                                    for tap in range(3):
                                        mm(part, ps[part][:, :Wc], dg[:, pi * 3 + tap, :], pad[part][:, t0 + tap:t0 + tap + Wc], tap == 0, tap == 2, ["dg", "pad%d" % part])
                                vb = vi % 2
                                vi += 1
                                P.op("act", lambda e: e.activation(out=v32[vb][:, :Wc], in_=ps[0][:, :Wc], func=AF.Identity, bias=cbs[:, c:c + 1], scale=1.0), reads=[PSR[0], "cws"], writes=["v32_%d" % vb])
                                P.op("dve", lambda e: e.scalar_tensor_tensor(out=zT[:, t0:t0 + Wc], in0=ps[1][:, :Wc], scalar=cbs[:, 4 + c:5 + c], in1=v32[vb][:, :Wc], op0=ALU.add, op1=ALU.mult),
                                     reads=[PSR[1], "cws", "v32_%d" % vb], writes=["zT"])
                                P.op("act", lambda e: e.activation(out=x0T[:, t0:t0 + Wc], in_=ps[2][:, :Wc], func=AF.Identity, bias=cbs[:, 8 + c:9 + c], scale=1.0), reads=[PSR[2], "cws"], writes=["x0T"])
                            P.dma("pool", zTd[it][c], zT[:], reads=["zT"], writes=["zTd%d" % it])
                            P.dma("pool", x0Td[it][c], x0T[:], reads=["x0T"], writes=["x0Td%d" % it])
                            for l4 in range(0, nK, 4):
                                nn = min(4, nK - l4)
                                bank = 4 + (l4 // 4) % 2
                                for q in range(nn):
                                    lc = l4 + q
                                    mm(bank, ps[bank][:, q * 128:(q + 1) * 128], zT[:, lc * 128:(lc + 1) * 128], ident_b[:], True, True, ["zT", "const"])
                                P.op("act", lambda e: e.copy(out=z_tok[:, l4:l4 + nn, c * 128:(c + 1) * 128], in_=ps[bank][:, 0:nn * 128].rearrange("p (a b) -> p a b", b=128)),
                                     reads=[PSR[bank]], writes=["z_tok"])
                        P.barrier()
                    Wre = sbt(esI, [128, nK, 512], BF16)
                    nWim = sbt(esI, [128, nK, 512], BF16)
                    with ExitStack() as es:
                        Mt = [sbt(es, [128, 2, 512], F32) for _ in range(2)]
                        p1 = sbt(es, [128, 512], F32)
                        p2 = sbt(es, [128, 512], F32)
                        p3 = sbt(es, [128, 512], F32)
                        p4 = sbt(es, [128, 512], F32)
                        for m in range(nK):
                            b = m % 2
                            P.dma("sp", tC[b][:], cst["thc" + tag][m], writes=["tC%d" % b])
                            P.dma("sp", tS[b][:], cst["ths" + tag][m], writes=["tS%d" % b])
                            P.dma("sp", Mt[b][:], Md[tag][m], reads=["Md" + tag], writes=["Mt%d" % b])
                            ba, bb = 2 * b, 2 * b + 1
                            for k in range(nK):
                                mm(ba, ps[ba][:, :], tC[b][:, k, :], z_tok[:, k, :], k == 0, k == nK - 1, ["tC%d" % b, "z_tok"])
                            for k in range(nK):
                                mm(bb, ps[bb][:, :], tS[b][:, k, :], z_tok[:, k, :], k == 0, k == nK - 1, ["tS%d" % b, "z_tok"])
                            P.op("dve", lambda e: e.tensor_tensor(out=p1[:], in0=ps[ba][:, :], in1=Mt[b][:, 0, :], op=ALU.mult), reads=[PSR[ba], "Mt%d" % b], writes=["p1"])
                            P.op("dve", lambda e: e.tensor_tensor(out=p2[:], in0=ps[bb][:, :], in1=Mt[b][:, 1, :], op=ALU.mult), reads=[PSR[bb], "Mt%d" % b], writes=["p2"])
                            P.op("pool", lambda e: e.tensor_tensor(out=Wre[:, m, :], in0=p1[:], in1=p2[:], op=ALU.add), reads=["p1", "p2"], writes=["Wre"])
                            P.op("dve", lambda e: e.tensor_tensor(out=p3[:], in0=ps[bb][:, :], in1=Mt[b][:, 0, :], op=ALU.mult), reads=[PSR[bb], "Mt%d" % b], writes=["p3"])
                            P.op("dve", lambda e: e.tensor_tensor(out=p4[:], in0=ps[ba][:, :], in1=Mt[b][:, 1, :], op=ALU.mult), reads=[PSR[ba], "Mt%d" % b], writes=["p4"])
                            P.op("pool", lambda e: e.tensor_tensor(out=nWim[:, m, :], in0=p3[:], in1=p4[:], op=ALU.subtract), reads=["p3", "p4"], writes=["nWim"])
                        P.barrier()
                    with ExitStack() as es:
                        hybs = sbt(es, [128, 4], F32)
                        ysb = sbt(es, [128, 512], BF16)
                        zs = [sbt(es, [128, 4, 128], BF16) for _ in range(2)]
                        xs = [sbt(es, [128, 4, 128], BF16) for _ in range(2)]
                        tq = sbt(es, [128, 4, 128], F32)
                        bT = [sbt(es, [128, 4, 128], BF16) for _ in range(2)]
                        P.dma("sp", hybs[:], hyb[:, :], writes=["hybs"])
                        for m in range(nK):
                            b = m % 2
                            P.dma("sp", tC[b][:], cst["thc" + tag][m], writes=["tC%d" % b])
                            P.dma("sp", tS[b][:], cst["ths" + tag][m], writes=["tS%d" % b])
                            P.dma("sp", zs[b][:], zTd[it].rearrange("c p t -> p c t")[:, :, m * 128:(m + 1) * 128], reads=["zTd%d" % it], writes=["zs%d" % b])
                            P.dma("sp", xs[b][:], x0Td[it].rearrange("c p t -> p c t")[:, :, m * 128:(m + 1) * 128], reads=["x0Td%d" % it], writes=["xs%d" % b])
                            for k in range(nK):
                                mm(b, ps[b][:, :], tC[b][:, k, :], Wre[:, k, :], k == 0, False, ["tC%d" % b, "Wre"])
                            for k in range(nK):
                                mm(b, ps[b][:, :], tS[b][:, k, :], nWim[:, k, :], False, k == nK - 1, ["tS%d" % b, "nWim"])
                            P.op("act", lambda e: e.copy(out=ysb[:], in_=ps[b][:, :]), reads=[PSR[b]], writes=["ysb"])
                            for c in range(4):
                                mm(2 + b, ps[2 + b][:, c * 128:(c + 1) * 128], ysb[:, c * 128:(c + 1) * 128], ident_b[:], True, True, ["ysb", "const"])
                            for c in range(4):
                                P.op("dve", lambda e: e.scalar_tensor_tensor(out=tq[:, c, :], in0=zs[b][:, c, :], scalar=hybs[:, c:c + 1], in1=ps[2 + b][:, c * 128:(c + 1) * 128], op0=ALU.mult, op1=ALU.add),
                                     reads=["zs%d" % b, "hybs", PSR[2 + b]], writes=["tq"])
                            P.op("pool", lambda e: e.tensor_tensor(out=bT[b][:], in0=tq[:], in1=xs[b][:], op=ALU.mult), reads=["tq", "xs%d" % b], writes=["bT%d" % b])
                            P.dma("pool", catTd[it].rearrange("c p t -> p c t")[:, 4:8, m * 128:(m + 1) * 128], bT[b][:], reads=["bT%d" % b], writes=["catTd%d" % it])
                        P.barrier()
                    with ExitStack() as es:
                        UfT = sbt(es, [128, 4, L], BF16)
                        c128 = sbt(es, [128, 128], BF16)
                        ns128 = sbt(es, [128, 128], BF16)
                        asb = sbt(es, [128, 512], BF16)
                        aT = [sbt(es, [128, 4, 128], BF16) for _ in range(2)]
                        P_tok, Q_tok = Wre, nWim
                        P.dma("sp", c128[:], cst["c128"][:, :], writes=["c128"])
                        P.dma("sp", ns128[:], cst["ns128"][:, :], writes=["c128"])
                        P.dma("sp", UfT[:], uTd[it].rearrange("c p t -> p c t")[:, 0:4, :], reads=["uTd%d_%d" % (it, b_) for b_ in NB], writes=["UfT"])
                        for lc in range(nK):
                            b = lc % 2
                            for g in range(4):
                                mm(b, ps[b][:, g * 128:(g + 1) * 128], UfT[:, g, lc * 128:(lc + 1) * 128], c128[:], True, True, ["UfT", "c128"])
                            for g in range(4):
                                mm(2 + b, ps[2 + b][:, g * 128:(g + 1) * 128], UfT[:, g, lc * 128:(lc + 1) * 128], ns128[:], True, True, ["UfT", "c128"])
                            P.op("act", lambda e: e.copy(out=P_tok[:, lc, :], in_=ps[b][:, :]), reads=[PSR[b]], writes=["Wre"])
                            P.op("dve", lambda e: e.tensor_copy(out=Q_tok[:, lc, :], in_=ps[2 + b][:, :]), reads=[PSR[2 + b]], writes=["nWim"])
                        sc = 1.0 / math.sqrt(128.0 * L)
                        for m in range(nK):
                            b = m % 2
                            P.dma("sp", tC[b][:], cst["tfc" + tag][m], writes=["tC%d" % b])
                            P.dma("sp", tS[b][:], cst["tfs" + tag][m], writes=["tS%d" % b])
                            for k in range(nK):
                                mm(4 + b, ps[4 + b][:, :], tC[b][:, k, :], P_tok[:, k, :], k == 0, False, ["tC%d" % b, "Wre"])
                            for k in range(nK):
                                mm(4 + b, ps[4 + b][:, :], tS[b][:, k, :], Q_tok[:, k, :], False, k == nK - 1, ["tS%d" % b, "nWim"])
                            P.op("act", lambda e: e.activation(out=asb[:], in_=ps[4 + b][:, :], func=AF.Copy, scale=sc), reads=[PSR[4 + b]], writes=["asb"])
                            for g in range(4):
                                mm(6 + b, ps[6 + b][:, g * 128:(g + 1) * 128], asb[:, g * 128:(g + 1) * 128], ident_b[:], True, True, ["asb", "const"])
                            P.op("dve", lambda e: e.tensor_copy(out=aT[b][:], in_=ps[6 + b][:, :].rearrange("p (a b) -> p a b", b=128)), reads=[PSR[6 + b]], writes=["aT%d" % b])
                            P.dma("pool", catTd[it].rearrange("c p t -> p c t")[:, 0:4, m * 128:(m + 1) * 128], aT[b][:], reads=["aT%d" % b], writes=["catTd%d" % it])
                    P.barrier()

        def outproj_phase(l, items, wout_view, kparts, cat_loader, router, npart=128):
            with ExitStack() as es:
                nkc = len(kparts)
                wb = load_cast_w(es, wout_view, nkc, D, "wout_b", npart=npart)
                xt = sbt(es, [128, 8, 512], F32)
                x1s = [sbt(es, [128, 8, 512], F32) for _ in range(2)]
                sq = sbt(es, [128, 8, 512], BF16)
                rs = sbt(es, [128, 512], F32)
                tmp = sbt(es, [128, 8, 512], F32)
                hx = sbt(es, [128, 8, 512], BF16)
                hxf = sbt(es, [128, 8, 512], F32) if router else None
                cat = cat_loader(es)
                if router:
                    rw = sbt(es, [128, 8, 8], F32)
                    lg = sbt(es, [128, 8], F32)
                    m1 = sbt(es, [128, 1], F32)
                    m2 = sbt(es, [128, 1], F32)
                    mk1 = sbt(es, [128, 8], F32)
                    mk2 = sbt(es, [128, 8], F32)
                    l2 = sbt(es, [128, 8], F32)
                    dd = sbt(es, [128, 1], F32)
                    g1t = sbt(es, [128, 1], F32)
                    g2t = sbt(es, [128, 1], F32)
                    gt = sbt(es, [128, 8], F32)
                    htok = [sbt(es, [128, D], BF16) for _ in range(2)]
                    P.dma("sp", rw[:], routerT[:, :, :], writes=["rw"])
                oblocks = []
                for it in items:
                    W = min(512, LS[it])
                    for blk in range(LS[it] // W):
                        oblocks.append((it, blk, W))

                def stM(i):
                    it, blk, W = oblocks[i]
                    t0 = blk * W
                    x1 = x1s[i % 2]
                    rx1 = "x1_%d" % (i % 2)
                    rcat = cat(it, t0, W)
                    P.dma("sp", xt[:, :, :W], xTd[it].rearrange("c p t -> p c t")[:, :, t0:t0 + W], reads=["xTd%d_%d" % (it, blk)], writes=["xt"])
                    for n in range(8):
                        bank = n % 4
                        for ki, (lh, rh) in enumerate(kparts):
                            mm(bank, ps[bank][:, :W], lh(wb, n), rh(W), ki == 0, ki == nkc - 1, ["wout_b", rcat])
                        P.op("dve", lambda e: e.scalar_tensor_tensor(out=x1[:, n, :W], in0=ps[bank][:, :W], scalar=GATE(l, 0, n, COL[it]), in1=xt[:, n, :W], op0=ALU.mult, op1=ALU.add),
                             reads=[PSR[bank], "xt", "mod"], writes=[rx1])
                    P.dma("pool", xTd[it].rearrange("c p t -> p c t")[:, :, t0:t0 + W], x1[:, :, :W], reads=[rx1], writes=["xTd%d_%d" % (it, blk)])
                def stN(i):
                    it, blk, W = oblocks[i]
                    t0 = blk * W
                    x1 = x1s[i % 2]
                    rx1 = "x1_%d" % (i % 2)
                    norm_block(es, x1, rx1, W, l, 1, COL[it], hx, "hx", (sq, rs, tmp), outf=hxf)
                    P.dma("pool", hTd[it].rearrange("c p t -> p c t")[:, :, t0:t0 + W], hx[:, :, :W], reads=["hx"], writes=["hTd%d_%d" % (it, blk)])
                def stR(i):
                    it, blk, W = oblocks[i]
                    t0 = blk * W
                    x1 = x1s[i % 2]
                    rx1 = "x1_%d" % (i % 2)
                    if router:
                        for t in range(W // 128):
                            ts_ = slice(t * 128, (t + 1) * 128)
                            for k in range(8):
                                mm(4, ps[4][:, 0:8], hxf[:, k, ts_], rw[:, k, :], k == 0, k == 7, ["hxf", "rw"])
                            P.op("dve", lambda e: e.tensor_copy(out=lg[:], in_=ps[4][:, 0:8]), reads=[PSR[4]], writes=["lg"])
                            P.op("dve", lambda e: e.reduce_max(out=m1[:], in_=lg[:], axis=mybir.AxisListType.X), reads=["lg"], writes=["m1"])
                            P.op("dve", lambda e: e.tensor_scalar(out=mk1[:], in0=lg[:], scalar1=m1[:, 0:1], scalar2=None, op0=ALU.is_ge), reads=["lg", "m1"], writes=["mk1"])
                            P.op("dve", lambda e: e.scalar_tensor_tensor(out=l2[:], in0=mk1[:], scalar=-1e30, in1=lg[:], op0=ALU.mult, op1=ALU.add), reads=["mk1", "lg"], writes=["l2"])
                            P.op("dve", lambda e: e.reduce_max(out=m2[:], in_=l2[:], axis=mybir.AxisListType.X), reads=["l2"], writes=["m2"])
                            P.op("dve", lambda e: e.tensor_scalar(out=mk2[:], in0=l2[:], scalar1=m2[:, 0:1], scalar2=None, op0=ALU.is_ge), reads=["l2", "m2"], writes=["mk2"])
                            P.op("dve", lambda e: e.tensor_tensor(out=dd[:], in0=m2[:], in1=m1[:], op=ALU.subtract), reads=["m1", "m2"], writes=["dd"])
                            P.op("act", lambda e: e.activation(out=g2t[:], in_=dd[:], func=AF.Exp), reads=["dd"], writes=["g2t"])
                            P.op("dve", lambda e: e.tensor_scalar(out=g1t[:], in0=g2t[:], scalar1=1.0, scalar2=None, op0=ALU.add), reads=["g2t"], writes=["g1t"])
                            P.op("dve", lambda e: e.reciprocal(out=g1t[:], in_=g1t[:]), reads=["g1t"], writes=["g1t"])
                            P.op("dve", lambda e: e.tensor_tensor(out=g2t[:], in0=g2t[:], in1=g1t[:], op=ALU.mult), reads=["g2t", "g1t"], writes=["g2t"])
                            P.op("dve", lambda e: e.tensor_scalar(out=gt[:], in0=mk1[:], scalar1=g1t[:, 0:1], scalar2=None, op0=ALU.mult), reads=["mk1", "g1t"], writes=["gt"])
                            P.op("dve", lambda e: e.scalar_tensor_tensor(out=gt[:], in0=mk2[:], scalar=g2t[:, 0:1], in1=gt[:], op0=ALU.mult, op1=ALU.add), reads=["mk2", "g2t", "gt"], writes=["gt"])
                            ti = it * 32 + blk * 4 + t
                            P.op("pool", lambda e: e.tensor_copy(out=MK[:, ti, 0:8], in_=mk1[:]), reads=["mk1"], writes=["MK"])
                            P.op("pool", lambda e: e.tensor_copy(out=MK[:, ti, 8:16], in_=mk2[:]), reads=["mk2"], writes=["MK"])
                            P.op("pool", lambda e: e.tensor_copy(out=GV[:, ti, 0:1], in_=g1t[:]), reads=["g1t"], writes=["GV"])
                            P.op("pool", lambda e: e.tensor_copy(out=GV[:, ti, 1:2], in_=g2t[:]), reads=["g2t"], writes=["GV"])
                            hb_ = ti % 2
                            for c in range(8):
                                bank = 5 + c // 4
                                mm(bank, ps[bank][:, (c % 4) * 128:(c % 4 + 1) * 128], hx[:, c, ts_], ident_b[:], True, True, ["hx", "const"])
                            P.op("act", lambda e: e.copy(out=htok[hb_][:, 0:512], in_=ps[5][:, :]), reads=[PSR[5]], writes=["htok%d" % hb_])
                            P.op("dve", lambda e: e.tensor_copy(out=htok[hb_][:, 512:1024], in_=ps[6][:, :]), reads=[PSR[6]], writes=["htok%d" % hb_])
                            P.dma("pool", Htok[ti * 128:(ti + 1) * 128, :], htok[hb_][:], reads=["htok%d" % hb_], writes=["Htok%d" % ti])

                nob = len(oblocks)
                stM(0)
                for i in range(nob):
                    stN(i)
                    if i + 1 < nob:
                        stM(i + 1)
                    if router:
                        stR(i)
                P.barrier()

        def cat_loader0(es):
            cat = sbt(es, [128, 8, 512], BF16)

            def load(it, t0, W):
                P.dma("sp", cat[:, :, :W], catTd[it].rearrange("c p t -> p c t")[:, :, t0:t0 + W], reads=["catTd%d" % it], writes=["cat"])
                return "cat"
            load.tile = cat
            return load

        if upto >= 4:
            holder = {}

            def cl0(es):
                f = cat_loader0(es)
                holder["cat"] = f.tile
                return f
            kparts0 = [((lambda wb, n, k=k: wb[:, k, n * 128:(n + 1) * 128]), (lambda W, k=k: holder["cat"][:, k, :W])) for k in range(8)]
            outproj_phase(0, items0, w_out0.rearrange("(k p) n -> p k n", p=128), kparts0, cl0, router=False)

        def ffn_phase(l, blocks, wg, wu, wd, E, gated, final):
            FG = 256
            NFG = DFF // FG
            with ExitStack() as es:
                NTmax = max(sum(s[2] for s in segs) for segs in blocks)
                hT = sbt(es, [128, 8, NTmax], BF16)
                acc = sbt(es, [128, 8, NTmax], F32)
                if gated:
                    gT = sbt(es, [8, NTmax], F32)
                    Gbc = sbt(es, [128, NTmax], F32)
                    selt = sbt(es, [8, 8, 128], F32)
                    P.dma("sp", selt[:], cst["sel"][:, :, :], writes=["selt"])
                wi = 0
                oi = 0
                assert not final and not gated
                esW = es
                stg = [sbt(esW, [128, 8, FG], F32) for _ in range(2)]
                stgd = [sbt(esW, [128, 2, D], F32) for _ in range(2)]
                wgb = [sbt(esW, [128, 8, FG], BF16) for _ in range(2)]
                wub = [sbt(esW, [128, 8, FG], BF16) for _ in range(2)]
                wdb = [sbt(esW, [128, 2, D], BF16) for _ in range(2)]
                sg = [sbt(esW, [128, 512], F32) for _ in range(2)]
                tu = [sbt(esW, [128, 512], F32) for _ in range(2)]
                hh = [sbt(esW, [128, 2, 512], BF16) for _ in range(2)]
                x1 = sbt(esW, [128, 8, 512], F32)
                x2 = sbt(esW, [128, 8, 512], F32)
                for segs in blocks:
                    NT = sum(s[2] for s in segs)
                    off = 0
                    for (it, t0, n) in segs:
                        rds = ["hTd%d_%d" % (it, b_) for b_ in range(t0 // min(512, LS[it]), (t0 + n + min(512, LS[it]) - 1) // min(512, LS[it]))]
                        P.dma("sp", hT[:, :, off:off + n], hTd[it].rearrange("c p t -> p c t")[:, :, t0:t0 + n], reads=rds, writes=["hT"])
                        if gated:
                            rdg = ["gTd%d_%d" % (it, b_) for b_ in range(t0 // 512, (t0 + n + 511) // 512)]
                            P.dma("sp", gT[:, off:off + n], gTd[it][:, t0:t0 + n], reads=rdg, writes=["gT"])
                        off += n
                    nsub = (NT + 511) // 512
                    first = True
                    steps = []
                    gu_done = [0]
                    dn_done = [0]

                    def emit_gu(st, si, NT=NT):
                        b, s_, fst = st
                        hb = si % 2
                        ss_ = slice(s_ * 512, min(NT, (s_ + 1) * 512))
                        wdt = ss_.stop - ss_.start
                        for j in range(2):
                            for k in range(8):
                                mm(0 + j, ps[0 + j][:, :wdt], wgb[b][:, k, j * 128:(j + 1) * 128], hT[:, k, ss_], k == 0, k == 7, ["wgb%d" % b, "hT"])
                            for k in range(8):
                                mm(2 + j, ps[2 + j][:, :wdt], wub[b][:, k, j * 128:(j + 1) * 128], hT[:, k, ss_], k == 0, k == 7, ["wub%d" % b, "hT"])
                            P.op("act", lambda e: e.activation(out=sg[j][:, :wdt], in_=ps[0 + j][:, :wdt], func=AF.Silu), reads=[PSR[0 + j]], writes=["sg%d" % j])
                            if gated:
                                P.op("dve", lambda e: e.tensor_tensor(out=tu[j][:, :wdt], in0=ps[2 + j][:, :wdt], in1=Gbc[:, ss_], op=ALU.mult), reads=[PSR[2 + j], "Gbc"], writes=["tu%d" % j])
                                P.op("pool", lambda e: e.tensor_tensor(out=hh[hb][:, j, :wdt], in0=sg[j][:, :wdt], in1=tu[j][:, :wdt], op=ALU.mult), reads=["sg%d" % j, "tu%d" % j], writes=["hh%d" % hb])
                            else:
                                P.op("dve", lambda e: e.tensor_tensor(out=hh[hb][:, j, :wdt], in0=ps[2 + j][:, :wdt], in1=sg[j][:, :wdt], op=ALU.mult), reads=[PSR[2 + j], "sg%d" % j], writes=["hh%d" % hb])

                    def emit_dn(st, si, NT=NT):
                        b, s_, fst = st
                        hb = si % 2
                        ss_ = slice(s_ * 512, min(NT, (s_ + 1) * 512))
                        wdt = ss_.stop - ss_.start
                        for n in range(8):
                            bank = 4 + (n % 2 if gated else n % 4)
                            for j in range(2):
                                mm(bank, ps[bank][:, :wdt], wdb[b][:, j, n * 128:(n + 1) * 128], hh[hb][:, j, :wdt], j == 0, j == 1, ["wdb%d" % b, "hh%d" % hb])
                            if fst:
                                P.op("dve", lambda e: e.tensor_copy(out=acc[:, n, ss_], in_=ps[bank][:, :wdt]), reads=[PSR[bank]], writes=["acc"])
                            else:
                                P.op("dve", lambda e: e.tensor_tensor(out=acc[:, n, ss_], in0=acc[:, n, ss_], in1=ps[bank][:, :wdt], op=ALU.add), reads=[PSR[bank], "acc"], writes=["acc"])
                    for e_ in range(E):
                        if gated:
                            for s in range(nsub):
                                ss_ = slice(s * 512, min(NT, (s + 1) * 512))
                                wdt = ss_.stop - ss_.start
                                mm(6, ps[6][:, :wdt], selt[:, e_, :], gT[:, ss_], True, True, ["selt", "gT"])
                                P.op("act", lambda e: e.copy(out=Gbc[:, ss_], in_=ps[6][:, :wdt]), reads=[PSR[6]], writes=["Gbc"])
                        wgv = wg[e_].rearrange("(k p) f -> p k f", p=128)
                        wuv = wu[e_].rearrange("(k p) f -> p k f", p=128)
                        for fg in range(NFG):
                            b = wi % 2
                            wi += 1
                            fs = slice(fg * FG, (fg + 1) * FG)
                            P.dma("sp", stg[0][:], wgv[:, :, fs], writes=["stg0"])
                            P.op("act", lambda e: e.copy(out=wgb[b][:], in_=stg[0][:]), reads=["stg0"], writes=["wgb%d" % b])
                            P.dma("sp", stg[1][:], wuv[:, :, fs], writes=["stg1"])
                            P.op("pool", lambda e: e.tensor_copy(out=wub[b][:], in_=stg[1][:]), reads=["stg1"], writes=["wub%d" % b])
                            P.dma("sp", stgd[0][:], wd[e_][fg * FG:(fg + 1) * FG, :].rearrange("(j p) n -> p j n", p=128), writes=["stgd0"])
                            P.op("act", lambda e: e.copy(out=wdb[b][:], in_=stgd[0][:]), reads=["stgd0"], writes=["wdb%d" % b])
                            for s_ in range(nsub):
                                steps.append((b, s_, first))
                            first = False
                            while gu_done[0] < len(steps):
                                emit_gu(steps[gu_done[0]], gu_done[0])
                                gu_done[0] += 1
                                if dn_done[0] < gu_done[0] - 1:
                                    emit_dn(steps[dn_done[0]], dn_done[0])
                                    dn_done[0] += 1
                    while dn_done[0] < len(steps):
                        emit_dn(steps[dn_done[0]], dn_done[0])
                        dn_done[0] += 1
                    off = 0
                    for (it, t0, n) in segs:
                        W = min(512, LS[it])
                        for q in range(n // W):
                            blk = (t0 + q * W) // W
                            tt = t0 + q * W
                            P.dma("sp", x1[:, :, :W], xTd[it].rearrange("c p t -> p c t")[:, :, tt:tt + W], reads=["xTd%d_%d" % (it, blk)], writes=["x1f"])
                            for c in range(8):
                                P.op("dve", lambda e: e.scalar_tensor_tensor(out=x2[:, c, :W], in0=acc[:, c, off + q * W:off + (q + 1) * W], scalar=GATE(l, 1, c, COL[it]), in1=x1[:, c, :W], op0=ALU.mult, op1=ALU.add),
                                     reads=["acc", "x1f", "mod"], writes=["x2"])
                            if not final:
                                P.dma("pool", xTd[it].rearrange("c p t -> p c t")[:, :, tt:tt + W], x2[:, :, :W], reads=["x2"], writes=["xTd%d_%d" % (it, blk)])
                            else:
                                ob = oi % 2
                                oi += 1
                                for t in range(W // 128):
                                    for c in range(8):
                                        bank = 6 + (c // 4) % 2
                                        P.op("pe", lambda e: e.transpose(out=ps[bank][:, (c % 4) * 128:(c % 4 + 1) * 128], in_=x2[:, c, t * 128:(t + 1) * 128], identity=ident_f[:]),
                                             reads=["x2", "const"], writes=[PSR[bank]])
                                        if c % 4 == 3:
                                            h0 = (c // 4) * 512
                                            if c // 4:
                                                P.op("act", lambda e: e.copy(out=xo[ob][:, t, h0:h0 + 512], in_=ps[bank][:, :]), reads=[PSR[bank]], writes=["xo%d" % ob])
                                            else:
                                                P.op("dve", lambda e: e.tensor_copy(out=xo[ob][:, t, h0:h0 + 512], in_=ps[bank][:, :]), reads=[PSR[bank]], writes=["xo%d" % ob])
                                P.dma("pool", out[it][tt:tt + W, :].rearrange("(t p) d -> p t d", p=128), xo[ob][:, 0:W // 128, :], reads=["xo%d" % ob], writes=["out"])
                        off += n
                P.barrier()


        def moe_sparse(l):
            AXX = mybir.AxisListType.X
            with ExitStack() as es:
                Mb = sbt(es, [128, 64, 8], BF16)
                utb = sbt(es, [128, 128], BF16)
                iota = sbt(es, [128, 1], F32)
                bvals = sbt(es, [128, NBLK], F32)
                cntt = sbt(es, [128, 8], F32)
                qq = sbt(es, [128, 8], F32)
                padded = sbt(es, [128, 8], F32)
                pend = sbt(es, [128, 8], F32)
                pstart = sbt(es, [128, 8], F32)
                be = sbt(es, [128, NBLK], F32)
                idxf = sbt(es, [128, NBLK, NFG], F32)
                base = sbt(es, [128, 64, 8], F32)
                slot = sbt(es, [128, 64, 8], F32)
                tsel = sbt(es, [128, 64, 8], F32)
                dsf = sbt(es, [128, 64, 2], F32)
                P.dma("sp", utb[:], cst["utb"][:, :], writes=["plc"])
                P.dma("sp", iota[:], cst["iota_p"][:, :], writes=["plc"])
                P.dma("sp", bvals[:], cst["bvals"][:, :], writes=["plc"])
                P.op("dve", lambda e: e.tensor_tensor(out=Mb[:], in0=MK[:, :, 0:8], in1=MK[:, :, 8:16], op=ALU.add), reads=["MK"], writes=["Mb"])
                for ti in range(64):
                    mm(0, ps[0][:, 0:8], ones_b[:], Mb[:, ti, :], ti == 0, ti == 63, ["Mb", "const"])
                for ti in range(64):
                    mm(1, ps[1][:, ti * 8:(ti + 1) * 8], ones_b[:], Mb[:, ti, :], True, True, ["Mb", "const"])
                for ti in range(64):
                    mm(2, ps[2][:, ti * 8:(ti + 1) * 8], utb[:], Mb[:, ti, :], True, True, ["Mb", "plc"])
                P.op("dve", lambda e: e.tensor_copy(out=cntt[:], in_=ps[0][:, 0:8]), reads=[PSR[0]], writes=["cntt"])
                P.op("dve", lambda e: e.tensor_scalar(out=qq[:], in0=cntt[:], scalar1=0.0, scalar2=None, op0=ALU.is_gt), reads=["cntt"], writes=["qq"])
                for m_ in range(1, 8):
                    P.op("dve", lambda e: e.scalar_tensor_tensor(out=qq[:], in0=cntt[:], scalar=float(m_ * SBLK), in1=qq[:], op0=ALU.is_gt, op1=ALU.add), reads=["cntt", "qq"], writes=["qq"])
                P.op("dve", lambda e: e.tensor_scalar(out=padded[:], in0=qq[:], scalar1=float(SBLK), scalar2=None, op0=ALU.mult), reads=["qq"], writes=["padded"])
                P.op("dve", lambda e: e.tensor_copy(out=pend[:, 0:1], in_=padded[:, 0:1]), reads=["padded"], writes=["pend"])
                for e_ in range(1, 8):
                    P.op("dve", lambda e: e.tensor_tensor(out=pend[:, e_:e_ + 1], in0=pend[:, e_ - 1:e_], in1=padded[:, e_:e_ + 1], op=ALU.add), reads=["pend", "padded"], writes=["pend"])
                P.op("dve", lambda e: e.tensor_tensor(out=pstart[:], in0=pend[:], in1=padded[:], op=ALU.subtract), reads=["pend", "padded"], writes=["pstart"])
                P.op("dve", lambda e: e.tensor_scalar(out=be[:], in0=bvals[:], scalar1=pend[:, 0:1], scalar2=None, op0=ALU.is_ge), reads=["plc", "pend"], writes=["be"])
                for e_ in range(1, 8):
                    P.op("dve", lambda e: e.scalar_tensor_tensor(out=be[:], in0=bvals[:], scalar=pend[:, e_:e_ + 1], in1=be[:], op0=ALU.is_ge, op1=ALU.add), reads=["plc", "pend", "be"], writes=["be"])
                P.op("dve", lambda e: e.tensor_scalar(out=be[:], in0=be[:], scalar1=7.0, scalar2=None, op0=ALU.min), reads=["be"], writes=["be"])
                for fg in range(NFG):
                    P.op("dve", lambda e: e.tensor_scalar(out=idxf[:, :, fg], in0=be[:], scalar1=float(NFG * 128), scalar2=float(fg * 128), op0=ALU.mult, op1=ALU.add), reads=["be"], writes=["idxf"])
                P.op("dve", lambda e: e.tensor_scalar(out=idxf[:], in0=idxf[:], scalar1=iota[:, 0:1], scalar2=None, op0=ALU.add), reads=["idxf", "plc"], writes=["idxf"])
                P.op("dve", lambda e: e.tensor_copy(out=idxw[:], in_=idxf[:].rearrange("p a b -> p (a b)")), reads=["idxf"], writes=["idxw"])
                P.op("dve", lambda e: e.tensor_copy(out=base[:, 0, :], in_=pstart[:]), reads=["pstart"], writes=["base"])
                for ti in range(1, 64):
                    P.op("dve", lambda e: e.tensor_tensor(out=base[:, ti, :], in0=base[:, ti - 1, :], in1=ps[1][:, (ti - 1) * 8:ti * 8], op=ALU.add), reads=["base", PSR[1]], writes=["base"])
                P.op("dve", lambda e: e.tensor_tensor(out=slot[:], in0=base[:], in1=ps[2][:, :].rearrange("p (a b) -> p a b", b=8), op=ALU.add), reads=["base", PSR[2]], writes=["slot"])
                for k_ in range(2):
                    P.op("dve", lambda e: e.tensor_tensor(out=tsel[:], in0=slot[:], in1=MK[:, :, k_ * 8:(k_ + 1) * 8], op=ALU.mult), reads=["slot", "MK"], writes=["tsel"])
                    P.op("dve", lambda e: e.reduce_sum(out=dsf[:, :, k_], in_=tsel[:], axis=AXX), reads=["tsel"], writes=["dsf"])
                P.op("dve", lambda e: e.tensor_scalar(out=dsf[:], in0=dsf[:], scalar1=float(NSLOT - 1), scalar2=None, op0=ALU.min), reads=["dsf"], writes=["dsf"])
                P.op("dve", lambda e: e.tensor_copy(out=dsl[:], in_=dsf[:].rearrange("p a b -> p (a b)")), reads=["dsf"], writes=["dsl"])
                if "dbg_plan" in debug:
                    P.dma("sp", dbg_plan[:, 0:128], dsf[:].rearrange("p a b -> p (a b)"), reads=["dsf"], writes=["dbgp"])
                    P.dma("sp", dbg_plan[:, 128:128 + NBLK], be[:], reads=["be"], writes=["dbgp"])
                    P.dma("sp", dbg_plan[:, 160:168], cntt[:], reads=["cntt"], writes=["dbgp"])
                P.barrier()
            with ExitStack() as es:
                ht = [sbt(es, [128, D], BF16) for _ in range(4)]
                for ti in range(64):
                    b = ti % 4
                    P.dma("sp", ht[b][:], Htok[ti * 128:(ti + 1) * 128, :], reads=["Htok%d" % ti], writes=["ht%d" % b])
                    for k_ in range(2):
                        P.idma(Xs[:, :], ht[b][:], dsl[:, 2 * ti + k_:2 * ti + k_ + 1], None, NSLOT - 1, reads=["ht%d" % b, "dsl"], writes=["Xs"])
                P.barrier()
            with ExitStack() as es:
                xtok = sbt(es, [128, 8, D], BF16)
                hT = sbt(es, [128, 8, SBLK], BF16)
                acc = sbt(es, [128, 8, SBLK], F32)
                ybs = [sbt(es, [128, 8, 128], BF16) for _ in range(2)]
                ytok = [sbt(es, [128, D], BF16) for _ in range(2)]
                stg = [[sbt(es, [128, 2048], F32) for _ in range(2)] for _ in range(3)]
                wgb = [sbt(es, [128, 8, 256], BF16) for _ in range(2)]
                wub = [sbt(es, [128, 8, 256], BF16) for _ in range(2)]
                wdb = [sbt(es, [128, 2, D], BF16) for _ in range(2)]
                sg = [sbt(es, [128, 512], F32) for _ in range(2)]
                hh = [sbt(es, [128, 2, 512], BF16) for _ in range(2)]
                wi = 0
                yi = 0
                for blk in range(NBLK):
                    P.dma("sp", xtok[:], Xs[blk * SBLK:(blk + 1) * SBLK, :].rearrange("(t p) d -> p t d", p=128), reads=["Xs"], writes=["xtok"])
                    for c in range(8):
                        for t4 in range(2):
                            bank = 6 + (c * 2 + t4) % 2
                            for q in range(4):
                                t = t4 * 4 + q
                                mm(bank, ps[bank][:, q * 128:(q + 1) * 128], xtok[:, t, c * 128:(c + 1) * 128], ident_b[:], True, True, ["xtok", "const"])
                            if (c * 2 + t4) % 2:
                                P.op("act", lambda e: e.copy(out=hT[:, c, t4 * 512:(t4 + 1) * 512], in_=ps[bank][:, :]), reads=[PSR[bank]], writes=["hT"])
                            else:
                                P.op("dve", lambda e: e.tensor_copy(out=hT[:, c, t4 * 512:(t4 + 1) * 512], in_=ps[bank][:, :]), reads=[PSR[bank]], writes=["hT"])
                    steps = []
                    gu_done = 0
                    dn_done = 0

                    def emit_gu(st, si):
                        b, s_, fst = st
                        hb = si % 2
                        ss_ = slice(s_ * 512, (s_ + 1) * 512)
                        for j in range(2):
                            for k in range(8):
                                mm(0 + j, ps[0 + j][:, :], wgb[b][:, k, j * 128:(j + 1) * 128], hT[:, k, ss_], k == 0, k == 7, ["wgb%d" % b, "hT"])
                            for k in range(8):
                                mm(2 + j, ps[2 + j][:, :], wub[b][:, k, j * 128:(j + 1) * 128], hT[:, k, ss_], k == 0, k == 7, ["wub%d" % b, "hT"])
                            P.op("act", lambda e: e.activation(out=sg[j][:], in_=ps[0 + j][:, :], func=AF.Silu), reads=[PSR[0 + j]], writes=["sg%d" % j])
                            P.op("dve", lambda e: e.tensor_tensor(out=hh[hb][:, j, :], in0=ps[2 + j][:, :], in1=sg[j][:], op=ALU.mult), reads=[PSR[2 + j], "sg%d" % j], writes=["hh%d" % hb])

                    def emit_dn(st, si):
                        b, s_, fst = st
                        hb = si % 2
                        ss_ = slice(s_ * 512, (s_ + 1) * 512)
                        for n in range(8):
                            bank = 4 + n % 4
                            for j in range(2):
                                mm(bank, ps[bank][:, :], wdb[b][:, j, n * 128:(n + 1) * 128], hh[hb][:, j, :], j == 0, j == 1, ["wdb%d" % b, "hh%d" % hb])
                            if fst:
                                P.op("dve", lambda e: e.tensor_copy(out=acc[:, n, ss_], in_=ps[bank][:, :]), reads=[PSR[bank]], writes=["acc"])
                            else:
                                P.op("dve", lambda e: e.tensor_tensor(out=acc[:, n, ss_], in0=acc[:, n, ss_], in1=ps[bank][:, :], op=ALU.add), reads=[PSR[bank], "acc"], writes=["acc"])

                    def gather_w(blk_, fg_, b_):
                        ic = blk_ * NFG + fg_
                        for wsrc, wk in ((moe_wg, 0), (moe_wu, 1), (moe_wd, 2)):
                            P.idma(stg[wk][b_][:], wsrc[:, :], None, idxw[:, ic:ic + 1], NE * NFG * 128 - 1, reads=["idxw"], writes=["stg%d_%d" % (wk, b_)])

                    if blk == 0:
                        gather_w(0, 0, wi % 2)
                    for fg in range(NFG):
                        b = wi % 2
                        wi += 1
                        if fg + 1 < NFG:
                            gather_w(blk, fg + 1, wi % 2)
                        elif blk + 1 < NBLK:
                            gather_w(blk + 1, 0, wi % 2)
                        P.op("act", lambda e: e.copy(out=wgb[b][:].rearrange("p k f -> p (k f)"), in_=stg[0][b][:]), reads=["stg0_%d" % b], writes=["wgb%d" % b])
                        P.op("act", lambda e: e.copy(out=wub[b][:].rearrange("p k f -> p (k f)"), in_=stg[1][b][:]), reads=["stg1_%d" % b], writes=["wub%d" % b])
                        P.op("act", lambda e: e.copy(out=wdb[b][:].rearrange("p k f -> p (k f)"), in_=stg[2][b][:]), reads=["stg2_%d" % b], writes=["wdb%d" % b])
                        for s_ in range(SBLK // 512):
                            steps.append((b, s_, fg == 0))
                        while gu_done < len(steps):
                            emit_gu(steps[gu_done], gu_done)
                            gu_done += 1
                            if dn_done < gu_done - 1:
                                emit_dn(steps[dn_done], dn_done)
                                dn_done += 1
                    while dn_done < len(steps):
                        emit_dn(steps[dn_done], dn_done)
                        dn_done += 1
                    for t in range(8):
                        yb_ = yi % 2
                        yi += 1
                        yb = ybs[yb_]
                        if yb_:
                            P.op("pool", lambda e: e.tensor_copy(out=yb[:], in_=acc[:, :, t * 128:(t + 1) * 128]), reads=["acc"], writes=["yb%d" % yb_])
                        else:
                            P.op("act", lambda e: e.copy(out=yb[:], in_=acc[:, :, t * 128:(t + 1) * 128]), reads=["acc"], writes=["yb%d" % yb_])
                        for c in range(8):
                            bank = 6 + c // 4
                            mm(bank, ps[bank][:, (c % 4) * 128:(c % 4 + 1) * 128], yb[:, c, :], ident_b[:], True, True, ["yb%d" % yb_, "const"])
                        P.op("act", lambda e: e.copy(out=ytok[yb_][:, 0:512], in_=ps[6][:, :]), reads=[PSR[6]], writes=["ytok%d" % yb_])
                        P.op("dve", lambda e: e.tensor_copy(out=ytok[yb_][:, 512:1024], in_=ps[7][:, :]), reads=[PSR[7]], writes=["ytok%d" % yb_])
                        r0 = blk * SBLK + t * 128
                        P.dma("sp", Ys[r0:r0 + 128, :], ytok[yb_][:], reads=["ytok%d" % yb_], writes=["Ys"])
                P.barrier()
            with ExitStack() as es:
                g2rep = sbt(es, [128, 128], F32)
                g2bc = [sbt(es, [128, D], F32) for _ in range(2)]
                o1 = [sbt(es, [128, D], BF16) for _ in range(4)]
                o2 = [sbt(es, [128, D], BF16) for _ in range(4)]
                yf = [sbt(es, [128, D], F32) for _ in range(4)]
                x1 = [sbt(es, [128, 8, 128], F32) for _ in range(4)]
                ot = [sbt(es, [128, D], F32) for _ in range(4)]
                for it in range(2):
                    for c in range(8):
                        P.op("dve", lambda e: e.tensor_scalar(out=g2rep[:], in0=ones_f[:], scalar1=GATE(l, 1, c, COL[it]), scalar2=None, op0=ALU.mult), reads=["const", "mod"], writes=["g2rep"])
                        bank = c // 4
                        P.op("pe", lambda e: e.transpose(out=ps[bank][:, (c % 4) * 128:(c % 4 + 1) * 128], in_=g2rep[:], identity=ident_f[:]), reads=["g2rep", "const"], writes=[PSR[bank]])
                        if c % 4 == 3:
                            P.op("act", lambda e: e.copy(out=g2bc[it][:, bank * 512:(bank + 1) * 512], in_=ps[bank][:, :]), reads=[PSR[bank]], writes=["g2bc"])
                for ti in range(64):
                    it = ti // 32
                    tt = (ti % 32) * 128
                    b = ti % 4
                    P.idma(o1[b][:], Ys[:, :], None, dsl[:, 2 * ti:2 * ti + 1], NSLOT - 1, reads=["Ys", "dsl"], writes=["o1_%d" % b])
                    P.idma(o2[b][:], Ys[:, :], None, dsl[:, 2 * ti + 1:2 * ti + 2], NSLOT - 1, reads=["Ys", "dsl"], writes=["o2_%d" % b])
                    P.dma("sp", x1[b][:], xTd[it].rearrange("c p t -> p c t")[:, :, tt:tt + 128], reads=["xTd%d_%d" % (it, tt // 512)], writes=["x1_%d" % b])
                    P.op("dve", lambda e: e.tensor_scalar(out=yf[b][:], in0=o1[b][:], scalar1=GV[:, ti, 0:1], scalar2=None, op0=ALU.mult), reads=["o1_%d" % b, "GV"], writes=["yf%d" % b])
                    P.op("dve", lambda e: e.scalar_tensor_tensor(out=yf[b][:], in0=o2[b][:], scalar=GV[:, ti, 1:2], in1=yf[b][:], op0=ALU.mult, op1=ALU.add), reads=["o2_%d" % b, "GV", "yf%d" % b], writes=["yf%d" % b])
                    P.op("dve", lambda e: e.tensor_tensor(out=yf[b][:], in0=yf[b][:], in1=g2bc[it][:], op=ALU.mult), reads=["yf%d" % b, "g2bc"], writes=["yf%d" % b])
                    for c in range(8):
                        bank = 2 * b + c // 4
                        P.op("pe", lambda e: e.transpose(out=ps[bank][:, (c % 4) * 128:(c % 4 + 1) * 128], in_=x1[b][:, c, :], identity=ident_f[:]), reads=["x1_%d" % b, "const"], writes=[PSR[bank]])
                    for h_ in range(2):
                        bank = 2 * b + h_
                        P.op("dve", lambda e: e.tensor_tensor(out=ot[b][:, h_ * 512:(h_ + 1) * 512], in0=ps[bank][:, :], in1=yf[b][:, h_ * 512:(h_ + 1) * 512], op=ALU.add), reads=[PSR[bank], "yf%d" % b], writes=["ot%d" % b])
                    P.dma("sp", out[it][tt:tt + 128, :], ot[b][:], reads=["ot%d" % b], writes=["out"])
                P.barrier()

        if upto >= 5:
            blocks0 = [[(0, 0, 2048)], [(0, 2048, 2048)], [(1, 0, 2048)], [(1, 2048, 2048)], [(2, 0, 256), (3, 0, 256)]]
            ffn_phase(0, blocks0, ffn_wg, ffn_wu, ffn_wd, 1, False, False)


        if upto >= 6:
            with ExitStack() as es:
                xts = [sbt(es, [128, 8, 512], F32) for _ in range(2)]
                sq = sbt(es, [128, 8, 512], BF16)
                rs = sbt(es, [128, 512], F32)
                tmp = sbt(es, [128, 8, 512], F32)
                hxs = [sbt(es, [128, 8, 512], BF16) for _ in range(2)]
                hi = 0
                for it in items0:
                    L = LS[it]
                    W = min(512, L)
                    for blk in range(L // W):
                        t0 = blk * W
                        b = hi % 2
                        hi += 1
                        P.dma("sp", xts[b][:, :, :W], xTd[it].rearrange("c p t -> p c t")[:, :, t0:t0 + W], reads=["xTd%d_%d" % (it, blk)], writes=["xt%d" % b])
                        norm_block(es, xts[b], "xt%d" % b, W, 1, 0, COL[it], hxs[b], "hx%d" % b, (sq, rs, tmp))
                        P.dma("pool", hTd[it].rearrange("c p t -> p c t")[:, :, t0:t0 + W], hxs[b][:, :, :W], reads=["hx%d" % b], writes=["hTd%d_%d" % (it, blk)])
                P.barrier()
            esQ = ExitStack()
            wq_b = load_cast_w(esQ, w_qkv.rearrange("(k p) n -> p k n", p=128), 8, 1536, "wq_b")
            for it in (0, 1):
                ci_ = it + 2
                L = SEQ
                nK = L // 128
                with ExitStack() as esI:
                    hxT = sbt(esI, [128, 8, L], BF16)
                    hcT = sbt(esI, [128, 8, CTX], BF16)
                    cosT = sbt(esI, [64, L], BF16)
                    sinT = sbt(esI, [64, L], BF16)
                    RT = sbt(esI, [64, 128], BF16)
                    qkgs = sbt(esI, [64, 2], F32)
                    esink = sbt(esI, [64, 16], F32)
                    mkp = sbt(esI, [128, 512], BF16)
                    mkn = sbt(esI, [128, 512], BF16)
                    mstage = sbt(esI, [128, 512], F32)
                    kT = sbt(esI, [64, L + CTX], BF16)
                    Vt = sbt(esI, [128, nK + 2, 128], BF16)
                    qT = sbt(esI, [64, 4, L], BF16)
                    ET = [[sbt(esI, [128, 512], BF16) for _ in range(5)] for _ in range(2)]
                    dns = [sbt(esI, [64, 512], F32) for _ in range(2)]
                    oT = [sbt(esI, [64, 512], BF16) for _ in range(2)]
                    P.dma("sp", hxT[:], hTd[it].rearrange("c p t -> p c t"), reads=["hTd%d_%d" % (it, b_) for b_ in range(8)], writes=["hxT"])
                    P.dma("sp", hcT[:], hTd[ci_].rearrange("c p t -> p c t"), reads=["hTd%d_0" % ci_], writes=["hcT"])
                    P.dma("sp", cosT[:], cst["rope_cos"][:, :], writes=["ropec"])
                    P.dma("sp", sinT[:], cst["rope_sin"][:, :], writes=["ropec"])
                    P.dma("sp", RT[:], cst["ropeRT"][:, :], writes=["ropec"])
                    P.dma("sp", qkgs[:], qkg[:, :], writes=["ropec"])
                    P.dma("sp", esink[:], sinkr[:, :], writes=["esink"])
                    P.op("act", lambda e: e.activation(out=esink[:], in_=esink[:], func=AF.Exp), reads=["esink"], writes=["esink"])
                    P.dma("sp", mstage[:], cst["maskp"].rearrange("p a b -> p (a b)"), writes=["mstage"])
                    P.op("dve", lambda e: e.tensor_copy(out=mkp[:], in_=mstage[:]), reads=["mstage"], writes=["mkp"])
                    P.dma("sp", mstage[:], cst["maskn"].rearrange("p a b -> p (a b)"), reads=[], writes=["mstage"])
                    P.op("dve", lambda e: e.tensor_copy(out=mkn[:], in_=mstage[:]), reads=["mstage"], writes=["mkn"])

                    sqqs = [sbt(esI, [64, 512], BF16) for _ in range(3)]
                    rsqs = [sbt(esI, [64, 512], F32) for _ in range(2)]
                    qns = [sbt(esI, [64, 512], BF16) for _ in range(2)]
                    r1s = [sbt(esI, [64, 512], F32) for _ in range(2)]
                    r2s = [sbt(esI, [64, 512], F32) for _ in range(2)]

                    def stA(u, ui):
                        (src, rsrc, c0, W, col0, gcol, rope, dest, rdest, vinfo) = u
                        pa = ui % 3
                        for k in range(8):
                            mm(pa, ps[pa][:, :W], wq_b[:, k, col0:col0 + 128], src[:, k, c0:c0 + W], k == 0, k == 7, ["wq_b", rsrc])
                        P.op("act", lambda e: e.activation(out=sqqs[pa][:, :W], in_=ps[pa][0:64, :W], func=AF.Square), reads=[PSR[pa]], writes=["sqq%d" % pa])
                        if vinfo is not None:
                            g_, vch0 = vinfo
                            nt = W // 128
                            for t in range(nt):
                                for k in range(8):
                                    mm(7, ps[7][:, t * 64:(t + 1) * 64], src[:, k, c0 + t * 128:c0 + (t + 1) * 128], wq_b[:, k, 1280 + g_ * 64:1280 + (g_ + 1) * 64], k == 0, k == 7, ["wq_b", rsrc])
                            P.op("act", lambda e: e.copy(out=Vt[:, vch0:vch0 + nt, 0:64], in_=ps[7][:, 0:nt * 64].rearrange("p (a b) -> p a b", b=64)), reads=[PSR[7]], writes=["Vt"])
                            P.op("dve", lambda e: e.tensor_copy(out=Vt[:, vch0:vch0 + nt, 64:128], in_=ps[7][:, 0:nt * 64].rearrange("p (a b) -> p a b", b=64)), reads=[PSR[7]], writes=["Vt"])

                    def stB(u, ui):
                        (src, rsrc, c0, W, col0, gcol, rope, dest, rdest, vinfo) = u
                        pa = ui % 3
                        p_ = ui % 2
                        bs = 3 + p_
                        mm(bs, ps[bs][:, :W], ones_b[0:64, :], sqqs[pa][:, :W], True, True, ["sqq%d" % pa, "const"])
                        P.op("act", lambda e: e.activation(out=rsqs[p_][:, :W], in_=ps[bs][0:64, :W], func=AF.Ln, bias=epst[0:64, 0:1], scale=1.0 / 64), reads=[PSR[bs], "const"], writes=["rsq%d" % p_])
                        P.op("act", lambda e: e.activation(out=rsqs[p_][:, :W], in_=rsqs[p_][:, :W], func=AF.Exp, scale=-0.5), reads=["rsq%d" % p_], writes=["rsq%d" % p_])
                        P.op("dve", lambda e: e.scalar_tensor_tensor(out=qns[p_][:, :W], in0=ps[pa][0:64, :W], scalar=qkgs[:, gcol:gcol + 1], in1=rsqs[p_][:, :W], op0=ALU.mult, op1=ALU.mult),
                             reads=[PSR[pa], "rsq%d" % p_, "ropec"], writes=["qn%d" % p_])

                    def stC(u, ui):
                        (src, rsrc, c0, W, col0, gcol, rope, dest, rdest, vinfo) = u
                        p_ = ui % 2
                        br = 5 + p_
                        if rope:
                            mm(br, ps[br][:, :W], RT[:], qns[p_][:, :W], True, True, ["qn%d" % p_, "ropec"])
                            P.op("pool", lambda e: e.tensor_tensor(out=r1s[p_][:, :W], in0=qns[p_][:, :W], in1=cosT[:, c0:c0 + W], op=ALU.mult), reads=["qn%d" % p_, "ropec"], writes=["r1%d" % p_])
                            P.op("dve", lambda e: e.tensor_tensor(out=r2s[p_][:, :W], in0=ps[br][0:64, :W], in1=sinT[:, c0:c0 + W], op=ALU.mult), reads=[PSR[br], "ropec"], writes=["r2%d" % p_])
                            P.op("pool", lambda e: e.tensor_tensor(out=dest, in0=r1s[p_][:, :W], in1=r2s[p_][:, :W], op=ALU.add), reads=["r1%d" % p_, "r2%d" % p_], writes=[rdest])
                        else:
                            P.op("pool", lambda e: e.tensor_copy(out=dest, in_=qns[p_][:, :W]), reads=["qn%d" % p_], writes=[rdest])

                    for g in range(4):
                        units = []
                        for b_ in range(L // 512):
                            c0 = b_ * 512
                            units.append((hxT, "hxT", c0, 512, 1024 + g * 64, 1, True, kT[:, c0:c0 + 512], "kT", (g, c0 // 128)))
                            for j in range(4):
                                units.append((hxT, "hxT", c0, 512, (4 * g + j) * 64, 0, True, qT[:, j, c0:c0 + 512], "qT", None))
                        units.append((hcT, "hcT", 0, CTX, 1024 + g * 64, 1, False, kT[:, L:L + CTX], "kT", (g, L // 128)))
                        nu = len(units)
                        for t_ in range(nu + 2):
                            if t_ < nu:
                                stA(units[t_], t_)
                            if 0 <= t_ - 1 < nu:
                                stB(units[t_ - 1], t_ - 1)
                            if 0 <= t_ - 2 < nu:
                                stC(units[t_ - 2], t_ - 2)

                        def chunks_of(i):
                            ch = []
                            if i > 0:
                                ch.append((i - 1, mkp, "mkp"))
                            ch.append((i, None, None))
                            if i < nK - 1:
                                ch.append((i + 1, mkn, "mkn"))
                            ch.append((nK, None, None))
                            ch.append((nK + 1, None, None))
                            return ch

                        def stS(i):
                            eb = i % 2
                            for ci, (kc, mk, rmk) in enumerate(chunks_of(i)):
                                sb_ = ci % 4
                                mm(sb_, ps[sb_][:, :].rearrange("p (a b) -> p a b", b=128), kT[:, kc * 128:(kc + 1) * 128], qT[:, :, i * 128:(i + 1) * 128], True, True, ["kT", "qT"])
                                P.op("act", lambda e: e.activation(out=ET[eb][ci][:], in_=ps[sb_][:, :], func=AF.Exp, scale=0.125), reads=[PSR[sb_]], writes=["ET%d_%d" % (eb, ci)])
                                if mk is not None:
                                    P.op("pool", lambda e: e.tensor_tensor(out=ET[eb][ci][:], in0=ET[eb][ci][:], in1=mk[:], op=ALU.mult), reads=["ET%d_%d" % (eb, ci), rmk], writes=["ET%d_%d" % (eb, ci)])

                        def stR(i):
                            eb = i % 2
                            chunks = chunks_of(i)
                            nch = len(chunks)
                            bo, bd = (4, 5) if eb == 0 else (6, 7)
                            dn = dns[eb]
                            for ci, (kc, mk, rmk) in enumerate(chunks):
                                mm(bo, ps[bo][:, :], Vt[:, kc, :], ET[eb][ci][:], ci == 0, ci == nch - 1, ["Vt", "ET%d_%d" % (eb, ci)])
                            for ci, (kc, mk, rmk) in enumerate(chunks):
                                mm(bd, ps[bd][:, :], ones_b[:, :], ET[eb][ci][:], ci == 0, ci == nch - 1, ["const", "ET%d_%d" % (eb, ci)])
                            for j in range(4):
                                h = 4 * g + j
                                P.op("act", lambda e: e.activation(out=dn[:, j * 128:(j + 1) * 128], in_=ps[bd][0:64, j * 128:(j + 1) * 128], func=AF.Ln, bias=esink[:, h:h + 1], scale=1.0),
                                     reads=[PSR[bd], "esink"], writes=["dn%d" % eb])
                            P.op("act", lambda e: e.activation(out=dn[:], in_=dn[:], func=AF.Exp, scale=-1.0), reads=["dn%d" % eb], writes=["dn%d" % eb])
                            P.op("dve", lambda e: e.tensor_tensor(out=oT[eb][:], in0=ps[bo][0:64, :], in1=dn[:], op=ALU.mult), reads=[PSR[bo], "dn%d" % eb], writes=["oT%d" % eb])
                            P.dma("pool", attTd[it].rearrange("h p t -> p h t")[:, 4 * g:4 * g + 4, i * 128:(i + 1) * 128], oT[eb][:].rearrange("p (a b) -> p a b", b=128),
                                  reads=["oT%d" % eb], writes=["attTd%d" % it])

                        stS(0)
                        for i in range(nK):
                            if i + 1 < nK:
                                stS(i + 1)
                            stR(i)
                    P.barrier()
            esQ.close()
            holder1 = {}

            def cl1(es):
                att = sbt(es, [64, 16, 512], BF16)
                holder1["att"] = att

                def load(it, t0, W):
                    P.dma("sp", att[:, :, :W], attTd[it].rearrange("h p t -> p h t")[:, :, t0:t0 + W], reads=["attTd%d" % it], writes=["att"])
                    return "att"
                return load
            kparts1 = [((lambda wb, n, h=h: wb[0:64, h, n * 128:(n + 1) * 128]), (lambda W, h=h: holder1["att"][:, h, :W])) for h in range(16)]
            outproj_phase(1, [0, 1], w_out1.rearrange("(h p) n -> p h n", p=64), kparts1, cl1, router=True, npart=64)
            if upto >= 7:
                moe_sparse(1)

        P.barrier()
        print("instructions:", P.ninst, flush=True)
    return nc


def _core_inputs(inp, core):
    b0 = 2 * core
    f32 = np.float32
    m = {}
    m["x"] = np.ascontiguousarray(inp["x"][b0:b0 + 2])
    m["ctx"] = np.ascontiguousarray(inp["ctx"][b0:b0 + 2])
    cc = np.stack([inp["c"][b0], inp["c"][b0 + 1], inp["c_ctx"]], axis=-1)
    m["cT"] = np.ascontiguousarray(cc.reshape(8, 128, 3).transpose(1, 0, 2)).astype(f32)
    m["ada_w"] = inp["ada_w"]
    m["ada_bT"] = np.ascontiguousarray(inp["ada_b"].reshape(2, 48, 128).transpose(2, 0, 1))
    m["n1g"] = np.ascontiguousarray(inp["norm1_g"].reshape(2, 8, 128).transpose(2, 0, 1))
    m["n2g"] = np.ascontiguousarray(inp["norm2_g"].reshape(2, 8, 128).transpose(2, 0, 1))
    m["ev_w_in"] = inp["ev_w_in"][0]
    m["ev_w_out"] = inp["ev_w_out"][0]
    m["cw"] = np.ascontiguousarray(inp["hy_conv_w"][0].reshape(3, 12, 128).transpose(2, 1, 0))
    m["cb"] = np.ascontiguousarray(inp["hy_conv_b"][0].reshape(12, 128).T)
    m["hyb"] = np.ascontiguousarray(inp["hy_bias"][0].reshape(4, 128).T)
    m["hf_w0"] = inp["hf_w0"][0]
    m["hf_w1"] = inp["hf_w1"][0]
    m["hf_w2"] = inp["hf_w2"][0]
    m["hf_w3"] = inp["hf_w3"][0]
    m["hf_vec"] = np.ascontiguousarray(np.stack([inp["hf_b0"][0], inp["hf_b1"][0], inp["hf_b2"][0], inp["hf_freq"][0]], axis=-1))
    m["ffn_w_gate"] = inp["ffn_w_gate"]
    m["ffn_w_up"] = inp["ffn_w_up"]
    m["ffn_w_down"] = inp["ffn_w_down"]
    m["od_w_qkv"] = inp["od_w_qkv"][0]
    m["od_w_out"] = inp["od_w_out"][0]
    m["qkg"] = np.ascontiguousarray(np.stack([inp["q_norm_g"][0], inp["k_norm_g"][0]], axis=-1))
    m["sinkr"] = np.ascontiguousarray(np.broadcast_to(inp["attn_sink"][0][None, :], (64, 16)))
    m["routerT"] = np.ascontiguousarray(inp["moe_router"][0].reshape(8, 128, 8).transpose(1, 0, 2))
    m["moe_w_gate"] = inp["_moe_wg_t"]
    m["moe_w_up"] = inp["_moe_wu_t"]
    m["moe_w_down"] = inp["_moe_wd_t"]
    for k, v in _CONSTS.items():
        m["k_" + k] = v
    return {k: np.ascontiguousarray(v) for k, v in m.items()}


def kernel(**inputs):
    global _CONSTS
    if _CONSTS is None:
        _CONSTS = host_consts()
    inp = {k: np.asarray(v) for k, v in inputs.items()}
    inp["_moe_wg_t"] = np.ascontiguousarray(inp["moe_w_gate"][0].reshape(NE, 8, 128, NFG, 256).transpose(0, 3, 2, 1, 4)).reshape(NE * NFG * 128, 2048)
    inp["_moe_wu_t"] = np.ascontiguousarray(inp["moe_w_up"][0].reshape(NE, 8, 128, NFG, 256).transpose(0, 3, 2, 1, 4)).reshape(NE * NFG * 128, 2048)
    inp["_moe_wd_t"] = np.ascontiguousarray(inp["moe_w_down"][0].reshape(NE, NFG, 2, 128, D).transpose(0, 1, 3, 2, 4)).reshape(NE * NFG * 128, 2048)
    nc = build()
    in_maps = [_core_inputs(inp, c) for c in range(8)]
    res = run_bass_kernel_spmd(nc, in_maps, core_ids=list(range(8)))
    return np.concatenate([r["out"] for r in res.results], axis=0).astype(np.float32)
```
